# Optimizing a Trainium2 kernel written in Bass

```python
import math
import jax
import jax.numpy as jnp
from jax import lax
import numpy as np

D_MODEL = 1024
BATCH = 4
SEQ = 8192
DEPTH = 4

MEM_LEN = 256
GRID_W = 64
CHUNK = 128
Q_BLOCK = 128
EPS = 1e-6
GM_GROUPS = 4
GM_GROUP_DIM = 128
GM_WIDTH = GM_GROUPS * GM_GROUP_DIM
HEAD_DIM = 64
N_Q_HEADS = 8
N_KV_HEADS = 2
Q_PER_KV = N_Q_HEADS // N_KV_HEADS
ATT_WIDTH = N_Q_HEADS * HEAD_DIM
KV_WIDTH = N_KV_HEADS * HEAD_DIM
ROPE_THETA = 10000.0
MIX_WIDTH = GM_WIDTH + ATT_WIDTH
IN_WIDTH = 2 * GM_WIDTH + ATT_WIDTH + 2 * KV_WIDTH
SPLITS = (GM_WIDTH, 2 * GM_WIDTH, 2 * GM_WIDTH + ATT_WIDTH, 2 * GM_WIDTH + ATT_WIDTH + KV_WIDTH)
X_HEADS = 4
X_HEAD_DIM = D_MODEL // X_HEADS
N_EXPERTS = 16
EC_FACTOR = 2
EXPERT_FF = 1024

kernel_name = 'hybrid_gmlp_gqa_ec_encoder'


def rms_norm(x, g):
    xf = x.astype(jnp.float32)
    y = xf * lax.rsqrt(jnp.mean(xf * xf, axis=-1, keepdims=True) + EPS)
    return (y * g.astype(jnp.float32)).astype(x.dtype)


def axial_rope_tables(seq_len):
    rows = seq_len // GRID_W
    row_id = jnp.repeat(jnp.arange(rows, dtype=jnp.float32), GRID_W)
    col_id = jnp.tile(jnp.arange(GRID_W, dtype=jnp.float32), rows)
    n_pairs = HEAD_DIM // 4
    freqs = jnp.exp(-math.log(ROPE_THETA) * jnp.arange(n_pairs, dtype=jnp.float32) / n_pairs)
    ang = jnp.concatenate([row_id[:, None] * freqs[None, :], col_id[:, None] * freqs[None, :]], axis=-1)
    return jnp.cos(ang), jnp.sin(ang)


def apply_rope(x, cos, sin):
    b, s, h, d = x.shape
    xf = x.astype(jnp.float32).reshape(b, s, h, d // 2, 2)
    x0, x1 = xf[..., 0], xf[..., 1]
    c = cos[None, :, None, :]
    sn = sin[None, :, None, :]
    out = jnp.stack([x0 * c - x1 * sn, x0 * sn + x1 * c], axis=-1)
    return out.reshape(b, s, h, d).astype(x.dtype)


def gmlp_group(u, v, v_norm_g, w_s, b_s):
    b, s, _ = u.shape
    v = rms_norm(v, v_norm_g).reshape(b, s // CHUNK, CHUNK, GM_GROUPS, GM_GROUP_DIM)
    mixed = jnp.einsum('gij,bnjgc->bnigc', w_s, v) + b_s.T[None, None, :, :, None]
    return u * mixed.reshape(b, s, GM_WIDTH)


def gqa_group(q, k, v, q_norm_g, k_norm_g, cos, sin):
    b, s, _ = q.shape
    q = q.reshape(b, s, N_Q_HEADS, HEAD_DIM)
    k = k.reshape(b, s, N_KV_HEADS, HEAD_DIM)
    v = v.reshape(b, s, N_KV_HEADS, HEAD_DIM)
    q = apply_rope(rms_norm(q, q_norm_g), cos, sin) * (HEAD_DIM ** -0.5)
    k = apply_rope(rms_norm(k, k_norm_g), cos, sin)
    n_blk = s // Q_BLOCK
    q_blocks = q.reshape(b, n_blk, Q_BLOCK, N_KV_HEADS, Q_PER_KV, HEAD_DIM).transpose(1, 0, 3, 4, 2, 5)

    def attend(qb):
        sc = jnp.einsum('bkgqd,bskd->bkgqs', qb, k, preferred_element_type=jnp.float32)
        p = jax.nn.softmax(sc, axis=-1).astype(v.dtype)
        return jnp.einsum('bkgqs,bskd->bkgqd', p, v)

    o = lax.map(attend, q_blocks)
    return o.transpose(1, 0, 4, 2, 3, 5).reshape(b, s, ATT_WIDTH)


def memory_cross_attention(h, mem_n, w_q, w_kv, w_o):
    b, s, _ = h.shape
    m = mem_n.shape[1]
    q = (h @ w_q).reshape(b, s, X_HEADS, X_HEAD_DIM) * (X_HEAD_DIM ** -0.5)
    kv = (mem_n @ w_kv).reshape(b, m, 2, X_HEADS, X_HEAD_DIM)
    k, v = kv[:, :, 0], kv[:, :, 1]
    sc = jnp.einsum('bshd,bmhd->bhsm', q, k, preferred_element_type=jnp.float32)
    p = jax.nn.softmax(sc, axis=-1).astype(v.dtype)
    o = jnp.einsum('bhsm,bmhd->bshd', p, v).reshape(b, s, D_MODEL)
    return o @ w_o


def expert_choice_moe(h, w_router, w_gate, w_up, w_down):
    b, s, _ = h.shape
    cap = EC_FACTOR * s // N_EXPERTS
    logits = jnp.einsum('bsd,de->bse', h, w_router, preferred_element_type=jnp.float32)
    aff = jax.nn.softmax(logits, axis=-1)
    gates, idx = lax.top_k(aff.transpose(0, 2, 1), cap)
    b_ix = jnp.arange(b)[:, None, None]
    xs = h[b_ix, idx]
    a = jnp.einsum('becd,edf->becf', xs, w_gate)
    u = jnp.einsum('becd,edf->becf', xs, w_up)
    y = jnp.einsum('becf,efd->becd', jax.nn.silu(a) * u, w_down)
    y = y * gates[..., None].astype(y.dtype)
    return jnp.zeros_like(h).at[b_ix, idx].add(y)


def setup_inputs(seed: int = 0) -> dict:
    key = jax.random.key(seed)
    ks = jax.random.split(key, 24)

    def nrm(k, shape, scale):
        return jax.random.normal(k, shape, jnp.float32) * scale

    def gain(k, shape):
        return 1.0 + 0.05 * jax.random.normal(k, shape, jnp.float32)

    L, D = DEPTH, D_MODEL
    return {
        'x': nrm(ks[0], (BATCH, SEQ, D), 1.0),
        'mem': nrm(ks[1], (BATCH, MEM_LEN, D), 1.0),
        'mix_norm_g': gain(ks[2], (L, D)),
        'w_in': nrm(ks[3], (L, D, IN_WIDTH), D ** -0.5),
        'gm_v_norm_g': gain(ks[4], (L, GM_WIDTH)),
        'gm_w_s': nrm(ks[5], (L, GM_GROUPS, CHUNK, CHUNK), CHUNK ** -0.5),
        'gm_b_s': gain(ks[6], (L, GM_GROUPS, CHUNK)),
        'q_norm_g': gain(ks[7], (L, HEAD_DIM)),
        'k_norm_g': gain(ks[8], (L, HEAD_DIM)),
        'branch_norm_g': gain(ks[9], (L, 2, GM_WIDTH)),
        'w_out': nrm(ks[10], (L, MIX_WIDTH, D), MIX_WIDTH ** -0.5),
        'xattn_norm_g': gain(ks[11], (L, D)),
        'mem_norm_g': gain(ks[12], (D,)),
        'xattn_w_q': nrm(ks[13], (L, D, D), D ** -0.5),
        'xattn_w_kv': nrm(ks[14], (L, D, 2 * D), D ** -0.5),
        'xattn_w_o': nrm(ks[15], (L, D, D), D ** -0.5),
        'ffn_norm_g': gain(ks[16], (L, D)),
        'w_router': nrm(ks[17], (L, D, N_EXPERTS), D ** -0.5),
        'w_gate': nrm(ks[18], (L, N_EXPERTS, D, EXPERT_FF), D ** -0.5),
        'w_up': nrm(ks[19], (L, N_EXPERTS, D, EXPERT_FF), D ** -0.5),
        'w_down': nrm(ks[20], (L, N_EXPERTS, EXPERT_FF, D), EXPERT_FF ** -0.5),
        'final_norm_g': gain(ks[21], (D,)),
    }


def reference(x, mem, mix_norm_g, w_in, gm_v_norm_g, gm_w_s, gm_b_s, q_norm_g, k_norm_g,
              branch_norm_g, w_out, xattn_norm_g, mem_norm_g, xattn_w_q, xattn_w_kv, xattn_w_o,
              ffn_norm_g, w_router, w_gate, w_up, w_down, final_norm_g):
    cos, sin = axial_rope_tables(x.shape[1])
    mem_n = rms_norm(mem, mem_norm_g)
    for l in range(DEPTH):
        h = rms_norm(x, mix_norm_g[l])
        proj = h @ w_in[l]
        u, v, q, k, vv = jnp.split(proj, SPLITS, axis=-1)
        gm = gmlp_group(jax.nn.gelu(u), jax.nn.gelu(v), gm_v_norm_g[l], gm_w_s[l], gm_b_s[l])
        at = gqa_group(q, k, vv, q_norm_g[l], k_norm_g[l], cos, sin)
        merged = jnp.concatenate([rms_norm(gm, branch_norm_g[l, 0]), rms_norm(at, branch_norm_g[l, 1])], axis=-1)
        x = x + merged @ w_out[l]
        x = x + memory_cross_attention(rms_norm(x, xattn_norm_g[l]), mem_n,
                                       xattn_w_q[l], xattn_w_kv[l], xattn_w_o[l])
        x = x + expert_choice_moe(rms_norm(x, ffn_norm_g[l]), w_router[l], w_gate[l], w_up[l], w_down[l])
    return rms_norm(x, final_norm_g)
```

```python
import numpy as np
import ml_dtypes
from contextlib import ExitStack
import concourse.bass as bass
import concourse.mybir as mybir
from concourse.bass_utils import run_bass_kernel_spmd

F32 = mybir.dt.float32
BF16 = mybir.dt.bfloat16
I32 = mybir.dt.int32
AF = mybir.ActivationFunctionType
ALU = mybir.AluOpType
AX = mybir.AxisListType

D = 1024
MEM = 256
NE = 16
EPS = 1e-6
GELU = AF.Gelu_apprx_tanh
ENGS = ['tensor', 'vector', 'scalar', 'gpsimd', 'sync']
EPOCH = 20000
DEPOCH = 1200
DK = 8


class Prog:
    def __init__(self, nc, stack):
        self.nc = nc
        self.stack = stack
        self.rec = {e: [] for e in ENGS}
        self.cnt = {e: 0 for e in ENGS}
        self.esem = {e: None for e in ENGS}
        self.nsem = 0
        self.dring = {e: [None] * DK for e in ENGS}
        self.duse = {e: [0] * DK for e in ENGS}
        self.dn = {e: 0 for e in ENGS}
        self.waited = {e: {} for e in ENGS}
        self.lastw = {}
        self.readers = {}
        self.ninst = 0

    def newsem(self, tag):
        self.nsem += 1
        return self.stack.enter_context(self.nc.semaphore(f"{tag}_{self.nsem}"))

    def _wait(self, eng, tok):
        sem, val = tok[0], tok[1]
        w = self.waited[eng]
        if w.get(id(sem), 0) >= val:
            return
        w[id(sem)] = val
        self.rec[eng].append(lambda e, s=sem, v=val: e.wait_ge(s, v))

    def op(self, eng, fn, r=(), w=(), dma=False):
        toks = []
        for k in r:
            t = self.lastw.get(k)
            if t is not None:
                toks.append(t)
        for k in w:
            t = self.lastw.get(k)
            if t is not None:
                toks.append(t)
            toks.extend(self.readers.get(k, {}).values())
        for t in toks:
            if t[2] == 'tensor' and eng == 'tensor' and not dma:
                continue
            self._wait(eng, t)
        self.ninst += 1
        if dma:
            slot = self.dn[eng] % DK
            self.dn[eng] += 1
            sem = self.dring[eng][slot]
            prev = self.duse[eng][slot]
            if sem is not None and prev > 0:
                self._wait(eng, (sem, 16 * prev))
            if sem is None or prev >= DEPOCH:
                sem = self.newsem('d' + eng[:2])
                self.dring[eng][slot] = sem
                prev = 0
            self.duse[eng][slot] = prev + 1
            tok = (sem, 16 * (prev + 1), 'dma')
            self.rec[eng].append(lambda e, f=fn, s=sem: f(e).then_inc(s, 16))
        else:
            if self.esem[eng] is None or self.cnt[eng] >= EPOCH:
                self.esem[eng] = self.newsem('e' + eng[:2])
                self.cnt[eng] = 0
            self.cnt[eng] += 1
            sem = self.esem[eng]
            tok = (sem, self.cnt[eng], eng)
            self.rec[eng].append(lambda e, f=fn, s=sem: f(e).then_inc(s, 1))
        for k in r:
            self.readers.setdefault(k, {})[id(tok[0])] = tok
        for k in w:
            self.lastw[k] = tok
            self.readers[k] = {}
        return tok

    def wait_all(self, eng, keys):
        for k in keys:
            t = self.lastw.get(k)
            if t is not None:
                self._wait(eng, t)

    def barrier(self):
        toks = []
        for e in ENGS:
            if self.esem[e] is not None and self.cnt[e] > 0:
                toks.append((self.esem[e], self.cnt[e], e))
            for slot in range(DK):
                sem = self.dring[e][slot]
                if sem is not None and self.duse[e][slot] > 0:
                    toks.append((sem, 16 * self.duse[e][slot], 'dma'))
        for e in ENGS:
            for t in toks:
                self._wait(e, t)

    def emit(self):
        nc = self.nc
        with nc.Block() as block:
            @block.tensor
            def _(e):
                for f in self.rec['tensor']:
                    f(e)

            @block.vector
            def _(e):
                for f in self.rec['vector']:
                    f(e)

            @block.scalar
            def _(e):
                for f in self.rec['scalar']:
                    f(e)

            @block.gpsimd
            def _(e):
                for f in self.rec['gpsimd']:
                    f(e)

            @block.sync
            def _(e):
                for f in self.rec['sync']:
                    f(e)


class Ctx:
    pass


def build(S, L, dbg=False, phases=('B', 'C', 'A', 'D', 'E')):
    NT = S // 128
    NG = S // 512
    CAP = 2 * S // NE
    nc = bass.Bass("TRN2", target_bir_lowering=False)
    stack = ExitStack()
    P = Prog(nc, stack)
    c = Ctx()
    c.nc, c.P, c.S, c.L, c.NT, c.NG, c.CAP = nc, P, S, L, NT, NG, CAP

    def din(name, shape, dt=F32):
        return nc.dram_tensor(name, list(shape), dt, kind="ExternalInput").ap()

    c.x_in = din('x', [S, D])
    c.mem = din('mem', [MEM, D])
    c.out = nc.dram_tensor('out', [S, D], F32, kind="ExternalOutput").ap()
    c.final_g = din('final_norm_g', [1, D])

    def sb(name, shape, dt=F32):
        return nc.alloc_sbuf_tensor(name, list(shape), dt)

    def ps(name, shape, dt=F32):
        return nc.alloc_psum_tensor(name, list(shape), dt)

    c.sb, c.ps = sb, ps

    def dma(out, in_, r, w, q='sync', **kw):
        return P.op(q, lambda e: e.dma_start(out=out, in_=in_, **kw), r, w, dma=True)

    def act(out, in_, func, r, w, **kw):
        return P.op('scalar', lambda e: e.activation(out=out, in_=in_, func=func, **kw), r, w)

    def mm(out, lhsT, rhs, start, stop, r, w):
        return P.op('tensor', lambda e: e.matmul(out, lhsT, rhs, start=start, stop=stop), r, w)

    def tr(out, in_, ident, r, w):
        return P.op('tensor', lambda e: e.transpose(out, in_, ident), r, w)

    def V(fn, r, w, eng='vector'):
        return P.op(eng, fn, r, w)

    c.dma, c.act, c.mm, c.tr, c.V = dma, act, mm, tr, V

    def rstd_from_ssq(out, ssq, n, r, w, eng='vector'):
        V(lambda e: e.tensor_scalar(out=out, in0=ssq, scalar1=1.0 / n, scalar2=EPS,
                                    op0=ALU.mult, op1=ALU.add), r, w)
        P.op('scalar', lambda e: e.sqrt(out=out, in_=out), w, w)
        V(lambda e: e.reciprocal(out=out, in_=out), w, w)

    c.rstd_from_ssq = rstd_from_ssq

    def dram(name, shape, dt):
        return nc.dram_tensor(name, list(shape), dt, kind=("ExternalOutput" if dbg else "Internal")).ap()

    X = dram('Xs', [S, D], F32)
    GMT = dram('GMT', [4, 128, S], BF16)
    QT = dram('QT', [4, 128, S], BF16)
    H3T = dram('H3T', [8, 128, S], BF16)
    W = {}
    for nm, shp in [('mix_norm_g', [L, D]), ('w_in', [L, D, 1792]), ('gm_v_norm_g', [L, 512]),
                    ('gm_w_s', [L, 4, 128, 128]), ('gm_b_s', [L, 4, 128]), ('q_norm_g', [L, 64]),
                    ('k_norm_g', [L, 64]), ('branch_norm_g', [L, 2, 512]), ('w_out', [L, D, D]),
                    ('xattn_norm_g', [L, D]), ('mem_norm_g', [1, D]), ('xattn_w_q', [L, D, D]),
                    ('xattn_w_kv', [L, D, 2 * D]), ('xattn_w_o', [L, D, D]), ('ffn_norm_g', [L, D]),
                    ('w_router', [L, D, NE]), ('w_gate', [L, NE, D, D]), ('w_up', [L, NE, D, D]),
                    ('w_down', [L, NE, D, D]), ('rope', [S, 128]), ('identf', [128, 128])]:
        W[nm] = din(nm, shp)
    W['identb'] = din('identb', [128, 128], BF16)

    F4 = [sb(f'F4{i}', [128, 1024]) for i in range(4)]
    F2 = [sb(f'F2{i}', [128, 512]) for i in range(5)]
    H8 = [sb(f'H8{i}', [128, 4096], BF16) for i in range(3)]
    H4 = [sb(f'H4{i}', [128, 2048], BF16) for i in range(2)]
    H2 = [sb(f'H2{i}', [128, 1024], BF16) for i in range(4)]
    WBt = sb('WB', [128, 4 * 8192], BF16)
    STG = [sb(f'STG{i}', [128, 2048]) for i in range(2)]
    RES = sb('RES', [128, 8704])
    SM = sb('SM', [128, 256])
    identb = sb('identb_s', [128, 128], BF16)
    identf = sb('identf_s', [128, 128])
    onesf = sb('onesf', [128, 128])
    onesb = sb('onesb', [128, 128], BF16)
    GV = sb('gvbc', [128, 512])
    GQ = sb('gqbc', [128, 64])
    GK = sb('gkbc', [128, 64])
    CV = sb('colv', [128, 64])
    ROPE = [sb(f'rope{i}', [128, 128]) for i in range(2)]
    PB = [ps(f'PB{i}', [128, 512]) for i in range(8)]

    def WB(k):
        return WBt[:, k * 8192:(k + 1) * 8192]

    KT = RES[:, 0:S // 2].bitcast(BF16)
    VAUG = RES[:, S // 2:S // 2 + NT * 65].bitcast(BF16).rearrange("p (t h d) -> p t h d", t=NT, h=2)
    AFF = RES[:, 0:NT * 16].rearrange("p (t e) -> p t e", e=16)
    MG = RES[:, NT * 16:2 * NT * 16].rearrange("p (t e) -> p t e", e=16)
    RSTDGM = SM[:, 0:NT]

    dma(identb[:], W['identb'], [], ['identb'])
    dma(identf[:], W['identf'], [], ['identf'])
    V(lambda e: e.memset(onesf[:], 1.0), [], ['onesf'], 'gpsimd')
    V(lambda e: e.memset(onesb[:], 1.0), [], ['onesb'], 'gpsimd')
    for i in range(0, S, 1024):
        j = min(S, i + 1024)
        dma(X[i:j, :], c.x_in[i:j, :], [], ['X'])

    stg_n = [0]

    def load_w(dst3, dkeys, src2, KC, N, rowscale=None, mul=1.0, part=128, rkeys=()):
        cols = max(1, 2048 // KC)
        for n0 in range(0, N, cols):
            n1 = min(N, n0 + cols)
            k = stg_n[0] % 2
            stg_n[0] += 1
            stv = STG[k][0:part, 0:KC * (n1 - n0)].rearrange("p (c n) -> p c n", c=KC)
            dma(stv, src2[:, n0:n1].rearrange("(c p) n -> p c n", p=part), [], [f'STG{k}'])
            if rowscale is None:
                V(lambda e, stv=stv, n0=n0, n1=n1: e.tensor_copy(out=dst3[:, :, n0:n1], in_=stv),
                  [f'STG{k}'], dkeys, 'gpsimd')
            else:
                for cc in range(KC):
                    V(lambda e, stv=stv, n0=n0, n1=n1, cc=cc: e.tensor_scalar(
                        out=dst3[:, cc, n0:n1], in0=stv[:, cc, :], scalar1=rowscale[:, cc:cc + 1],
                        scalar2=mul, op0=ALU.mult, op1=ALU.mult),
                      [f'STG{k}'] + list(rkeys), dkeys, 'gpsimd')

    def colvec(dst, src1d, p, key):
        P.op('sync', lambda e: e.dma_start(out=dst, in_=src1d.rearrange("(c p) -> p c", p=p),
                                           allow_slow_non_contiguous=True), [], [key], dma=True)

    tb_n = [0]

    def norm_T(xt, xkey, dst3, dkeys, hbuf, hkey, gain_bc=None, gkey=None, tbanks=(0, 1)):
        ssq = SM[:, 128:129]
        rs = SM[:, 129:130]
        act(hbuf[:, 0:1024], xt, AF.Square, [xkey], [hkey, 'ssq'], accum_out=ssq)
        rstd_from_ssq(rs, ssq, D, ['ssq'], ['rs'])
        act(hbuf[:, 0:1024], xt, AF.Copy, [xkey, 'rs'], [hkey], scale=rs)
        if gain_bc is not None:
            V(lambda e: e.tensor_tensor(out=hbuf[:, 0:1024], in0=hbuf[:, 0:1024], in1=gain_bc, op=ALU.mult),
              [hkey, gkey], [hkey])
        bk = tbanks[tb_n[0] % len(tbanks)]
        tb_n[0] += 1
        pt = PB[bk][:].bitcast(BF16).rearrange("p (c n) -> p c n", c=8)
        for cc in range(8):
            tr(pt[:, cc, :], hbuf[:, cc * 128:(cc + 1) * 128], identb[:], [hkey, 'identb'], [f'PB{bk}'])
        V(lambda e: e.tensor_copy(out=dst3, in_=pt), [f'PB{bk}'], dkeys)

    def phase_B(l):
        colvec(CV[:, 0:8], W['mix_norm_g'][l], 128, 'cv_mix')
        WIN = WBt[:, 2 * 8192:2 * 8192 + 8 * 1792].rearrange("p (c n) -> p c n", c=8)
        load_w(WIN, ['WB2', 'WB3'], W['w_in'][l], 8, 1792, rowscale=CV[:, 0:8], rkeys=['cv_mix'])
        dma(GV[:], W['gm_v_norm_g'][l:l + 1, :].partition_broadcast(128), [], ['gvbc'])
        dma(GQ[:], W['q_norm_g'][l:l + 1, :].partition_broadcast(128), [], ['gqbc'])
        dma(GK[:], W['k_norm_g'][l:l + 1, :].partition_broadcast(128), [], ['gkbc'])
        V(lambda e: e.tensor_scalar(out=GQ[:], in0=GQ[:], scalar1=0.125, scalar2=None, op0=ALU.mult),
          ['gqbc'], ['gqbc'])
        P.op('sync', lambda e: e.dma_start(out=CV[:, 8:12], in_=W['gm_b_s'][l].rearrange("g i -> i g"),
                                           allow_slow_non_contiguous=True), [], ['cv_bs'], dma=True)
        WST = H8[2][:, 0:512].rearrange("p (g i) -> p g i", g=4)
        for g in range(4):
            dma(F2[4][:, g * 128:(g + 1) * 128], W['gm_w_s'][l, g], [], ['F24'])
        V(lambda e: e.tensor_copy(out=H8[2][:, 512:1024], in_=F2[4][:]), ['F24'], ['H82s'])
        ptw = PB[0][:].bitcast(BF16)
        for g in range(4):
            tr(ptw[:, g * 128:(g + 1) * 128], H8[2][:, 512 + g * 128:512 + (g + 1) * 128], identb[:], ['H82s', 'identb'], ['PB0'])
        V(lambda e: e.tensor_copy(out=H8[2][:, 0:512], in_=ptw[:, 0:512]), ['PB0'], ['H82w'])
        V(lambda e: e.memset(VAUG[:, :, :, 64:65], 1.0), [], ['VAUG'], 'gpsimd')

        for i in range(NT):
            b = i % 2
            xt, xk = F4[b], f'F4{b}'
            dma(xt[:], X[i * 128:(i + 1) * 128, :], ['X'], [xk])
            dma(ROPE[b][:], W['rope'][i * 128:(i + 1) * 128, :], [], [f'rope{b}'])
            hT = H2[b][:].rearrange("p (c n) -> p c n", c=8)
            norm_T(xt[:], xk, hT, [f'H2{b}'], H4[0], 'H40')
            for gi, (c0, c1, bk) in enumerate([(0, 512, 2), (512, 1024, 3), (1024, 1536, 4), (1536, 1792, 5)]):
                for cc in range(8):
                    mm(PB[bk][:, 0:c1 - c0], hT[:, cc, :], WIN[:, cc, c0:c1], cc == 0, cc == 7,
                       [f'H2{b}', 'WB2', 'WB3'], [f'PB{bk}'])
            GU, GVt, GM = F2[0], F2[1], F2[2]
            act(GU[:], PB[2][:], GELU, ['PB2'], ['F20'])
            act(GVt[:], PB[3][:], GELU, ['PB3'], ['F21'])
            act(F2[3][:], GVt[:], AF.Square, ['F21'], ['F23', 'ssqv'], accum_out=SM[:, 130:131])
            rstd_from_ssq(SM[:, 131:132], SM[:, 130:131], 512, ['ssqv'], ['rsv'])
            VN = H4[1][:, 0:512]
            V(lambda e: e.scalar_tensor_tensor(out=VN, in0=GVt[:], scalar=SM[:, 131:132], in1=GV[:],
                                               op0=ALU.mult, op1=ALU.mult), ['F21', 'rsv', 'gvbc'], ['H41'])
            for g in range(4):
                mm(PB[6][:, g * 128:(g + 1) * 128], WST[:, g, :], VN[:, g * 128:(g + 1) * 128], True, True,
                   ['H82w', 'H41'], ['PB6'])
            for g in range(4):
                V(lambda e, g=g: e.scalar_tensor_tensor(
                    out=GM[:, g * 128:(g + 1) * 128], in0=PB[6][:, g * 128:(g + 1) * 128], scalar=CV[:, 8 + g:9 + g],
                    in1=GU[:, g * 128:(g + 1) * 128], op0=ALU.add, op1=ALU.mult), ['PB6', 'cv_bs', 'F20'], ['F22'])
            act(F2[3][:], GM[:], AF.Square, ['F22'], ['F23', 'ssqg'], accum_out=SM[:, 132:133])
            rstd_from_ssq(RSTDGM[:, i:i + 1], SM[:, 132:133], 512, ['ssqg'], ['rstdgm'])
            GMB = H4[1][:, 512:1024]
            act(GMB, GM[:], AF.Copy, ['F22'], ['H41b'])
            QN = F2[3]
            act(F2[4][:], PB[4][:], AF.Square, ['PB4'], ['F24'])
            V(lambda e: e.tensor_reduce(out=SM[:, 136:144], in_=F2[4][:].rearrange("p (h d) -> p h d", h=8),
                                        axis=AX.X, op=ALU.add), ['F24'], ['ssqq'])
            rstd_from_ssq(SM[:, 136:144], SM[:, 136:144], 64, ['ssqq'], ['ssqq'])
            V(lambda e: e.tensor_tensor(out=QN[:].rearrange("p (h d) -> p h d", h=8),
                                        in0=PB[4][:].rearrange("p (h d) -> p h d", h=8),
                                        in1=SM[:, 136:144].unsqueeze(2).to_broadcast([128, 8, 64]), op=ALU.mult),
              ['PB4', 'ssqq'], ['F23'])
            V(lambda e: e.tensor_tensor(out=QN[:].rearrange("p (h d) -> p h d", h=8),
                                        in0=QN[:].rearrange("p (h d) -> p h d", h=8),
                                        in1=GQ[:].unsqueeze(1).to_broadcast([128, 8, 64]), op=ALU.mult),
              ['F23', 'gqbc'], ['F23'])
            rp = ROPE[b]
            rk = f'rope{b}'

            def rope(src, nh, dst_view, skey, dkey, TT, tkey, rp=rp, rk=rk):
                s4 = src.rearrange("p (h i two) -> p h i two", h=nh, two=2)
                t4 = TT.rearrange("p (h i two) -> p h i two", h=nh, two=2)
                V(lambda e: e.tensor_tensor(out=t4[:, :, :, 0], in0=s4[:, :, :, 1],
                                            in1=rp[:, 64:96].unsqueeze(1).to_broadcast([128, nh, 32]), op=ALU.mult),
                  [skey, rk], [tkey])
                V(lambda e: e.tensor_tensor(out=t4[:, :, :, 1], in0=s4[:, :, :, 0],
                                            in1=rp[:, 96:128].unsqueeze(1).to_broadcast([128, nh, 32]), op=ALU.mult),
                  [skey, rk], [tkey])
                s3 = src.rearrange("p (h d) -> p h d", h=nh)
                V(lambda e: e.tensor_tensor(out=s3, in0=s3, in1=rp[:, 0:64].unsqueeze(1).to_broadcast([128, nh, 64]),
                                            op=ALU.mult), [skey, rk], [skey])
                if nh == 8:
                    a4 = src.rearrange("p (hh j d) -> p hh j d", hh=2, j=4)
                    b4 = TT.rearrange("p (hh j d) -> p hh j d", hh=2, j=4)
                else:
                    a4 = s3
                    b4 = TT.rearrange("p (h d) -> p h d", h=nh)
                V(lambda e: e.tensor_tensor(out=dst_view, in0=a4, in1=b4, op=ALU.add), [skey, tkey], [dkey])

            QR = H4[0][:, 1024:1536]
            rope(QN[:], 8, QR.rearrange("p (j hh d) -> p hh j d", j=4, hh=2), 'F23', 'H40q', F2[4][:], 'F24')
            KN = F2[4][:, 0:128]
            act(F2[3][:, 0:128], PB[5][:, 0:128], AF.Square, ['PB5', 'H40q'], ['F23'])
            V(lambda e: e.tensor_reduce(out=SM[:, 144:146], in_=F2[3][:, 0:128].rearrange("p (h d) -> p h d", h=2),
                                        axis=AX.X, op=ALU.add), ['F23'], ['ssqk'])
            rstd_from_ssq(SM[:, 144:146], SM[:, 144:146], 64, ['ssqk'], ['ssqk'])
            V(lambda e: e.tensor_tensor(out=KN.rearrange("p (h d) -> p h d", h=2),
                                        in0=PB[5][:, 0:128].rearrange("p (h d) -> p h d", h=2),
                                        in1=SM[:, 144:146].unsqueeze(2).to_broadcast([128, 2, 64]), op=ALU.mult),
              ['PB5', 'ssqk'], ['F24'])
            V(lambda e: e.tensor_tensor(out=KN.rearrange("p (h d) -> p h d", h=2),
                                        in0=KN.rearrange("p (h d) -> p h d", h=2),
                                        in1=GK[:].unsqueeze(1).to_broadcast([128, 2, 64]), op=ALU.mult),
              ['F24', 'gkbc'], ['F24'])
            KR = H4[0][:, 1536:1664]
            rope(KN, 2, KR.rearrange("p (h d) -> p h d", h=2), 'F24', 'H40k', F2[3][:, 128:256], 'F23')
            V(lambda e, i=i: e.tensor_copy(out=VAUG[:, i, :, 0:64],
                                           in_=PB[5][:, 128:256].rearrange("p (h d) -> p h d", h=2)),
              ['PB5'], ['VAUG'])
            pt = PB[7][:].bitcast(BF16)
            for g in range(4):
                tr(pt[:, g * 128:(g + 1) * 128], GMB[:, g * 128:(g + 1) * 128], identb[:], ['H41b', 'identb'], ['PB7'])
            for j in range(4):
                tr(pt[:, 512 + j * 128:512 + (j + 1) * 128], QR[:, j * 128:(j + 1) * 128], identb[:],
                   ['H40q', 'identb'], ['PB7'])
            TS = H4[b][:, 0:0]
            OUTS = H2[2 + b]
            ok = f'H2{2 + b}'
            if i == 0:
                pass
            V(lambda e, OUTS=OUTS: e.tensor_copy(out=OUTS[:], in_=pt), ['PB7'], [ok])
            dma(GMT[:, :, i * 128:(i + 1) * 128].rearrange("c p n -> p c n"),
                OUTS[:, 0:512].rearrange("p (c n) -> p c n", c=4), [ok], ['GMT'])
            dma(QT[:, :, i * 128:(i + 1) * 128].rearrange("c p n -> p c n"),
                OUTS[:, 512:1024].rearrange("p (c n) -> p c n", c=4), [ok], ['QT'])
            bk = 'PB6'
            ptk = PB[6][:].bitcast(BF16)
            tr(ptk[:, 0:128], KR, identb[:], ['H40k', 'identb'], ['PB6'])
            V(lambda e, i=i: e.tensor_copy(out=KT[:, i * 128:(i + 1) * 128], in_=ptk[:, 0:128]), ['PB6'], ['KT'])

    def phase_C(l):
        colvec(CV[:, 16:20], W['branch_norm_g'][l, 0], 128, 'cv_b0')
        colvec(CV[0:64, 20:28], W['branch_norm_g'][l, 1], 64, 'cv_b1')
        WO0 = WB(0)[:, 0:4096].rearrange("p (c n) -> p c n", c=4)
        WO1 = WB(1)[0:64, :].rearrange("p (c n) -> p c n", c=8)
        load_w(WO0, ['WB0'], W['w_out'][l, 0:512, :], 4, 1024, rowscale=CV[:, 16:20], rkeys=['cv_b0'])
        load_w(WO1, ['WB1'], W['w_out'][l, 512:1024, :], 8, 1024, rowscale=CV[0:64, 20:28], rkeys=['cv_b1'], part=64)
        NKP = NT // 2
        GS = min(512, S)
        NGG = S // GS
        for g in range(NGG):
            t0 = g * GS
            qTg = H4[g % 2][:, 0:4 * GS].rearrange("p (j n) -> p j n", j=4)
            qk = f'H4{g % 2}'
            dma(qTg, QT[:, :, t0:t0 + GS].rearrange("c p n -> p c n"), ['QT'], [qk])
            ATT = H8[0][0:64, 0:8 * GS].rearrange("p (h n) -> p h n", h=8)
            SQACC = F2[4][0:64, 0:GS]
            for h in range(8):
                j, hh = h % 4, h // 4
                ob = 4 + (h % 2)
                OT = PB[ob]
                for kp in range(NKP):
                    sbk = (kp % 2) * 2
                    for k2 in range(2):
                        kt = kp * 2 + k2
                        mm(PB[sbk + k2][:, 0:GS], KT[hh * 64:(hh + 1) * 64, kt * 128:(kt + 1) * 128],
                           qTg[hh * 64:(hh + 1) * 64, j, :], True, True, ['KT', qk], [f'PB{sbk + k2}'])
                    PT = H2[kp % 3]
                    pk = f'H2{kp % 3}'
                    for k2 in range(2):
                        act(PT[:, k2 * 512:k2 * 512 + GS], PB[sbk + k2][:, 0:GS], AF.Exp, [f'PB{sbk + k2}'], [pk])
                    for k2 in range(2):
                        kt = kp * 2 + k2
                        mm(OT[0:65, 0:GS], VAUG[:, kt, hh, :], PT[:, k2 * 512:k2 * 512 + GS],
                           kt == 0, kt == NT - 1, ['VAUG', pk], [f'PB{ob}'])
                SR = F2[3]
                V(lambda e, OT=OT: e.tensor_copy(out=SR[64:65, 0:GS], in_=OT[64:65, 0:GS]), [f'PB{ob}'], ['F23'])
                mm(PB[6][0:64, 0:GS], onesf[64:65, 0:64], SR[64:65, 0:GS], True, True, ['onesf', 'F23'], ['PB6'])
                Rr = F2[2]
                V(lambda e: e.reciprocal(out=Rr[0:64, 0:GS], in_=PB[6][0:64, 0:GS]), ['PB6'], ['F22'])
                V(lambda e, OT=OT, h=h: e.tensor_tensor(out=ATT[:, h, :], in0=OT[0:64, 0:GS], in1=Rr[0:64, 0:GS],
                                                        op=ALU.mult), [f'PB{ob}', 'F22'], ['H80'])
                if h == 0:
                    V(lambda e, h=h: e.tensor_tensor(out=SQACC, in0=ATT[:, h, :], in1=ATT[:, h, :], op=ALU.mult),
                      ['H80'], ['F24'], 'gpsimd')
                else:
                    V(lambda e, h=h: e.tensor_tensor(out=F2[1][0:64, 0:GS], in0=ATT[:, h, :], in1=ATT[:, h, :],
                                                     op=ALU.mult), ['H80'], ['F21'], 'gpsimd')
                    V(lambda e: e.tensor_tensor(out=SQACC, in0=SQACC, in1=F2[1][0:64, 0:GS], op=ALU.add),
                      ['F21', 'F24'], ['F24'], 'gpsimd')
            mm(PB[6][0:1, 0:GS], onesf[0:64, 0:1], SQACC, True, True, ['onesf', 'F24'], ['PB6'])
            V(lambda e: e.tensor_copy(out=F2[3][0:1, 0:GS], in_=PB[6][0:1, 0:GS]), ['PB6'], ['F23'])
            for tt in range(GS // 128):
                mm(PB[6][:, tt:tt + 1], F2[3][0:1, tt * 128:(tt + 1) * 128], onesf[0:1, 0:1], True, True,
                   ['F23', 'onesf'], ['PB6'])
            rstd_from_ssq(SM[:, 148:148 + GS // 128], PB[6][:, 0:GS // 128], 512, ['PB6'], ['rsat'])
            gmTg = H8[1][:, 0:4 * GS].rearrange("p (c n) -> p c n", c=4)
            dma(gmTg, GMT[:, :, t0:t0 + GS].rearrange("c p n -> p c n"), ['GMT'], ['H81'])
            for tt in range(GS // 128):
                i = g * (GS // 128) + tt
                b = tt % 2
                xt, xk = F4[b], f'F4{b}'
                dma(xt[:], X[i * 128:(i + 1) * 128, :], ['X'], [xk])
                for half in range(2):
                    hs = slice(half * 512, (half + 1) * 512)
                    for cc in range(4):
                        mm(PB[7][:], gmTg[:, cc, tt * 128:(tt + 1) * 128], WO0[:, cc, hs], cc == 0, cc == 3,
                           ['H81', 'WB0'], ['PB7'])
                    V(lambda e, xt=xt, hs=hs, i=i: e.scalar_tensor_tensor(
                        out=xt[:, hs], in0=PB[7][:], scalar=RSTDGM[:, i:i + 1], in1=xt[:, hs],
                        op0=ALU.mult, op1=ALU.add), ['PB7', 'rstdgm', xk], [xk])
                    for h in range(8):
                        mm(PB[7][:], ATT[:, h, tt * 128:(tt + 1) * 128], WO1[:, h, hs], h == 0, h == 7,
                           ['H80', 'WB1'], ['PB7'])
                    V(lambda e, xt=xt, hs=hs, tt=tt: e.scalar_tensor_tensor(
                        out=xt[:, hs], in0=PB[7][:], scalar=SM[:, 148 + tt:149 + tt], in1=xt[:, hs],
                        op0=ALU.mult, op1=ALU.add), ['PB7', 'rsat', xk], [xk])
                dma(X[i * 128:(i + 1) * 128, :], xt[:], [xk], ['X'])

    MNTt = sb('MNT', [128, 2048], BF16)
    MNT = MNTt[:].rearrange("p (c n) -> p c n", c=8)
    KXt = sb('KX', [128, 2048], BF16)
    KX = KXt[:].rearrange("p (c n) -> p c n", c=8)
    VXt = sb('VX', [128, 2048], BF16)
    VX = VXt[:].rearrange("p (m n) -> p m n", m=2)
    WRf = sb('WRf', [128, 128])

    def phase_A0():
        dma(F4[2][:], W['mem_norm_g'].partition_broadcast(128), [], ['F42'])
        for mt in range(2):
            dma(F4[mt][:], c.mem[mt * 128:(mt + 1) * 128, :], [], [f'F4{mt}'])
            norm_T(F4[mt][:], f'F4{mt}', MNT[:, :, mt * 128:(mt + 1) * 128], ['MNT'], H4[0], 'H40',
                   gain_bc=F4[2][:], gkey='F42')

    def phase_A(l):
        WK = WB(2).rearrange("p (c n) -> p c n", c=8)
        WV = WB(3).rearrange("p (c n) -> p c n", c=8)
        load_w(WK, ['WB2'], W['xattn_w_kv'][l, :, 0:1024], 8, 1024)
        load_w(WV, ['WB3'], W['xattn_w_kv'][l, :, 1024:2048], 8, 1024)
        for dc in range(8):
            bk = dc % 2
            for cc in range(8):
                mm(PB[bk][:, 0:256], WK[:, cc, dc * 128:(dc + 1) * 128], MNT[:, cc, :], cc == 0, cc == 7,
                   ['WB2', 'MNT'], [f'PB{bk}'])
            V(lambda e, dc=dc, bk=bk: e.tensor_copy(out=KX[:, dc, :], in_=PB[bk][:, 0:256]), [f'PB{bk}'], ['KX'])
        for mt in range(2):
            for half in range(2):
                bk = 2 + half
                for cc in range(8):
                    mm(PB[bk][:], MNT[:, cc, mt * 128:(mt + 1) * 128], WV[:, cc, half * 512:(half + 1) * 512],
                       cc == 0, cc == 7, ['WB3', 'MNT'], [f'PB{bk}'])
                V(lambda e, mt=mt, half=half, bk=bk: e.tensor_copy(out=VX[:, mt, half * 512:(half + 1) * 512],
                                                                   in_=PB[bk][:]), [f'PB{bk}'], ['VX'])

    def phase_D(l):
        colvec(CV[:, 32:40], W['xattn_norm_g'][l], 128, 'cv_x')
        WQ = WB(0).rearrange("p (c n) -> p c n", c=8)
        WOX = WB(1).rearrange("p (c n) -> p c n", c=8)
        load_w(WQ, ['WB0'], W['xattn_w_q'][l], 8, 1024, rowscale=CV[:, 32:40], mul=1.0 / 16.0, rkeys=['cv_x'])
        load_w(WOX, ['WB1'], W['xattn_w_o'][l], 8, 1024)
        GS = min(512, S)
        for g in range(S // GS):
            t0 = g * GS
            H2Tg = H8[1][:, 0:8 * GS].rearrange("p (c n) -> p c n", c=8)
            for tt in range(GS // 128):
                i = g * (GS // 128) + tt
                b = tt % 2
                dma(F4[b][:], X[i * 128:(i + 1) * 128, :], ['X'], [f'F4{b}'])
                norm_T(F4[b][:], f'F4{b}', H2Tg[:, :, tt * 128:(tt + 1) * 128], ['H81'], H4[0], 'H40')
            QX = H8[2][:, 0:8 * GS].rearrange("p (c n) -> p c n", c=8)
            for dc in range(8):
                bk = 2 + dc % 2
                for cc in range(8):
                    mm(PB[bk][:, 0:GS], WQ[:, cc, dc * 128:(dc + 1) * 128], H2Tg[:, cc, :], cc == 0, cc == 7,
                       ['WB0', 'H81'], [f'PB{bk}'])
                act(QX[:, dc, :], PB[bk][:, 0:GS], AF.Copy, [f'PB{bk}'], ['H82'])
            OXT = H8[0][:, 0:8 * GS].rearrange("p (c n) -> p c n", c=8)
            for h in range(4):
                PX = H2[2 + h % 2]
                pk = f'H2{2 + h % 2}'
                for mt in range(2):
                    for dd in range(2):
                        mm(PB[4 + mt][:, 0:GS], KX[:, 2 * h + dd, mt * 128:(mt + 1) * 128], QX[:, 2 * h + dd, :],
                           dd == 0, dd == 1, ['KX', 'H82'], [f'PB{4 + mt}'])
                    act(PX[:, mt * 512:mt * 512 + GS], PB[4 + mt][:, 0:GS], AF.Exp, [f'PB{4 + mt}'], [pk])
                for mt in range(2):
                    mm(PB[6][:, 0:GS], onesb[:], PX[:, mt * 512:mt * 512 + GS], mt == 0, mt == 1,
                       ['onesb', pk], ['PB6'])
                V(lambda e: e.reciprocal(out=F2[2][:, 0:GS], in_=PB[6][:, 0:GS]), ['PB6'], ['F22'])
                for dd in range(2):
                    for mt in range(2):
                        mm(PB[7][:, 0:GS], VX[:, mt, (2 * h + dd) * 128:(2 * h + dd + 1) * 128],
                           PX[:, mt * 512:mt * 512 + GS], mt == 0, mt == 1, ['VX', pk], ['PB7'])
                    V(lambda e, h=h, dd=dd: e.tensor_tensor(out=OXT[:, 2 * h + dd, :], in0=PB[7][:, 0:GS],
                                                            in1=F2[2][:, 0:GS], op=ALU.mult),
                      ['PB7', 'F22'], ['H80'])
            for tt in range(GS // 128):
                i = g * (GS // 128) + tt
                b = tt % 2
                xt, xk = F4[2 + b], f'F4{2 + b}'
                dma(xt[:], X[i * 128:(i + 1) * 128, :], ['X'], [xk])
                for half in range(2):
                    hs = slice(half * 512, (half + 1) * 512)
                    bk = 2 + half
                    for cc in range(8):
                        mm(PB[bk][:], OXT[:, cc, tt * 128:(tt + 1) * 128], WOX[:, cc, hs], cc == 0, cc == 7,
                           ['H80', 'WB1'], [f'PB{bk}'])
                    V(lambda e, xt=xt, hs=hs, bk=bk: e.tensor_tensor(out=xt[:, hs], in0=PB[bk][:], in1=xt[:, hs],
                                                                     op=ALU.add), [f'PB{bk}', xk], [xk])
                dma(X[i * 128:(i + 1) * 128, :], xt[:], [xk], ['X'])

    def phase_E(l):
        colvec(CV[:, 40:48], W['ffn_norm_g'][l], 128, 'cv_f')
        WR3 = WRf[:].rearrange("p (c e) -> p c e", c=8)
        dma(WR3, W['w_router'][l].rearrange("(c p) e -> p c e", p=128), [], ['WRf'])
        for cc in range(8):
            V(lambda e, cc=cc: e.tensor_scalar(out=WR3[:, cc, :], in0=WR3[:, cc, :], scalar1=CV[:, 40 + cc:41 + cc],
                                               scalar2=None, op0=ALU.mult), ['WRf', 'cv_f'], ['WRf'])
        for i in range(NT):
            b = i % 2
            xt, xk = F4[b], f'F4{b}'
            dma(xt[:], X[i * 128:(i + 1) * 128, :], ['X'], [xk])
            act(F4[2][:], xt[:], AF.Square, [xk], ['F42', 'ssq'], accum_out=SM[:, 128:129])
            rstd_from_ssq(SM[:, 129:130], SM[:, 128:129], D, ['ssq'], ['rs'])
            act(F4[2][:], xt[:], AF.Copy, [xk, 'rs'], ['F42'], scale=SM[:, 129:130])
            for cc in range(8):
                bk = cc // 4
                tr(PB[bk][:, (cc % 4) * 128:(cc % 4 + 1) * 128], F4[2][:, cc * 128:(cc + 1) * 128], identf[:],
                   ['F42', 'identf'], [f'PB{bk}'])
            V(lambda e: e.tensor_copy(out=F4[3][:, 0:512], in_=PB[0][:]), ['PB0'], ['F43'])
            V(lambda e: e.tensor_copy(out=F4[3][:, 512:1024], in_=PB[1][:]), ['PB1'], ['F43'])
            for cc in range(8):
                mm(PB[2][:, 0:16], F4[3][:, cc * 128:(cc + 1) * 128], WR3[:, cc, :], cc == 0, cc == 7,
                   ['F43', 'WRf'], ['PB2'])
            V(lambda e: e.reduce_max(out=SM[:, 150:151], in_=PB[2][:, 0:16], axis=AX.X), ['PB2'], ['smx'])
            V(lambda e: e.tensor_scalar(out=SM[:, 150:151], in0=SM[:, 150:151], scalar1=-1.0, scalar2=None,
                                        op0=ALU.mult), ['smx'], ['smx'])
            act(SM[:, 160:176], PB[2][:, 0:16], AF.Exp, ['PB2', 'smx'], ['sme', 'sms'], bias=SM[:, 150:151],
                accum_out=SM[:, 151:152])
            V(lambda e: e.reciprocal(out=SM[:, 151:152], in_=SM[:, 151:152]), ['sms'], ['sms'])
            V(lambda e, i=i: e.tensor_scalar(out=AFF[:, i, :], in0=SM[:, 160:176], scalar1=SM[:, 151:152],
                                             scalar2=None, op0=ALU.mult), ['sme', 'sms'], ['AFF'])
            hb = H2[b]
            V(lambda e, hb=hb: e.tensor_copy(out=hb[:], in_=F4[3][:]), ['F43'], [f'H2{b}'])
            dma(H3T[:, :, i * 128:(i + 1) * 128].rearrange("c p n -> p c n"),
                hb[:].rearrange("p (c n) -> p c n", c=8), [f'H2{b}'], ['H3T'])
        LO, HI, MID = SM[:, 160:176], SM[:, 176:192], SM[:, 192:208]
        PART, CC, D1 = SM[:, 208:224], SM[:, 224:240], SM[:, 240:256]
        CMP = F4[2][:, 0:NT * 16].rearrange("p (t e) -> p t e", e=16)
        V(lambda e: e.memset(LO, 0.0), ['sme'], ['lo'])
        V(lambda e: e.memset(HI, 1.0), [], ['hi'])
        for it in range(32):
            V(lambda e: e.tensor_tensor(out=MID, in0=LO, in1=HI, op=ALU.add), ['lo', 'hi'], ['mid'])
            V(lambda e: e.tensor_scalar(out=MID, in0=MID, scalar1=0.5, scalar2=None, op0=ALU.mult), ['mid'], ['mid'])
            V(lambda e: e.tensor_tensor(out=CMP, in0=AFF, in1=MID.unsqueeze(1).to_broadcast([128, NT, 16]),
                                        op=ALU.is_gt), ['AFF', 'mid'], ['F42'])
            V(lambda e: e.tensor_reduce(out=PART, in_=CMP.rearrange("p t e -> p e t"), axis=AX.X, op=ALU.add),
              ['F42'], ['part'])
            mm(PB[3][:, 0:16], onesf[:], PART, True, True, ['onesf', 'part'], ['PB3'])
            V(lambda e: e.tensor_scalar(out=CC, in0=PB[3][:, 0:16], scalar1=float(CAP) - 0.5, scalar2=None,
                                        op0=ALU.is_ge), ['PB3'], ['cc'])
            V(lambda e: e.tensor_tensor(out=D1, in0=MID, in1=LO, op=ALU.subtract), ['mid', 'lo'], ['d1'])
            V(lambda e: e.tensor_tensor(out=D1, in0=D1, in1=CC, op=ALU.mult), ['d1', 'cc'], ['d1'])
            V(lambda e: e.tensor_tensor(out=LO, in0=LO, in1=D1, op=ALU.add), ['d1', 'lo'], ['lo'])
            V(lambda e: e.tensor_tensor(out=D1, in0=HI, in1=MID, op=ALU.subtract), ['mid', 'hi'], ['d1'])
            V(lambda e: e.tensor_tensor(out=D1, in0=D1, in1=CC, op=ALU.mult), ['d1', 'cc'], ['d1'])
            V(lambda e: e.tensor_tensor(out=HI, in0=MID, in1=D1, op=ALU.add), ['d1', 'mid'], ['hi'])
        V(lambda e: e.tensor_tensor(out=CMP, in0=AFF, in1=LO.unsqueeze(1).to_broadcast([128, NT, 16]),
                                    op=ALU.is_gt), ['AFF', 'lo'], ['F42'])
        V(lambda e: e.tensor_tensor(out=MG, in0=CMP, in1=AFF, op=ALU.mult), ['F42', 'AFF'], ['MG'])
        GS = min(512, S)
        for ex in range(NE):
            sl = [(3 * ex + k) % 4 for k in range(3)]
            WGv, WUv, WDv = [WB(s_).rearrange("p (c n) -> p c n", c=8) for s_ in sl]
            kg, ku, kd = [f'WB{s_}' for s_ in sl]
            load_w(WGv, [kg], W['w_gate'][l, ex], 8, 1024, rowscale=CV[:, 40:48], rkeys=['cv_f'])
            load_w(WUv, [ku], W['w_up'][l, ex], 8, 1024, rowscale=CV[:, 40:48], rkeys=['cv_f'])
            load_w(WDv, [kd], W['w_down'][l, ex], 8, 1024)
            for g in range(S // GS):
                t0 = g * GS
                hb = H8[g % 2]
                hk = f'H8{g % 2}'
                H3Tg = hb[:, 0:8 * GS].rearrange("p (c n) -> p c n", c=8)
                dma(H3Tg, H3T[:, :, t0:t0 + GS].rearrange("c p n -> p c n"), ['H3T'], [hk])
                ACTT = H8[2][:, 0:8 * GS].rearrange("p (c n) -> p c n", c=8)
                for fc in range(8):
                    ba, bu = fc % 2, 2 + fc % 2
                    for cc in range(8):
                        mm(PB[ba][:, 0:GS], WGv[:, cc, fc * 128:(fc + 1) * 128], H3Tg[:, cc, :], cc == 0, cc == 7,
                           [kg, hk], [f'PB{ba}'])
                    for cc in range(8):
                        mm(PB[bu][:, 0:GS], WUv[:, cc, fc * 128:(fc + 1) * 128], H3Tg[:, cc, :], cc == 0, cc == 7,
                           [ku, hk], [f'PB{bu}'])
                    SA = F2[fc % 2]
                    act(SA[:, 0:GS], PB[ba][:, 0:GS], AF.Silu, [f'PB{ba}'], [f'F2{fc % 2}'])
                    V(lambda e, fc=fc, SA=SA, bu=bu: e.tensor_tensor(out=ACTT[:, fc, :], in0=PB[bu][:, 0:GS],
                                                                     in1=SA[:, 0:GS], op=ALU.mult),
                      [f'PB{bu}', f'F2{fc % 2}'], ['H82'])
                for tt in range(GS // 128):
                    i = g * (GS // 128) + tt
                    ys = F4[tt % 2]
                    yk = f'F4{tt % 2}'
                    for half in range(2):
                        hs = slice(half * 512, (half + 1) * 512)
                        bk = 4 + half
                        for fc in range(8):
                            mm(PB[bk][:], ACTT[:, fc, tt * 128:(tt + 1) * 128], WDv[:, fc, hs], fc == 0, fc == 7,
                               ['H82', kd], [f'PB{bk}'])
                        V(lambda e, ys=ys, hs=hs, bk=bk, i=i, ex=ex: e.tensor_scalar(
                            out=ys[:, hs], in0=PB[bk][:], scalar1=MG[:, i, ex:ex + 1], scalar2=None, op0=ALU.mult),
                          [f'PB{bk}', 'MG'], [yk])
                    P.op('gpsimd', lambda e, ys=ys, i=i: e.dma_start(out=X[i * 128:(i + 1) * 128, :], in_=ys[:],
                                                                     accum_op=ALU.add),
                         [yk], [('X', i)], dma=True)
        for i in range(NT):
            t = P.lastw.get(('X', i))
            if t is not None:
                P.readers.setdefault('X', {})[('acc', i)] = t

    def phase_F():
        dma(F4[2][:], c.final_g.partition_broadcast(128), [], ['F42'])
        for i in range(NT):
            b = i % 2
            xt, xk = F4[b], f'F4{b}'
            dma(xt[:], X[i * 128:(i + 1) * 128, :], ['X'], [xk])
            act(F4[3][:], xt[:], AF.Square, [xk], ['F43', 'ssq'], accum_out=SM[:, 128:129])
            rstd_from_ssq(SM[:, 129:130], SM[:, 128:129], D, ['ssq'], ['rs'])
            yt = H8[b][:].bitcast(F32)[:, 0:1024]
            V(lambda e, xt=xt, yt=yt: e.scalar_tensor_tensor(out=yt, in0=xt[:], scalar=SM[:, 129:130],
                                                             in1=F4[2][:], op0=ALU.mult, op1=ALU.mult),
              [xk, 'rs', 'F42'], [f'H8{b}'])
            dma(c.out[i * 128:(i + 1) * 128, :], yt, [f'H8{b}'], ['OUT'])

    c.phases = dict(B=phase_B, C=phase_C, A=phase_A, D=phase_D, E=phase_E)
    phase_A0()
    P.barrier()
    for l in range(L):
        for ph in phases:
            if ph in c.phases:
                c.phases[ph](l)
                P.barrier()
    phase_F()
    P.barrier()
    P.emit()
    return nc


def rope_table(S):
    rows = S // 64
    row_id = np.repeat(np.arange(rows, dtype=np.float32), 64)
    col_id = np.tile(np.arange(64, dtype=np.float32), rows)
    n_pairs = 16
    freqs = np.exp(-np.log(np.float32(10000.0)) * np.arange(n_pairs, dtype=np.float32) / n_pairs).astype(np.float32)
    ang = np.concatenate([row_id[:, None] * freqs[None, :], col_id[:, None] * freqs[None, :]], axis=-1)
    cos, sin = np.cos(ang).astype(np.float32), np.sin(ang).astype(np.float32)
    tab = np.zeros((S, 128), np.float32)
    tab[:, 0:64:2] = cos
    tab[:, 1:64:2] = cos
    tab[:, 64:96] = -sin
    tab[:, 96:128] = sin
    return tab


WNAMES = ['mix_norm_g', 'w_in', 'gm_v_norm_g', 'gm_w_s', 'gm_b_s', 'q_norm_g', 'k_norm_g', 'branch_norm_g',
          'w_out', 'xattn_norm_g', 'xattn_w_q', 'xattn_w_kv', 'xattn_w_o', 'ffn_norm_g', 'w_router',
          'w_gate', 'w_up', 'w_down']


def make_in_maps(inputs, S, L, nb):
    shared = {k: np.ascontiguousarray(np.asarray(inputs[k], dtype=np.float32)[:L]) for k in WNAMES}
    shared['mem_norm_g'] = np.asarray(inputs['mem_norm_g'], np.float32).reshape(1, D)
    shared['final_norm_g'] = np.asarray(inputs['final_norm_g'], np.float32).reshape(1, D)
    shared['rope'] = rope_table(S)
    shared['identf'] = np.eye(128, dtype=np.float32)
    shared['identb'] = np.eye(128, dtype=np.float32).astype(ml_dtypes.bfloat16)
    maps = []
    for b in range(nb):
        m = dict(shared)
        m['x'] = np.ascontiguousarray(np.asarray(inputs['x'], np.float32)[b])
        m['mem'] = np.ascontiguousarray(np.asarray(inputs['mem'], np.float32)[b])
        maps.append(m)
    return maps


def kernel(**inputs):
    x = np.asarray(inputs['x'])
    B, S, _ = x.shape
    L = np.asarray(inputs['w_in']).shape[0]
    nc = build(S, L)
    maps = make_in_maps(inputs, S, L, B)
    res = run_bass_kernel_spmd(nc, maps, core_ids=list(range(B)))
    return np.stack([np.asarray(r['out'], dtype=np.float32) for r in res.results], axis=0)
```

```python
import numpy as np
import ml_dtypes
from contextlib import ExitStack
import concourse.bass as bass
import concourse.mybir as mybir
from concourse.bass_utils import run_bass_kernel_spmd

F32 = mybir.dt.float32
BF16 = mybir.dt.bfloat16
I32 = mybir.dt.int32
AF = mybir.ActivationFunctionType
ALU = mybir.AluOpType
AX = mybir.AxisListType

D = 1024
MEM = 256
NE = 16
EPS = 1e-6
GELU = AF.Gelu_apprx_tanh
ENGS = ['tensor', 'vector', 'scalar', 'gpsimd', 'sync']
EPOCH = 20000
DEPOCH = 1200
DK = 8


class Prog:
    def __init__(self, nc, stack):
        self.nc = nc
        self.stack = stack
        self.rec = {e: [] for e in ENGS}
        self.cnt = {e: 0 for e in ENGS}
        self.esem = {e: None for e in ENGS}
        self.nsem = 0
        self.dring = {e: [None] * DK for e in ENGS}
        self.duse = {e: [0] * DK for e in ENGS}
        self.dn = {e: 0 for e in ENGS}
        self.waited = {e: {} for e in ENGS}
        self.lastw = {}
        self.readers = {}
        self.ninst = 0

    def newsem(self, tag):
        self.nsem += 1
        return self.stack.enter_context(self.nc.semaphore(f"{tag}_{self.nsem}"))

    def _wait(self, eng, tok):
        sem, val = tok[0], tok[1]
        w = self.waited[eng]
        if w.get(id(sem), 0) >= val:
            return
        w[id(sem)] = val
        self.rec[eng].append(lambda e, s=sem, v=val: e.wait_ge(s, v))

    def op(self, eng, fn, r=(), w=(), dma=False):
        toks = []
        for k in r:
            t = self.lastw.get(k)
            if t is not None:
                toks.append(t)
        for k in w:
            t = self.lastw.get(k)
            if t is not None:
                toks.append(t)
            toks.extend(self.readers.get(k, {}).values())
        for t in toks:
            if t[2] == 'tensor' and eng == 'tensor' and not dma:
                continue
            self._wait(eng, t)
        self.ninst += 1
        if dma:
            slot = self.dn[eng] % DK
            self.dn[eng] += 1
            sem = self.dring[eng][slot]
            prev = self.duse[eng][slot]
            if sem is not None and prev > 0:
                self._wait(eng, (sem, 16 * prev))
            if sem is None or prev >= DEPOCH:
                sem = self.newsem('d' + eng[:2])
                self.dring[eng][slot] = sem
                prev = 0
            self.duse[eng][slot] = prev + 1
            tok = (sem, 16 * (prev + 1), 'dma')
            self.rec[eng].append(lambda e, f=fn, s=sem: f(e).then_inc(s, 16))
        else:
            if self.esem[eng] is None or self.cnt[eng] >= EPOCH:
                self.esem[eng] = self.newsem('e' + eng[:2])
                self.cnt[eng] = 0
            self.cnt[eng] += 1
            sem = self.esem[eng]
            tok = (sem, self.cnt[eng], eng)
            self.rec[eng].append(lambda e, f=fn, s=sem: f(e).then_inc(s, 1))
        for k in r:
            self.readers.setdefault(k, {})[id(tok[0])] = tok
        for k in w:
            self.lastw[k] = tok
            self.readers[k] = {}
        return tok

    def wait_all(self, eng, keys):
        for k in keys:
            t = self.lastw.get(k)
            if t is not None:
                self._wait(eng, t)

    def barrier(self):
        toks = []
        for e in ENGS:
            if self.esem[e] is not None and self.cnt[e] > 0:
                toks.append((self.esem[e], self.cnt[e], e))
            for slot in range(DK):
                sem = self.dring[e][slot]
                if sem is not None and self.duse[e][slot] > 0:
                    toks.append((sem, 16 * self.duse[e][slot], 'dma'))
        for e in ENGS:
            for t in toks:
                self._wait(e, t)

    def emit(self):
        nc = self.nc
        with nc.Block() as block:
            @block.tensor
            def _(e):
                for f in self.rec['tensor']:
                    f(e)

            @block.vector
            def _(e):
                for f in self.rec['vector']:
                    f(e)

            @block.scalar
            def _(e):
                for f in self.rec['scalar']:
                    f(e)

            @block.gpsimd
            def _(e):
                for f in self.rec['gpsimd']:
                    f(e)

            @block.sync
            def _(e):
                for f in self.rec['sync']:
                    f(e)


class Ctx:
    pass


def build(S, L, dbg=False, phases=('B', 'C', 'A', 'D', 'E'), rg=None):
    NT = S // 128
    NG = S // 512
    CAP = 2 * S // NE
    nc = bass.Bass("TRN2", target_bir_lowering=False)
    stack = ExitStack()
    P = Prog(nc, stack)
    c = Ctx()
    c.nc, c.P, c.S, c.L, c.NT, c.NG, c.CAP = nc, P, S, L, NT, NG, CAP

    def din(name, shape, dt=F32):
        return nc.dram_tensor(name, list(shape), dt, kind="ExternalInput").ap()

    c.x_in = din('x', [S, D])
    c.mem = din('mem', [MEM, D])
    c.out = nc.dram_tensor('out', [S, D], F32, kind="ExternalOutput").ap()
    c.final_g = din('final_norm_g', [1, D])

    def sb(name, shape, dt=F32):
        return nc.alloc_sbuf_tensor(name, list(shape), dt)

    def ps(name, shape, dt=F32):
        return nc.alloc_psum_tensor(name, list(shape), dt)

    c.sb, c.ps = sb, ps

    def dma(out, in_, r, w, q='sync', **kw):
        return P.op(q, lambda e: e.dma_start(out=out, in_=in_, **kw), r, w, dma=True)

    def act(out, in_, func, r, w, **kw):
        return P.op('scalar', lambda e: e.activation(out=out, in_=in_, func=func, **kw), r, w)

    def mm(out, lhsT, rhs, start, stop, r, w):
        return P.op('tensor', lambda e: e.matmul(out, lhsT, rhs, start=start, stop=stop), r, w)

    def tr(out, in_, ident, r, w):
        return P.op('tensor', lambda e: e.transpose(out, in_, ident), r, w)

    def V(fn, r, w, eng='vector'):
        return P.op(eng, fn, r, w)

    c.dma, c.act, c.mm, c.tr, c.V = dma, act, mm, tr, V

    def rstd_from_ssq(out, ssq, n, r, w, eng='vector'):
        V(lambda e: e.tensor_scalar(out=out, in0=ssq, scalar1=1.0 / n, scalar2=EPS,
                                    op0=ALU.mult, op1=ALU.add), r, w)
        P.op('scalar', lambda e: e.sqrt(out=out, in_=out), w, w)
        V(lambda e: e.reciprocal(out=out, in_=out), w, w)

    c.rstd_from_ssq = rstd_from_ssq

    NTK = NT * (2 if rg else 1)
    SK = NTK * 128
    CAP = 2 * SK // NE
    def dram(name, shape, dt):
        return nc.dram_tensor(name, list(shape), dt, kind=("ExternalOutput" if dbg else "Internal")).ap()

    X = dram('Xs', [S, D], F32)
    GMT = dram('GMT', [4, 128, S], BF16)
    QT = dram('QT', [4, 128, S], BF16)
    H3T = dram('H3T', [8, 128, S], BF16)
    H3R = dram('H3R', [S, D], BF16)
    AFFD = dram('AFFD', [S, 16], F32)
    LISTD = dram('LISTD', [16, CAP], I32)
    M8 = CAP // 128
    NA = CAP // 32
    if rg:
        KTD = nc.dram_tensor('KTD', [128, S], BF16, kind='Internal').ap()
        NR = len(rg[0])
        KTA = nc.dram_tensor('KTA', [NR * 128, S], BF16, kind='Internal').ap()
        VAD = nc.dram_tensor('VAD', [128, NT * 130], BF16, kind='Internal').ap()
        VAA = nc.dram_tensor('VAA', [NR * 128, NT * 130], BF16, kind='Internal').ap()
        AFD = nc.dram_tensor('AFD', [128, NT * 16], F32, kind='Internal').ap()
        AFA = nc.dram_tensor('AFA', [NR * 128, NT * 16], F32, kind='Internal').ap()
        sel_in = din('sel', [128, 2], I32)
        SEL = sb('sel_s', [128, 2], I32)
        dma(SEL[:], sel_in, [], ['sel'])

    def gather_rows(out_ap, src_ap, r_, r, w):
        return P.op('gpsimd', lambda e: e.indirect_dma_start(
            out=out_ap, out_offset=None, in_=src_ap,
            in_offset=bass.IndirectOffsetOnAxis(ap=SEL[:, r_:r_ + 1], axis=0)), list(r) + ['sel'], w, dma=True)

    def allgather(out_ap, in_ap, r, w):
        return P.op('gpsimd', lambda e: e.collective_compute('AllGather', op=ALU.bypass, replica_groups=rg,
                                                             ins=[in_ap], outs=[out_ap]), r, w, dma=True)
    W = {}
    for nm, shp in [('mix_norm_g', [L, D]), ('w_in', [L, D, 1792]), ('gm_v_norm_g', [L, 512]),
                    ('gm_w_s', [L, 4, 128, 128]), ('gm_b_s', [L, 4, 128]), ('q_norm_g', [L, 64]),
                    ('k_norm_g', [L, 64]), ('branch_norm_g', [L, 2, 512]), ('w_out', [L, D, D]),
                    ('xattn_norm_g', [L, D]), ('mem_norm_g', [1, D]), ('xattn_w_q', [L, D, D]),
                    ('xattn_w_kv', [L, D, 2 * D]), ('xattn_w_o', [L, D, D]), ('ffn_norm_g', [L, D]),
                    ('w_router', [L, D, NE]), ('w_gate', [L, NE, D, D]), ('w_up', [L, NE, D, D]),
                    ('w_down', [L, NE, D, D]), ('rope', [S, 128]), ('identf', [128, 128])]:
        W[nm] = din(nm, shp)
    W['identb'] = din('identb', [128, 128], BF16)
    W['triu'] = din('triu', [128, 128], BF16)
    W['iota32'] = din('iota32', [128, 32])
    W['pidx'] = din('pidx', [128, 1])

    F4 = [sb(f'F4{i}', [128, 1024]) for i in range(4)]
    F2 = [sb(f'F2{i}', [128, 512]) for i in range(5)]
    H8 = [sb(f'H8{i}', [128, 4096], BF16) for i in range(3)]
    H4 = [sb(f'H4{i}', [128, 2048], BF16) for i in range(2)]
    H2 = [sb(f'H2{i}', [128, 1024], BF16) for i in range(4)]
    WBt = sb('WB', [128, 4 * 8192], BF16)
    STG = [sb(f'STG{i}', [128, 2048]) for i in range(2)]
    RES = sb('RES', [128, 8704])
    SM = sb('SM', [128, 256])
    identb = sb('identb_s', [128, 128], BF16)
    identf = sb('identf_s', [128, 128])
    onesf = sb('onesf', [128, 128])
    onesb = sb('onesb', [128, 128], BF16)
    GV = sb('gvbc', [128, 512])
    GQ = sb('gqbc', [128, 64])
    GK = sb('gkbc', [128, 64])
    CV = sb('colv', [128, 64])
    ROPE = [sb(f'rope{i}', [128, 128]) for i in range(2)]
    PB = [ps(f'PB{i}', [128, 512]) for i in range(8)]

    def WB(k):
        return WBt[:, k * 8192:(k + 1) * 8192]

    KT = RES[:, 0:SK // 2].bitcast(BF16)
    VAUG = RES[:, SK // 2:SK // 2 + NTK * 65].bitcast(BF16).rearrange("p (t h d) -> p t h d", t=NTK, h=2)
    AFF = RES[:, 0:NT * 16].rearrange("p (t e) -> p t e", e=16)
    MG = RES[:, NT * 16:2 * NT * 16].rearrange("p (t e) -> p t e", e=16)
    AFFA = RES[:, 2 * NT * 16:2 * NT * 16 + NTK * 16].rearrange("p (t e) -> p t e", e=16) if rg else AFF
    RSTDGM = SM[:, 0:NT]

    dma(identb[:], W['identb'], [], ['identb'])
    dma(identf[:], W['identf'], [], ['identf'])
    triu = sb('triu_s', [128, 128], BF16)
    IOTA = sb('iota_s', [128, 32])
    PIDX = sb('pidx_s', [128, 1])
    IDXt = sb('idx_s', [128, 128], I32)
    GAt = sb('ga_s', [128, 128])
    dma(triu[:], W['triu'], [], ['triu'])
    dma(IOTA[:], W['iota32'], [], ['iota'])
    dma(PIDX[:], W['pidx'], [], ['pidx'])
    V(lambda e: e.memset(onesf[:], 1.0), [], ['onesf'], 'gpsimd')
    V(lambda e: e.memset(onesb[:], 1.0), [], ['onesb'], 'gpsimd')
    for i in range(0, S, 1024):
        j = min(S, i + 1024)
        dma(X[i:j, :], c.x_in[i:j, :], [], ['X'])

    stg_n = [0]

    def load_w(dst3, dkeys, src2, KC, N, rowscale=None, mul=1.0, part=128, rkeys=(), eng='gpsimd'):
        cols = max(1, 2048 // KC)
        for n0 in range(0, N, cols):
            n1 = min(N, n0 + cols)
            k = stg_n[0] % 2
            stg_n[0] += 1
            stv = STG[k][0:part, 0:KC * (n1 - n0)].rearrange("p (c n) -> p c n", c=KC)
            dma(stv, src2[:, n0:n1].rearrange("(c p) n -> p c n", p=part), [], [f'STG{k}'])
            if rowscale is None and eng == 'scalar':
                act(dst3[:, :, n0:n1], stv, AF.Copy, [f'STG{k}'], dkeys)
            elif rowscale is None:
                V(lambda e, stv=stv, n0=n0, n1=n1: e.tensor_copy(out=dst3[:, :, n0:n1], in_=stv),
                  [f'STG{k}'], dkeys, 'gpsimd')
            else:
                for cc in range(KC):
                    V(lambda e, stv=stv, n0=n0, n1=n1, cc=cc: e.tensor_scalar(
                        out=dst3[:, cc, n0:n1], in0=stv[:, cc, :], scalar1=rowscale[:, cc:cc + 1],
                        scalar2=mul, op0=ALU.mult, op1=ALU.mult),
                      [f'STG{k}'] + list(rkeys), dkeys, 'gpsimd')

    def colvec(dst, src1d, p, key):
        P.op('sync', lambda e: e.dma_start(out=dst, in_=src1d.rearrange("(c p) -> p c", p=p),
                                           allow_slow_non_contiguous=True), [], [key], dma=True)

    tb_n = [0]

    def norm_T(xt, xkey, dst3, dkeys, hbuf, hkey, gain_bc=None, gkey=None, tbanks=(0, 1)):
        ssq = SM[:, 128:129]
        rs = SM[:, 129:130]
        act(hbuf[:, 0:1024], xt, AF.Square, [xkey], [hkey, 'ssq'], accum_out=ssq)
        rstd_from_ssq(rs, ssq, D, ['ssq'], ['rs'])
        act(hbuf[:, 0:1024], xt, AF.Copy, [xkey, 'rs'], [hkey], scale=rs)
        if gain_bc is not None:
            V(lambda e: e.tensor_tensor(out=hbuf[:, 0:1024], in0=hbuf[:, 0:1024], in1=gain_bc, op=ALU.mult),
              [hkey, gkey], [hkey])
        bk = tbanks[tb_n[0] % len(tbanks)]
        tb_n[0] += 1
        pt = PB[bk][:].bitcast(BF16).rearrange("p (c n) -> p c n", c=8)
        for cc in range(8):
            tr(pt[:, cc, :], hbuf[:, cc * 128:(cc + 1) * 128], identb[:], [hkey, 'identb'], [f'PB{bk}'])
        V(lambda e: e.tensor_copy(out=dst3, in_=pt), [f'PB{bk}'], dkeys)

    def phase_B(l):
        colvec(CV[:, 0:8], W['mix_norm_g'][l], 128, 'cv_mix')
        WIN = WBt[:, 2 * 8192:2 * 8192 + 8 * 1792].rearrange("p (c n) -> p c n", c=8)
        load_w(WIN, ['WB2', 'WB3'], W['w_in'][l], 8, 1792, rowscale=CV[:, 0:8], rkeys=['cv_mix'])
        dma(GV[:], W['gm_v_norm_g'][l:l + 1, :].partition_broadcast(128), [], ['gvbc'])
        dma(GQ[:], W['q_norm_g'][l:l + 1, :].partition_broadcast(128), [], ['gqbc'])
        dma(GK[:], W['k_norm_g'][l:l + 1, :].partition_broadcast(128), [], ['gkbc'])
        V(lambda e: e.tensor_scalar(out=GQ[:], in0=GQ[:], scalar1=0.125, scalar2=None, op0=ALU.mult),
          ['gqbc'], ['gqbc'])
        P.op('sync', lambda e: e.dma_start(out=CV[:, 8:12], in_=W['gm_b_s'][l].rearrange("g i -> i g"),
                                           allow_slow_non_contiguous=True), [], ['cv_bs'], dma=True)
        WST = H8[2][:, 0:512].rearrange("p (g i) -> p g i", g=4)
        for g in range(4):
            dma(F2[4][:, g * 128:(g + 1) * 128], W['gm_w_s'][l, g], [], ['F24'])
        V(lambda e: e.tensor_copy(out=H8[2][:, 512:1024], in_=F2[4][:]), ['F24'], ['H82s'])
        ptw = PB[0][:].bitcast(BF16)
        for g in range(4):
            tr(ptw[:, g * 128:(g + 1) * 128], H8[2][:, 512 + g * 128:512 + (g + 1) * 128], identb[:], ['H82s', 'identb'], ['PB0'])
        V(lambda e: e.tensor_copy(out=H8[2][:, 0:512], in_=ptw[:, 0:512]), ['PB0'], ['H82w'])
        V(lambda e: e.memset(VAUG[:, :, :, 64:65], 1.0), [], ['VAUG'], 'gpsimd')

        for i in range(NT):
            b = i % 2
            xt, xk = F4[b], f'F4{b}'
            dma(xt[:], X[i * 128:(i + 1) * 128, :], ['X'], [xk])
            dma(ROPE[b][:], W['rope'][i * 128:(i + 1) * 128, :], [], [f'rope{b}'])
            hT = H2[b][:].rearrange("p (c n) -> p c n", c=8)
            norm_T(xt[:], xk, hT, [f'H2{b}'], H4[0], 'H40')
            for gi, (c0, c1, bk) in enumerate([(0, 512, 2), (512, 1024, 3), (1024, 1536, 4), (1536, 1792, 5)]):
                for cc in range(8):
                    mm(PB[bk][:, 0:c1 - c0], hT[:, cc, :], WIN[:, cc, c0:c1], cc == 0, cc == 7,
                       [f'H2{b}', 'WB2', 'WB3'], [f'PB{bk}'])
            GU, GVt, GM = F2[0], F2[1], F2[2]
            act(GU[:], PB[2][:], GELU, ['PB2'], ['F20'])
            act(GVt[:], PB[3][:], GELU, ['PB3'], ['F21'])
            act(F2[3][:], GVt[:], AF.Square, ['F21'], ['F23', 'ssqv'], accum_out=SM[:, 130:131])
            rstd_from_ssq(SM[:, 131:132], SM[:, 130:131], 512, ['ssqv'], ['rsv'])
            VN = H4[1][:, 0:512]
            V(lambda e: e.scalar_tensor_tensor(out=VN, in0=GVt[:], scalar=SM[:, 131:132], in1=GV[:],
                                               op0=ALU.mult, op1=ALU.mult), ['F21', 'rsv', 'gvbc'], ['H41'])
            for g in range(4):
                mm(PB[6][:, g * 128:(g + 1) * 128], WST[:, g, :], VN[:, g * 128:(g + 1) * 128], True, True,
                   ['H82w', 'H41'], ['PB6'])
            for g in range(4):
                V(lambda e, g=g: e.scalar_tensor_tensor(
                    out=GM[:, g * 128:(g + 1) * 128], in0=PB[6][:, g * 128:(g + 1) * 128], scalar=CV[:, 8 + g:9 + g],
                    in1=GU[:, g * 128:(g + 1) * 128], op0=ALU.add, op1=ALU.mult), ['PB6', 'cv_bs', 'F20'], ['F22'])
            act(F2[3][:], GM[:], AF.Square, ['F22'], ['F23', 'ssqg'], accum_out=SM[:, 132:133])
            rstd_from_ssq(RSTDGM[:, i:i + 1], SM[:, 132:133], 512, ['ssqg'], ['rstdgm'])
            GMB = H4[1][:, 512:1024]
            act(GMB, GM[:], AF.Copy, ['F22'], ['H41b'])
            QN = F2[3]
            act(F2[4][:], PB[4][:], AF.Square, ['PB4'], ['F24'])
            V(lambda e: e.tensor_reduce(out=SM[:, 136:144], in_=F2[4][:].rearrange("p (h d) -> p h d", h=8),
                                        axis=AX.X, op=ALU.add), ['F24'], ['ssqq'])
            rstd_from_ssq(SM[:, 136:144], SM[:, 136:144], 64, ['ssqq'], ['ssqq'])
            V(lambda e: e.tensor_tensor(out=QN[:].rearrange("p (h d) -> p h d", h=8),
                                        in0=PB[4][:].rearrange("p (h d) -> p h d", h=8),
                                        in1=SM[:, 136:144].unsqueeze(2).to_broadcast([128, 8, 64]), op=ALU.mult),
              ['PB4', 'ssqq'], ['F23'])
            V(lambda e: e.tensor_tensor(out=QN[:].rearrange("p (h d) -> p h d", h=8),
                                        in0=QN[:].rearrange("p (h d) -> p h d", h=8),
                                        in1=GQ[:].unsqueeze(1).to_broadcast([128, 8, 64]), op=ALU.mult),
              ['F23', 'gqbc'], ['F23'])
            rp = ROPE[b]
            rk = f'rope{b}'

            def rope(src, nh, dst_view, skey, dkey, TT, tkey, rp=rp, rk=rk):
                s4 = src.rearrange("p (h i two) -> p h i two", h=nh, two=2)
                t4 = TT.rearrange("p (h i two) -> p h i two", h=nh, two=2)
                V(lambda e: e.tensor_tensor(out=t4[:, :, :, 0], in0=s4[:, :, :, 1],
                                            in1=rp[:, 64:96].unsqueeze(1).to_broadcast([128, nh, 32]), op=ALU.mult),
                  [skey, rk], [tkey])
                V(lambda e: e.tensor_tensor(out=t4[:, :, :, 1], in0=s4[:, :, :, 0],
                                            in1=rp[:, 96:128].unsqueeze(1).to_broadcast([128, nh, 32]), op=ALU.mult),
                  [skey, rk], [tkey])
                s3 = src.rearrange("p (h d) -> p h d", h=nh)
                V(lambda e: e.tensor_tensor(out=s3, in0=s3, in1=rp[:, 0:64].unsqueeze(1).to_broadcast([128, nh, 64]),
                                            op=ALU.mult), [skey, rk], [skey])
                if nh == 8:
                    a4 = src.rearrange("p (hh j d) -> p hh j d", hh=2, j=4)
                    b4 = TT.rearrange("p (hh j d) -> p hh j d", hh=2, j=4)
                else:
                    a4 = s3
                    b4 = TT.rearrange("p (h d) -> p h d", h=nh)
                V(lambda e: e.tensor_tensor(out=dst_view, in0=a4, in1=b4, op=ALU.add), [skey, tkey], [dkey])

            QR = H4[0][:, 1024:1536]
            rope(QN[:], 8, QR.rearrange("p (j hh d) -> p hh j d", j=4, hh=2), 'F23', 'H40q', F2[4][:], 'F24')
            KN = F2[4][:, 0:128]
            act(F2[3][:, 0:128], PB[5][:, 0:128], AF.Square, ['PB5', 'H40q'], ['F23'])
            V(lambda e: e.tensor_reduce(out=SM[:, 144:146], in_=F2[3][:, 0:128].rearrange("p (h d) -> p h d", h=2),
                                        axis=AX.X, op=ALU.add), ['F23'], ['ssqk'])
            rstd_from_ssq(SM[:, 144:146], SM[:, 144:146], 64, ['ssqk'], ['ssqk'])
            V(lambda e: e.tensor_tensor(out=KN.rearrange("p (h d) -> p h d", h=2),
                                        in0=PB[5][:, 0:128].rearrange("p (h d) -> p h d", h=2),
                                        in1=SM[:, 144:146].unsqueeze(2).to_broadcast([128, 2, 64]), op=ALU.mult),
              ['PB5', 'ssqk'], ['F24'])
            V(lambda e: e.tensor_tensor(out=KN.rearrange("p (h d) -> p h d", h=2),
                                        in0=KN.rearrange("p (h d) -> p h d", h=2),
                                        in1=GK[:].unsqueeze(1).to_broadcast([128, 2, 64]), op=ALU.mult),
              ['F24', 'gkbc'], ['F24'])
            KR = H4[0][:, 1536:1664]
            rope(KN, 2, KR.rearrange("p (h d) -> p h d", h=2), 'F24', 'H40k', F2[3][:, 128:256], 'F23')
            V(lambda e, i=i: e.tensor_copy(out=VAUG[:, i, :, 0:64],
                                           in_=PB[5][:, 128:256].rearrange("p (h d) -> p h d", h=2)),
              ['PB5'], ['VAUG'])
            pt = PB[7][:].bitcast(BF16)
            for g in range(4):
                tr(pt[:, g * 128:(g + 1) * 128], GMB[:, g * 128:(g + 1) * 128], identb[:], ['H41b', 'identb'], ['PB7'])
            for j in range(4):
                tr(pt[:, 512 + j * 128:512 + (j + 1) * 128], QR[:, j * 128:(j + 1) * 128], identb[:],
                   ['H40q', 'identb'], ['PB7'])
            TS = H4[b][:, 0:0]
            OUTS = H2[2 + b]
            ok = f'H2{2 + b}'
            if i == 0:
                pass
            V(lambda e, OUTS=OUTS: e.tensor_copy(out=OUTS[:], in_=pt), ['PB7'], [ok])
            dma(GMT[:, :, i * 128:(i + 1) * 128].rearrange("c p n -> p c n"),
                OUTS[:, 0:512].rearrange("p (c n) -> p c n", c=4), [ok], ['GMT'])
            dma(QT[:, :, i * 128:(i + 1) * 128].rearrange("c p n -> p c n"),
                OUTS[:, 512:1024].rearrange("p (c n) -> p c n", c=4), [ok], ['QT'])
            bk = 'PB6'
            ptk = PB[6][:].bitcast(BF16)
            tr(ptk[:, 0:128], KR, identb[:], ['H40k', 'identb'], ['PB6'])
            V(lambda e, i=i: e.tensor_copy(out=KT[:, i * 128:(i + 1) * 128], in_=ptk[:, 0:128]), ['PB6'], ['KT'])

    def exchange_kv():
        if not rg:
            return
        dma(KTD, KT[:, 0:S], ['KT'], ['KTD'])
        dma(VAD, RES[:, SK // 2:SK // 2 + NT * 65].bitcast(BF16), ['VAUG'], ['VAD'])
        allgather(KTA, KTD, ['KTD'], ['KTA'])
        allgather(VAA, VAD, ['VAD'], ['VAA'])
        for r_ in range(2):
            gather_rows(KT[:, r_ * S:(r_ + 1) * S], KTA, r_, ['KTA'], ['KT'])
            gather_rows(RES[:, SK // 2 + r_ * NT * 65:SK // 2 + (r_ + 1) * NT * 65].bitcast(BF16), VAA, r_,
                        ['VAA'], ['VAUG'])

    def phase_C(l):
        exchange_kv()
        colvec(CV[:, 16:20], W['branch_norm_g'][l, 0], 128, 'cv_b0')
        colvec(CV[0:64, 20:28], W['branch_norm_g'][l, 1], 64, 'cv_b1')
        WO0 = WB(0)[:, 0:4096].rearrange("p (c n) -> p c n", c=4)
        WO1 = WB(1)[0:64, :].rearrange("p (c n) -> p c n", c=8)
        load_w(WO0, ['WB0'], W['w_out'][l, 0:512, :], 4, 1024, rowscale=CV[:, 16:20], rkeys=['cv_b0'])
        load_w(WO1, ['WB1'], W['w_out'][l, 512:1024, :], 8, 1024, rowscale=CV[0:64, 20:28], rkeys=['cv_b1'], part=64)
        NKP = NTK // 2
        GS = min(512, S)
        NGG = S // GS
        for g in range(NGG):
            t0 = g * GS
            qTg = H4[g % 2][:, 0:4 * GS].rearrange("p (j n) -> p j n", j=4)
            qk = f'H4{g % 2}'
            dma(qTg, QT[:, :, t0:t0 + GS].rearrange("c p n -> p c n"), ['QT'], [qk])
            ATT = H8[0][0:64, 0:8 * GS].rearrange("p (h n) -> p h n", h=8)
            SQACC = F2[4][0:64, 0:GS]
            for h in range(8):
                j, hh = h % 4, h // 4
                ob = 4 + (h % 2)
                OT = PB[ob]
                for kp in range(NKP):
                    sbk = (kp % 2) * 2
                    for k2 in range(2):
                        kt = kp * 2 + k2
                        mm(PB[sbk + k2][:, 0:GS], KT[hh * 64:(hh + 1) * 64, kt * 128:(kt + 1) * 128],
                           qTg[hh * 64:(hh + 1) * 64, j, :], True, True, ['KT', qk], [f'PB{sbk + k2}'])
                    PT = H2[kp % 3]
                    pk = f'H2{kp % 3}'
                    for k2 in range(2):
                        act(PT[:, k2 * 512:k2 * 512 + GS], PB[sbk + k2][:, 0:GS], AF.Exp, [f'PB{sbk + k2}'], [pk])
                    for k2 in range(2):
                        kt = kp * 2 + k2
                        mm(OT[0:65, 0:GS], VAUG[:, kt, hh, :], PT[:, k2 * 512:k2 * 512 + GS],
                           kt == 0, kt == NTK - 1, ['VAUG', pk], [f'PB{ob}'])
                SR = F2[3]
                V(lambda e, OT=OT: e.tensor_copy(out=SR[64:65, 0:GS], in_=OT[64:65, 0:GS]), [f'PB{ob}'], ['F23'])
                mm(PB[6][0:64, 0:GS], onesf[64:65, 0:64], SR[64:65, 0:GS], True, True, ['onesf', 'F23'], ['PB6'])
                Rr = F2[2]
                V(lambda e: e.reciprocal(out=Rr[0:64, 0:GS], in_=PB[6][0:64, 0:GS]), ['PB6'], ['F22'])
                V(lambda e, OT=OT, h=h: e.tensor_tensor(out=ATT[:, h, :], in0=OT[0:64, 0:GS], in1=Rr[0:64, 0:GS],
                                                        op=ALU.mult), [f'PB{ob}', 'F22'], ['H80'])
                if h == 0:
                    V(lambda e, h=h: e.tensor_tensor(out=SQACC, in0=ATT[:, h, :], in1=ATT[:, h, :], op=ALU.mult),
                      ['H80'], ['F24'], 'gpsimd')
                else:
                    V(lambda e, h=h: e.tensor_tensor(out=F2[1][0:64, 0:GS], in0=ATT[:, h, :], in1=ATT[:, h, :],
                                                     op=ALU.mult), ['H80'], ['F21'], 'gpsimd')
                    V(lambda e: e.tensor_tensor(out=SQACC, in0=SQACC, in1=F2[1][0:64, 0:GS], op=ALU.add),
                      ['F21', 'F24'], ['F24'], 'gpsimd')
            mm(PB[6][0:1, 0:GS], onesf[0:64, 0:1], SQACC, True, True, ['onesf', 'F24'], ['PB6'])
            V(lambda e: e.tensor_copy(out=F2[3][0:1, 0:GS], in_=PB[6][0:1, 0:GS]), ['PB6'], ['F23'])
            for tt in range(GS // 128):
                mm(PB[6][:, tt:tt + 1], F2[3][0:1, tt * 128:(tt + 1) * 128], onesf[0:1, 0:1], True, True,
                   ['F23', 'onesf'], ['PB6'])
            rstd_from_ssq(SM[:, 148:148 + GS // 128], PB[6][:, 0:GS // 128], 512, ['PB6'], ['rsat'])
            gmTg = H8[1][:, 0:4 * GS].rearrange("p (c n) -> p c n", c=4)
            dma(gmTg, GMT[:, :, t0:t0 + GS].rearrange("c p n -> p c n"), ['GMT'], ['H81'])
            for tt in range(GS // 128):
                i = g * (GS // 128) + tt
                b = tt % 2
                xt, xk = F4[b], f'F4{b}'
                dma(xt[:], X[i * 128:(i + 1) * 128, :], ['X'], [xk])
                for half in range(2):
                    hs = slice(half * 512, (half + 1) * 512)
                    for cc in range(4):
                        mm(PB[7][:], gmTg[:, cc, tt * 128:(tt + 1) * 128], WO0[:, cc, hs], cc == 0, cc == 3,
                           ['H81', 'WB0'], ['PB7'])
                    V(lambda e, xt=xt, hs=hs, i=i: e.scalar_tensor_tensor(
                        out=xt[:, hs], in0=PB[7][:], scalar=RSTDGM[:, i:i + 1], in1=xt[:, hs],
                        op0=ALU.mult, op1=ALU.add), ['PB7', 'rstdgm', xk], [xk])
                    for h in range(8):
                        mm(PB[7][:], ATT[:, h, tt * 128:(tt + 1) * 128], WO1[:, h, hs], h == 0, h == 7,
                           ['H80', 'WB1'], ['PB7'])
                    V(lambda e, xt=xt, hs=hs, tt=tt: e.scalar_tensor_tensor(
                        out=xt[:, hs], in0=PB[7][:], scalar=SM[:, 148 + tt:149 + tt], in1=xt[:, hs],
                        op0=ALU.mult, op1=ALU.add), ['PB7', 'rsat', xk], [xk])
                dma(X[i * 128:(i + 1) * 128, :], xt[:], [xk], ['X'])

    MNTt = sb('MNT', [128, 2048], BF16)
    MNT = MNTt[:].rearrange("p (c n) -> p c n", c=8)
    KXt = sb('KX', [128, 2048], BF16)
    KX = KXt[:].rearrange("p (c n) -> p c n", c=8)
    VXt = sb('VX', [128, 2048], BF16)
    VX = VXt[:].rearrange("p (m n) -> p m n", m=2)
    WRf = sb('WRf', [128, 128])

    def phase_A0():
        dma(F4[2][:], W['mem_norm_g'].partition_broadcast(128), [], ['F42'])
        for mt in range(2):
            dma(F4[mt][:], c.mem[mt * 128:(mt + 1) * 128, :], [], [f'F4{mt}'])
            norm_T(F4[mt][:], f'F4{mt}', MNT[:, :, mt * 128:(mt + 1) * 128], ['MNT'], H4[0], 'H40',
                   gain_bc=F4[2][:], gkey='F42')

    def phase_A(l):
        WK = WB(2).rearrange("p (c n) -> p c n", c=8)
        WV = WB(3).rearrange("p (c n) -> p c n", c=8)
        load_w(WK, ['WB2'], W['xattn_w_kv'][l, :, 0:1024], 8, 1024)
        load_w(WV, ['WB3'], W['xattn_w_kv'][l, :, 1024:2048], 8, 1024)
        for dc in range(8):
            bk = dc % 2
            for cc in range(8):
                mm(PB[bk][:, 0:256], WK[:, cc, dc * 128:(dc + 1) * 128], MNT[:, cc, :], cc == 0, cc == 7,
                   ['WB2', 'MNT'], [f'PB{bk}'])
            V(lambda e, dc=dc, bk=bk: e.tensor_copy(out=KX[:, dc, :], in_=PB[bk][:, 0:256]), [f'PB{bk}'], ['KX'])
        for mt in range(2):
            for half in range(2):
                bk = 2 + half
                for cc in range(8):
                    mm(PB[bk][:], MNT[:, cc, mt * 128:(mt + 1) * 128], WV[:, cc, half * 512:(half + 1) * 512],
                       cc == 0, cc == 7, ['WB3', 'MNT'], [f'PB{bk}'])
                V(lambda e, mt=mt, half=half, bk=bk: e.tensor_copy(out=VX[:, mt, half * 512:(half + 1) * 512],
                                                                   in_=PB[bk][:]), [f'PB{bk}'], ['VX'])

    def phase_D(l):
        colvec(CV[:, 32:40], W['xattn_norm_g'][l], 128, 'cv_x')
        WQ = WB(0).rearrange("p (c n) -> p c n", c=8)
        WOX = WB(1).rearrange("p (c n) -> p c n", c=8)
        load_w(WQ, ['WB0'], W['xattn_w_q'][l], 8, 1024, rowscale=CV[:, 32:40], mul=1.0 / 16.0, rkeys=['cv_x'])
        load_w(WOX, ['WB1'], W['xattn_w_o'][l], 8, 1024)
        GS = min(512, S)
        for g in range(S // GS):
            t0 = g * GS
            H2Tg = H8[1][:, 0:8 * GS].rearrange("p (c n) -> p c n", c=8)
            for tt in range(GS // 128):
                i = g * (GS // 128) + tt
                b = tt % 2
                dma(F4[b][:], X[i * 128:(i + 1) * 128, :], ['X'], [f'F4{b}'])
                norm_T(F4[b][:], f'F4{b}', H2Tg[:, :, tt * 128:(tt + 1) * 128], ['H81'], H4[0], 'H40')
            QX = H8[2][:, 0:8 * GS].rearrange("p (c n) -> p c n", c=8)
            for dc in range(8):
                bk = 2 + dc % 2
                for cc in range(8):
                    mm(PB[bk][:, 0:GS], WQ[:, cc, dc * 128:(dc + 1) * 128], H2Tg[:, cc, :], cc == 0, cc == 7,
                       ['WB0', 'H81'], [f'PB{bk}'])
                act(QX[:, dc, :], PB[bk][:, 0:GS], AF.Copy, [f'PB{bk}'], ['H82'])
            OXT = H8[0][:, 0:8 * GS].rearrange("p (c n) -> p c n", c=8)
            for h in range(4):
                PX = H2[2 + h % 2]
                pk = f'H2{2 + h % 2}'
                for mt in range(2):
                    for dd in range(2):
                        mm(PB[4 + mt][:, 0:GS], KX[:, 2 * h + dd, mt * 128:(mt + 1) * 128], QX[:, 2 * h + dd, :],
                           dd == 0, dd == 1, ['KX', 'H82'], [f'PB{4 + mt}'])
                    act(PX[:, mt * 512:mt * 512 + GS], PB[4 + mt][:, 0:GS], AF.Exp, [f'PB{4 + mt}'], [pk])
                for mt in range(2):
                    mm(PB[6][:, 0:GS], onesb[:], PX[:, mt * 512:mt * 512 + GS], mt == 0, mt == 1,
                       ['onesb', pk], ['PB6'])
                V(lambda e: e.reciprocal(out=F2[2][:, 0:GS], in_=PB[6][:, 0:GS]), ['PB6'], ['F22'])
                for dd in range(2):
                    for mt in range(2):
                        mm(PB[7][:, 0:GS], VX[:, mt, (2 * h + dd) * 128:(2 * h + dd + 1) * 128],
                           PX[:, mt * 512:mt * 512 + GS], mt == 0, mt == 1, ['VX', pk], ['PB7'])
                    V(lambda e, h=h, dd=dd: e.tensor_tensor(out=OXT[:, 2 * h + dd, :], in0=PB[7][:, 0:GS],
                                                            in1=F2[2][:, 0:GS], op=ALU.mult),
                      ['PB7', 'F22'], ['H80'])
            for tt in range(GS // 128):
                i = g * (GS // 128) + tt
                b = tt % 2
                xt, xk = F4[2 + b], f'F4{2 + b}'
                dma(xt[:], X[i * 128:(i + 1) * 128, :], ['X'], [xk])
                for half in range(2):
                    hs = slice(half * 512, (half + 1) * 512)
                    bk = 2 + half
                    for cc in range(8):
                        mm(PB[bk][:], OXT[:, cc, tt * 128:(tt + 1) * 128], WOX[:, cc, hs], cc == 0, cc == 7,
                           ['H80', 'WB1'], [f'PB{bk}'])
                    V(lambda e, xt=xt, hs=hs, bk=bk: e.tensor_tensor(out=xt[:, hs], in0=PB[bk][:], in1=xt[:, hs],
                                                                     op=ALU.add), [f'PB{bk}', xk], [xk])
                dma(X[i * 128:(i + 1) * 128, :], xt[:], [xk], ['X'])

    def phase_E(l):
        colvec(CV[:, 40:48], W['ffn_norm_g'][l], 128, 'cv_f')
        WR3 = WRf[:].rearrange("p (c e) -> p c e", c=8)
        dma(WR3, W['w_router'][l].rearrange("(c p) e -> p c e", p=128), [], ['WRf'])
        for cc in range(8):
            V(lambda e, cc=cc: e.tensor_scalar(out=WR3[:, cc, :], in0=WR3[:, cc, :], scalar1=CV[:, 40 + cc:41 + cc],
                                               scalar2=None, op0=ALU.mult), ['WRf', 'cv_f'], ['WRf'])
        for i in range(NT):
            b = i % 2
            xt, xk = F4[b], f'F4{b}'
            dma(xt[:], X[i * 128:(i + 1) * 128, :], ['X'], [xk])
            act(F4[2][:], xt[:], AF.Square, [xk], ['F42', 'ssq'], accum_out=SM[:, 128:129])
            rstd_from_ssq(SM[:, 129:130], SM[:, 128:129], D, ['ssq'], ['rs'])
            act(F4[2][:], xt[:], AF.Copy, [xk, 'rs'], ['F42'], scale=SM[:, 129:130])
            for cc in range(8):
                bk = cc // 4
                tr(PB[bk][:, (cc % 4) * 128:(cc % 4 + 1) * 128], F4[2][:, cc * 128:(cc + 1) * 128], identf[:],
                   ['F42', 'identf'], [f'PB{bk}'])
            V(lambda e: e.tensor_copy(out=F4[3][:, 0:512], in_=PB[0][:]), ['PB0'], ['F43'])
            V(lambda e: e.tensor_copy(out=F4[3][:, 512:1024], in_=PB[1][:]), ['PB1'], ['F43'])
            for cc in range(8):
                mm(PB[2][:, 0:16], F4[3][:, cc * 128:(cc + 1) * 128], WR3[:, cc, :], cc == 0, cc == 7,
                   ['F43', 'WRf'], ['PB2'])
            V(lambda e: e.reduce_max(out=SM[:, 150:151], in_=PB[2][:, 0:16], axis=AX.X), ['PB2'], ['smx'])
            V(lambda e: e.tensor_scalar(out=SM[:, 150:151], in0=SM[:, 150:151], scalar1=-1.0, scalar2=None,
                                        op0=ALU.mult), ['smx'], ['smx'])
            act(SM[:, 160:176], PB[2][:, 0:16], AF.Exp, ['PB2', 'smx'], ['sme', 'sms'], bias=SM[:, 150:151],
                accum_out=SM[:, 151:152])
            V(lambda e: e.reciprocal(out=SM[:, 151:152], in_=SM[:, 151:152]), ['sms'], ['sms'])
            V(lambda e, i=i: e.tensor_scalar(out=AFF[:, i, :], in0=SM[:, 160:176], scalar1=SM[:, 151:152],
                                             scalar2=None, op0=ALU.mult), ['sme', 'sms'], ['AFF'])
            hb = H2[b]
            V(lambda e, hb=hb: e.tensor_copy(out=hb[:], in_=F4[2][:]), ['F42'], [f'H2{b}'])
            dma(H3R[i * 128:(i + 1) * 128, :], hb[:], [f'H2{b}'], ['H3R'])
            dma(AFFD[i * 128:(i + 1) * 128, :], AFF[:, i, :], ['AFF'], ['AFFD'])
        if rg:
            dma(AFD, RES[:, 0:NT * 16], ['AFF'], ['AFD'])
            allgather(AFA, AFD, ['AFD'], ['AFA'])
            for r_ in range(2):
                gather_rows(RES[:, 2 * NT * 16 + r_ * NT * 16:2 * NT * 16 + (r_ + 1) * NT * 16], AFA, r_,
                            ['AFA'], ['AFFA'])
        akey = 'AFFA' if rg else 'AFF'
        LO, HI, MID = SM[:, 160:176], SM[:, 176:192], SM[:, 192:208]
        PART, CC, D1 = SM[:, 208:224], SM[:, 224:240], SM[:, 240:256]
        CMPA = F4[2][:, 0:NTK * 16].rearrange("p (t e) -> p t e", e=16)
        CMP = F4[2][:, 0:NT * 16].rearrange("p (t e) -> p t e", e=16)
        V(lambda e: e.memset(LO, 0.0), ['sme'], ['lo'])
        V(lambda e: e.memset(HI, 1.0), [], ['hi'])
        for it in range(32):
            V(lambda e: e.tensor_tensor(out=MID, in0=LO, in1=HI, op=ALU.add), ['lo', 'hi'], ['mid'])
            V(lambda e: e.tensor_scalar(out=MID, in0=MID, scalar1=0.5, scalar2=None, op0=ALU.mult), ['mid'], ['mid'])
            V(lambda e: e.tensor_tensor(out=CMPA, in0=AFFA, in1=MID.unsqueeze(1).to_broadcast([128, NTK, 16]),
                                        op=ALU.is_gt), [akey, 'mid'], ['F42'])
            V(lambda e: e.tensor_reduce(out=PART, in_=CMPA.rearrange("p t e -> p e t"), axis=AX.X, op=ALU.add),
              ['F42'], ['part'])
            mm(PB[3][:, 0:16], onesf[:], PART, True, True, ['onesf', 'part'], ['PB3'])
            V(lambda e: e.tensor_scalar(out=CC, in0=PB[3][:, 0:16], scalar1=float(CAP) - 0.5, scalar2=None,
                                        op0=ALU.is_ge), ['PB3'], ['cc'])
            V(lambda e: e.tensor_tensor(out=D1, in0=MID, in1=LO, op=ALU.subtract), ['mid', 'lo'], ['d1'])
            V(lambda e: e.tensor_tensor(out=D1, in0=D1, in1=CC, op=ALU.mult), ['d1', 'cc'], ['d1'])
            V(lambda e: e.tensor_tensor(out=LO, in0=LO, in1=D1, op=ALU.add), ['d1', 'lo'], ['lo'])
            V(lambda e: e.tensor_tensor(out=D1, in0=HI, in1=MID, op=ALU.subtract), ['mid', 'hi'], ['d1'])
            V(lambda e: e.tensor_tensor(out=D1, in0=D1, in1=CC, op=ALU.mult), ['d1', 'cc'], ['d1'])
            V(lambda e: e.tensor_tensor(out=HI, in0=MID, in1=D1, op=ALU.add), ['d1', 'mid'], ['hi'])
        V(lambda e: e.tensor_tensor(out=CMP, in0=AFF, in1=LO.unsqueeze(1).to_broadcast([128, NT, 16]),
                                    op=ALU.is_gt), ['AFF', 'lo'], ['F42'])
        V(lambda e: e.tensor_tensor(out=MG, in0=CMP, in1=AFF, op=ALU.mult), ['F42', 'AFF'], ['MG'])
        NTE = NT * 16
        Mflat = F4[2][:, 0:NTE]
        Mb = H2[0][:, 0:NTE]
        V(lambda e: e.tensor_copy(out=Mb, in_=Mflat), ['F42'], ['H20'])
        W1 = F4[0][:, 0:NTE]
        NRr = F4[1][:, 0:NTE]
        nhb = (NTE + 511) // 512
        for hb in range(nhb):
            c0, c1 = hb * 512, min(NTE, (hb + 1) * 512)
            mm(PB[hb][:, 0:c1 - c0], triu[:], Mb[:, c0:c1], True, True, ['triu', 'H20'], [f'PB{hb}'])
            mm(PB[2 + hb][:, 0:c1 - c0], onesb[:], Mb[:, c0:c1], True, True, ['onesb', 'H20'], [f'PB{2 + hb}'])
            V(lambda e, hb=hb, c0=c0, c1=c1: e.tensor_copy(out=W1[:, c0:c1], in_=PB[hb][:, 0:c1 - c0]),
              [f'PB{hb}'], ['F40'])
            V(lambda e, hb=hb, c0=c0, c1=c1: e.tensor_copy(out=NRr[:, c0:c1], in_=PB[2 + hb][:, 0:c1 - c0]),
              [f'PB{2 + hb}'], ['F41'])
        cur, ck = F4[1], 'F41'
        oth, ok_ = F4[3], 'F43'
        st_ = 1
        while st_ < NT:
            c3 = cur[:, 0:NTE].rearrange("p (t e) -> p t e", e=16)
            o3 = oth[:, 0:NTE].rearrange("p (t e) -> p t e", e=16)
            V(lambda e, c3=c3, o3=o3, st_=st_: e.tensor_copy(out=o3[:, 0:st_, :], in_=c3[:, 0:st_, :]), [ck], [ok_])
            V(lambda e, c3=c3, o3=o3, st_=st_: e.tensor_tensor(out=o3[:, st_:NT, :], in0=c3[:, st_:NT, :],
                                                               in1=c3[:, 0:NT - st_, :], op=ALU.add), [ck], [ok_])
            cur, ck, oth, ok_ = oth, ok_, cur, ck
            st_ *= 2
        INC = cur[:, 0:NTE]
        V(lambda e: e.tensor_tensor(out=W1, in0=W1, in1=INC, op=ALU.add), ['F40', ck], ['F40'])
        for hb in range(nhb):
            c0, c1 = hb * 512, min(NTE, (hb + 1) * 512)
            V(lambda e, hb=hb, c0=c0, c1=c1: e.tensor_tensor(out=W1[:, c0:c1], in0=W1[:, c0:c1],
                                                             in1=PB[2 + hb][:, 0:c1 - c0], op=ALU.subtract),
              ['F40', f'PB{2 + hb}'], ['F40'])
        V(lambda e: e.tensor_tensor(out=W1, in0=W1, in1=Mflat, op=ALU.mult), ['F40', 'F42'], ['F40'])
        V(lambda e: e.tensor_scalar(out=W1, in0=W1, scalar1=-1.0, scalar2=None, op0=ALU.add), ['F40'], ['F40'])
        POSI = F4[1][:, 0:NTE].bitcast(I32)
        V(lambda e: e.tensor_copy(out=POSI, in_=W1), ['F40'], ['F41'])
        AI = F4[3][:, 0:NTE].bitcast(I32)
        BI = F4[0][:, 0:NTE].bitcast(I32)
        V(lambda e: e.tensor_single_scalar(out=AI, in_=POSI, scalar=5, op=ALU.arith_shift_right), ['F41'], ['F43'])
        V(lambda e: e.tensor_single_scalar(out=BI, in_=POSI, scalar=31, op=ALU.bitwise_and), ['F41'], ['F40'])
        AFl = F4[2][:, 0:NTE].rearrange("p (t e) -> p t e", e=16)
        BFl = F4[1][:, 0:NTE].rearrange("p (t e) -> p t e", e=16)
        V(lambda e: e.tensor_copy(out=F4[2][:, 0:NTE], in_=AI), ['F43'], ['F42'])
        V(lambda e: e.tensor_copy(out=F4[1][:, 0:NTE], in_=BI), ['F40'], ['F41'])
        for i in range(NT):
            b = i % 2
            OHa = H4[b][:, 0:16 * NA]
            OHa3 = OHa.rearrange("p (e a) -> p e a", e=16)
            OHb3 = F2[b][:, 0:512].rearrange("p (e a) -> p e a", e=16)
            Rr_ = H4[b][:, 1024:2048]
            R3 = Rr_.rearrange("p (e a) -> p e a", e=16)
            V(lambda e, i=i, OHa3=OHa3: e.tensor_tensor(
                out=OHa3, in0=AFl[:, i, :].unsqueeze(2).to_broadcast([128, 16, NA]),
                in1=IOTA[:, 0:NA].unsqueeze(1).to_broadcast([128, 16, NA]), op=ALU.is_equal),
              ['F42', 'iota'], [f'H4{b}a'])
            V(lambda e, i=i, OHb3=OHb3: e.tensor_tensor(
                out=OHb3, in0=BFl[:, i, :].unsqueeze(2).to_broadcast([128, 16, 32]),
                in1=IOTA[:, 0:32].unsqueeze(1).to_broadcast([128, 16, 32]), op=ALU.is_equal),
              ['F41', 'iota', f'H4{b}r'], [f'F2{b}'])
            V(lambda e, OHb3=OHb3, R3=R3: e.tensor_scalar(out=R3[:, :, 0:32], in0=OHb3, scalar1=PIDX[:, 0:1],
                                                          scalar2=None, op0=ALU.mult),
              [f'F2{b}', 'pidx'], [f'H4{b}r'])
            V(lambda e, OHb3=OHb3, R3=R3, i=i: e.tensor_scalar(out=R3[:, :, 32:64], in0=OHb3, scalar1=float(i),
                                                               scalar2=None, op0=ALU.mult),
              [f'F2{b}'], [f'H4{b}r'])
            for q in range(4):
                mm(PB[q][0:4 * NA, 0:256], OHa[:, q * 4 * NA:(q + 1) * 4 * NA], Rr_[:, q * 256:(q + 1) * 256],
                   i == 0, i == NT - 1, [f'H4{b}a', f'H4{b}r'], [f'PB{q}'])
        LF = F2[2][:, 0:512]
        LF4 = LF.rearrange("p (q k b) -> p q k b", q=4, k=4)
        CP = F2[3][:, 0:256]
        CP3 = CP.rearrange("p (k b) -> p k b", k=4)
        for q in range(4):
            V(lambda e, q=q: e.tensor_copy(out=CP[0:4 * NA, :], in_=PB[q][0:4 * NA, 0:256]), [f'PB{q}'], ['F23'])
            V(lambda e, q=q: e.scalar_tensor_tensor(out=LF4[0:4 * NA, q, :, :], in0=CP3[0:4 * NA, :, 32:64], scalar=128.0,
                                                    in1=CP3[0:4 * NA, :, 0:32], op0=ALU.mult, op1=ALU.add),
              ['F23'], ['F22'])
        LI = F2[4][:, 0:512].bitcast(I32)
        LI4 = LI.rearrange("p (q k b) -> p q k b", q=4, k=4)
        V(lambda e: e.tensor_copy(out=LI[0:4 * NA, :], in_=LF[0:4 * NA, :]), ['F22'], ['F24'])
        for ex in range(NE):
            q, k = ex // 4, ex % 4
            dma(LISTD[ex].rearrange("(a b) -> a b", b=32), LI4[k * NA:(k + 1) * NA, q, k, :], ['F24'], ['LISTD'])
        IDX3 = IDXt[:, 0:16 * M8].rearrange("p (e m) -> p e m", e=16)
        dma(IDX3, LISTD.rearrange("e (p m) -> p e m", m=M8), ['LISTD'], ['IDX'], allow_slow_non_contiguous=True)
        SG = min(512, CAP)
        for ex in range(NE):
            sl = [(3 * ex + k) % 4 for k in range(3)]
            WGv, WUv, WDv = [WB(s_).rearrange("p (c n) -> p c n", c=8) for s_ in sl]
            kg, ku, kd = [f'WB{s_}' for s_ in sl]
            load_w(WGv, [kg], W['w_gate'][l, ex], 8, 1024, rowscale=CV[:, 40:48], rkeys=['cv_f'])
            load_w(WUv, [ku], W['w_up'][l, ex], 8, 1024, rowscale=CV[:, 40:48], rkeys=['cv_f'])
            load_w(WDv, [kd], W['w_down'][l, ex], 8, 1024, eng='scalar')
            for sg in range(CAP // SG):
                xb = H8[sg % 2]
                xk_ = f'H8{sg % 2}'
                XST = xb[:, 0:8 * SG].rearrange("p (c n) -> p c n", c=8)
                nm = SG // 128
                for m4 in range(nm):
                    m = sg * nm + m4
                    b = m % 2
                    xs_, xsk = H2[2 + b], f'H2{2 + b}'
                    P.op('gpsimd', lambda e, xs_=xs_, ex=ex, m=m: e.indirect_dma_start(
                        out=xs_[:], out_offset=None, in_=H3R,
                        in_offset=bass.IndirectOffsetOnAxis(ap=IDX3[:, ex, m:m + 1], axis=0)),
                         ['H3R', 'IDX'], [xsk], dma=True)
                    P.op('gpsimd', lambda e, ex=ex, m=m: e.indirect_dma_start(
                        out=GAt[:, (m % 8) * 16:(m % 8) * 16 + 16], out_offset=None, in_=AFFD,
                        in_offset=bass.IndirectOffsetOnAxis(ap=IDX3[:, ex, m:m + 1], axis=0)),
                         ['AFFD', 'IDX'], [f'ga{m % 8}'], dma=True)
                    pt = PB[6 + b][:].bitcast(BF16).rearrange("p (c n) -> p c n", c=8)
                    for cc in range(8):
                        tr(pt[:, cc, :], xs_[:, cc * 128:(cc + 1) * 128], identb[:], [xsk, 'identb'], [f'PB{6 + b}'])
                    V(lambda e, pt=pt, m4=m4, XST=XST: e.tensor_copy(out=XST[:, :, m4 * 128:(m4 + 1) * 128], in_=pt),
                      [f'PB{6 + b}'], [xk_])
                ACTT = H8[2][:, 0:8 * SG].rearrange("p (c n) -> p c n", c=8)
                for fc in range(8):
                    ba, bu = fc % 2, 2 + fc % 2
                    for cc in range(8):
                        mm(PB[ba][:, 0:SG], WGv[:, cc, fc * 128:(fc + 1) * 128], XST[:, cc, :], cc == 0, cc == 7,
                           [kg, xk_], [f'PB{ba}'])
                    for cc in range(8):
                        mm(PB[bu][:, 0:SG], WUv[:, cc, fc * 128:(fc + 1) * 128], XST[:, cc, :], cc == 0, cc == 7,
                           [ku, xk_], [f'PB{bu}'])
                    SA = F2[fc % 2]
                    act(SA[:, 0:SG], PB[ba][:, 0:SG], AF.Silu, [f'PB{ba}'], [f'F2{fc % 2}'])
                    V(lambda e, fc=fc, SA=SA, bu=bu, ACTT=ACTT: e.tensor_tensor(out=ACTT[:, fc, :], in0=PB[bu][:, 0:SG],
                                                                               in1=SA[:, 0:SG], op=ALU.mult),
                      [f'PB{bu}', f'F2{fc % 2}'], ['H82'])
                for m4 in range(nm):
                    m = sg * nm + m4
                    ys = F4[m % 2]
                    yk = f'F4{m % 2}'
                    for half in range(2):
                        hs = slice(half * 512, (half + 1) * 512)
                        bk = 4 + half
                        for fc in range(8):
                            mm(PB[bk][:], ACTT[:, fc, m4 * 128:(m4 + 1) * 128], WDv[:, fc, hs], fc == 0, fc == 7,
                               ['H82', kd], [f'PB{bk}'])
                        V(lambda e, ys=ys, hs=hs, bk=bk, m=m, ex=ex: e.tensor_scalar(
                            out=ys[:, hs], in0=PB[bk][:], scalar1=GAt[:, (m % 8) * 16 + ex:(m % 8) * 16 + ex + 1],
                            scalar2=None, op0=ALU.mult), [f'PB{bk}', f'ga{m % 8}'], [yk])
                    P.op('gpsimd', lambda e, ys=ys, ex=ex, m=m: e.indirect_dma_start(
                        out=X, out_offset=bass.IndirectOffsetOnAxis(ap=IDX3[:, ex, m:m + 1], axis=0),
                        in_=ys[:], in_offset=None, compute_op=ALU.add), [yk, 'IDX'], ['X'], dma=True)

    def phase_F():
        dma(F4[2][:], c.final_g.partition_broadcast(128), [], ['F42'])
        for i in range(NT):
            b = i % 2
            xt, xk = F4[b], f'F4{b}'
            dma(xt[:], X[i * 128:(i + 1) * 128, :], ['X'], [xk])
            act(F4[3][:], xt[:], AF.Square, [xk], ['F43', 'ssq'], accum_out=SM[:, 128:129])
            rstd_from_ssq(SM[:, 129:130], SM[:, 128:129], D, ['ssq'], ['rs'])
            yt = H8[b][:].bitcast(F32)[:, 0:1024]
            V(lambda e, xt=xt, yt=yt: e.scalar_tensor_tensor(out=yt, in0=xt[:], scalar=SM[:, 129:130],
                                                             in1=F4[2][:], op0=ALU.mult, op1=ALU.mult),
              [xk, 'rs', 'F42'], [f'H8{b}'])
            dma(c.out[i * 128:(i + 1) * 128, :], yt, [f'H8{b}'], ['OUT'])

    c.phases = dict(B=phase_B, C=phase_C, A=phase_A, D=phase_D, E=phase_E)
    phase_A0()
    P.barrier()
    for l in range(L):
        for ph in phases:
            if ph in c.phases:
                c.phases[ph](l)
                P.barrier()
    phase_F()
    P.barrier()
    P.emit()
    return nc


def rope_table(S):
    rows = S // 64
    row_id = np.repeat(np.arange(rows, dtype=np.float32), 64)
    col_id = np.tile(np.arange(64, dtype=np.float32), rows)
    n_pairs = 16
    freqs = np.exp(-np.log(np.float32(10000.0)) * np.arange(n_pairs, dtype=np.float32) / n_pairs).astype(np.float32)
    ang = np.concatenate([row_id[:, None] * freqs[None, :], col_id[:, None] * freqs[None, :]], axis=-1)
    cos, sin = np.cos(ang).astype(np.float32), np.sin(ang).astype(np.float32)
    tab = np.zeros((S, 128), np.float32)
    tab[:, 0:64:2] = cos
    tab[:, 1:64:2] = cos
    tab[:, 64:96] = -sin
    tab[:, 96:128] = sin
    return tab


WNAMES = ['mix_norm_g', 'w_in', 'gm_v_norm_g', 'gm_w_s', 'gm_b_s', 'q_norm_g', 'k_norm_g', 'branch_norm_g',
          'w_out', 'xattn_norm_g', 'xattn_w_q', 'xattn_w_kv', 'xattn_w_o', 'ffn_norm_g', 'w_router',
          'w_gate', 'w_up', 'w_down']


def make_in_maps(inputs, S, L, nb, split=1):
    shared = {k: np.ascontiguousarray(np.asarray(inputs[k], dtype=np.float32)[:L]) for k in WNAMES}
    shared['mem_norm_g'] = np.asarray(inputs['mem_norm_g'], np.float32).reshape(1, D)
    shared['final_norm_g'] = np.asarray(inputs['final_norm_g'], np.float32).reshape(1, D)
    rope = rope_table(S)
    SL = S // split
    shared['identf'] = np.eye(128, dtype=np.float32)
    shared['identb'] = np.eye(128, dtype=np.float32).astype(ml_dtypes.bfloat16)
    shared['triu'] = np.triu(np.ones((128, 128), np.float32)).astype(ml_dtypes.bfloat16)
    shared['iota32'] = np.tile(np.arange(32, dtype=np.float32)[None, :], (128, 1))
    shared['pidx'] = np.arange(128, dtype=np.float32).reshape(128, 1)
    maps = []
    for b in range(nb):
        for r in range(split):
            m = dict(shared)
            m['x'] = np.ascontiguousarray(np.asarray(inputs['x'], np.float32)[b, r * SL:(r + 1) * SL])
            m['rope'] = np.ascontiguousarray(rope[r * SL:(r + 1) * SL])
            m['mem'] = np.ascontiguousarray(np.asarray(inputs['mem'], np.float32)[b])
            if split > 1:
                m['sel'] = np.stack([(split * b + q) * 128 + np.arange(128) for q in range(split)], axis=1).astype(np.int32)
            maps.append(m)
    return maps


def kernel(**inputs):
    x = np.asarray(inputs['x'])
    B, S, _ = x.shape
    L = np.asarray(inputs['w_in']).shape[0]
    nc = build(S, L)
    maps = make_in_maps(inputs, S, L, B)
    res = run_bass_kernel_spmd(nc, maps, core_ids=list(range(B)))
    return np.stack([np.asarray(r['out'], dtype=np.float32) for r in res.results], axis=0)
```

```python
import numpy as np
import ml_dtypes
from contextlib import ExitStack
import concourse.bass as bass
import concourse.mybir as mybir
from concourse.bass_utils import run_bass_kernel_spmd

F32 = mybir.dt.float32
BF16 = mybir.dt.bfloat16
I32 = mybir.dt.int32
AF = mybir.ActivationFunctionType
ALU = mybir.AluOpType
AX = mybir.AxisListType

D = 1024
MEM = 256
NE = 16
EPS = 1e-6
GELU = AF.Gelu_apprx_tanh
ENGS = ['tensor', 'vector', 'scalar', 'gpsimd', 'sync']
EPOCH = 20000
DEPOCH = 1200
DK = 8


class Prog:
    def __init__(self, nc, stack):
        self.nc = nc
        self.stack = stack
        self.rec = {e: [] for e in ENGS}
        self.cnt = {e: 0 for e in ENGS}
        self.esem = {e: None for e in ENGS}
        self.nsem = 0
        self.dring = {e: [None] * DK for e in ENGS}
        self.duse = {e: [0] * DK for e in ENGS}
        self.dn = {e: 0 for e in ENGS}
        self.waited = {e: {} for e in ENGS}
        self.lastw = {}
        self.readers = {}
        self.ninst = 0

    def newsem(self, tag):
        self.nsem += 1
        return self.stack.enter_context(self.nc.semaphore(f"{tag}_{self.nsem}"))

    def _wait(self, eng, tok):
        sem, val = tok[0], tok[1]
        w = self.waited[eng]
        if w.get(id(sem), 0) >= val:
            return
        w[id(sem)] = val
        self.rec[eng].append(lambda e, s=sem, v=val: e.wait_ge(s, v))

    def op(self, eng, fn, r=(), w=(), dma=False):
        toks = []
        for k in r:
            t = self.lastw.get(k)
            if t is not None:
                toks.append(t)
        for k in w:
            t = self.lastw.get(k)
            if t is not None:
                toks.append(t)
            toks.extend(self.readers.get(k, {}).values())
        for t in toks:
            if t[2] == 'tensor' and eng == 'tensor' and not dma:
                continue
            self._wait(eng, t)
        self.ninst += 1
        if dma:
            slot = self.dn[eng] % DK
            self.dn[eng] += 1
            sem = self.dring[eng][slot]
            prev = self.duse[eng][slot]
            if sem is not None and prev > 0:
                self._wait(eng, (sem, 16 * prev))
            if sem is None or prev >= DEPOCH:
                sem = self.newsem('d' + eng[:2])
                self.dring[eng][slot] = sem
                prev = 0
            self.duse[eng][slot] = prev + 1
            tok = (sem, 16 * (prev + 1), 'dma')
            self.rec[eng].append(lambda e, f=fn, s=sem: f(e).then_inc(s, 16))
        else:
            if self.esem[eng] is None or self.cnt[eng] >= EPOCH:
                self.esem[eng] = self.newsem('e' + eng[:2])
                self.cnt[eng] = 0
            self.cnt[eng] += 1
            sem = self.esem[eng]
            tok = (sem, self.cnt[eng], eng)
            self.rec[eng].append(lambda e, f=fn, s=sem: f(e).then_inc(s, 1))
        for k in r:
            self.readers.setdefault(k, {})[id(tok[0])] = tok
        for k in w:
            self.lastw[k] = tok
            self.readers[k] = {}
        return tok

    def wait_all(self, eng, keys):
        for k in keys:
            t = self.lastw.get(k)
            if t is not None:
                self._wait(eng, t)

    def barrier(self):
        toks = []
        for e in ENGS:
            if self.esem[e] is not None and self.cnt[e] > 0:
                toks.append((self.esem[e], self.cnt[e], e))
            for slot in range(DK):
                sem = self.dring[e][slot]
                if sem is not None and self.duse[e][slot] > 0:
                    toks.append((sem, 16 * self.duse[e][slot], 'dma'))
        for e in ENGS:
            for t in toks:
                self._wait(e, t)

    def emit(self):
        nc = self.nc
        with nc.Block() as block:
            @block.tensor
            def _(e):
                for f in self.rec['tensor']:
                    f(e)

            @block.vector
            def _(e):
                for f in self.rec['vector']:
                    f(e)

            @block.scalar
            def _(e):
                for f in self.rec['scalar']:
                    f(e)

            @block.gpsimd
            def _(e):
                for f in self.rec['gpsimd']:
                    f(e)

            @block.sync
            def _(e):
                for f in self.rec['sync']:
                    f(e)


class Ctx:
    pass


def build(S, L, dbg=False, phases=('B', 'C', 'A', 'D', 'E'), rg=None):
    NT = S // 128
    NG = S // 512
    CAP = 2 * S // NE
    nc = bass.Bass("TRN2", target_bir_lowering=False)
    stack = ExitStack()
    P = Prog(nc, stack)
    c = Ctx()
    c.nc, c.P, c.S, c.L, c.NT, c.NG, c.CAP = nc, P, S, L, NT, NG, CAP

    def din(name, shape, dt=F32):
        return nc.dram_tensor(name, list(shape), dt, kind="ExternalInput").ap()

    c.x_in = din('x', [S, D])
    c.mem = din('mem', [MEM, D])
    c.out = nc.dram_tensor('out', [S, D], F32, kind="ExternalOutput").ap()
    c.final_g = din('final_norm_g', [1, D])

    def sb(name, shape, dt=F32):
        return nc.alloc_sbuf_tensor(name, list(shape), dt)

    def ps(name, shape, dt=F32):
        return nc.alloc_psum_tensor(name, list(shape), dt)

    c.sb, c.ps = sb, ps

    def dma(out, in_, r, w, q='sync', **kw):
        return P.op(q, lambda e: e.dma_start(out=out, in_=in_, **kw), r, w, dma=True)

    def act(out, in_, func, r, w, **kw):
        return P.op('scalar', lambda e: e.activation(out=out, in_=in_, func=func, **kw), r, w)

    def mm(out, lhsT, rhs, start, stop, r, w):
        return P.op('tensor', lambda e: e.matmul(out, lhsT, rhs, start=start, stop=stop), r, w)

    def tr(out, in_, ident, r, w):
        return P.op('tensor', lambda e: e.transpose(out, in_, ident), r, w)

    def V(fn, r, w, eng='vector'):
        return P.op(eng, fn, r, w)

    c.dma, c.act, c.mm, c.tr, c.V = dma, act, mm, tr, V

    def rstd_from_ssq(out, ssq, n, r, w, eng='vector'):
        V(lambda e: e.tensor_scalar(out=out, in0=ssq, scalar1=1.0 / n, scalar2=EPS,
                                    op0=ALU.mult, op1=ALU.add), r, w)
        P.op('scalar', lambda e: e.sqrt(out=out, in_=out), w, w)
        V(lambda e: e.reciprocal(out=out, in_=out), w, w)

    c.rstd_from_ssq = rstd_from_ssq

    NTK = NT * (2 if rg else 1)
    SK = NTK * 128
    CAP = 2 * SK // NE
    def dram(name, shape, dt):
        return nc.dram_tensor(name, list(shape), dt, kind=("ExternalOutput" if dbg else "Internal")).ap()

    X = dram('Xs', [S, D], F32)
    GMT = dram('GMT', [4, 128, S], BF16)
    QT = dram('QT', [4, 128, S], BF16)
    H3T = dram('H3T', [8, 128, S], BF16)
    H3R = dram('H3R', [S, D], BF16)
    AFFD = dram('AFFD', [S, 16], F32)
    LISTD = dram('LISTD', [16, CAP], I32)
    M8 = CAP // 128
    NA = CAP // 32
    if rg:
        KTD = nc.dram_tensor('KTD', [128, S], BF16, kind='Internal').ap()
        NR = len(rg[0])
        KTA = nc.dram_tensor('KTA', [NR * 128, S], BF16, kind='Internal').ap()
        VAD = nc.dram_tensor('VAD', [128, NT * 130], BF16, kind='Internal').ap()
        VAA = nc.dram_tensor('VAA', [NR * 128, NT * 130], BF16, kind='Internal').ap()
        AFD = nc.dram_tensor('AFD', [128, NT * 16], F32, kind='Internal').ap()
        AFA = nc.dram_tensor('AFA', [NR * 128, NT * 16], F32, kind='Internal').ap()
        sel_in = din('sel', [128, 2], I32)
        SEL = sb('sel_s', [128, 2], I32)
        dma(SEL[:], sel_in, [], ['sel'])

    def gather_rows(out_ap, src_ap, r_, r, w):
        return P.op('gpsimd', lambda e: e.indirect_dma_start(
            out=out_ap, out_offset=None, in_=src_ap,
            in_offset=bass.IndirectOffsetOnAxis(ap=SEL[:, r_:r_ + 1], axis=0)), list(r) + ['sel'], w, dma=True)

    def allgather(out_ap, in_ap, r, w):
        return P.op('gpsimd', lambda e: e.collective_compute('AllGather', op=ALU.bypass, replica_groups=rg,
                                                             ins=[in_ap], outs=[out_ap]), r, w, dma=True)
    W = {}
    for nm, shp in [('mix_norm_g', [L, D]), ('w_in', [L, D, 1792]), ('gm_v_norm_g', [L, 512]),
                    ('gm_w_s', [L, 4, 128, 128]), ('gm_b_s', [L, 4, 128]), ('q_norm_g', [L, 64]),
                    ('k_norm_g', [L, 64]), ('branch_norm_g', [L, 2, 512]), ('w_out', [L, D, D]),
                    ('xattn_norm_g', [L, D]), ('mem_norm_g', [1, D]), ('xattn_w_q', [L, D, D]),
                    ('xattn_w_kv', [L, D, 2 * D]), ('xattn_w_o', [L, D, D]), ('ffn_norm_g', [L, D]),
                    ('w_router', [L, D, NE]), ('w_gate', [L, NE, D, D]), ('w_up', [L, NE, D, D]),
                    ('w_down', [L, NE, D, D]), ('rope', [S, 128]), ('identf', [128, 128])]:
        W[nm] = din(nm, shp)
    W['identb'] = din('identb', [128, 128], BF16)
    W['triu'] = din('triu', [128, 128], BF16)
    W['iota32'] = din('iota32', [128, 32])
    W['pidx'] = din('pidx', [128, 1])

    F4 = [sb(f'F4{i}', [128, 1024]) for i in range(4)]
    F2 = [sb(f'F2{i}', [128, 512]) for i in range(5)]
    H8 = [sb(f'H8{i}', [128, 4096], BF16) for i in range(3)]
    H4 = [sb(f'H4{i}', [128, 2048], BF16) for i in range(2)]
    H2 = [sb(f'H2{i}', [128, 1024], BF16) for i in range(4)]
    WBt = sb('WB', [128, 4 * 8192], BF16)
    STG = [sb(f'STG{i}', [128, 2048]) for i in range(2)]
    RES = sb('RES', [128, 8704])
    SM = sb('SM', [128, 256])
    identb = sb('identb_s', [128, 128], BF16)
    identf = sb('identf_s', [128, 128])
    onesf = sb('onesf', [128, 128])
    onesb = sb('onesb', [128, 128], BF16)
    GV = sb('gvbc', [128, 512])
    GQ = sb('gqbc', [128, 64])
    GK = sb('gkbc', [128, 64])
    CV = sb('colv', [128, 64])
    ROPE = [sb(f'rope{i}', [128, 128]) for i in range(2)]
    PBW = [ps(f'PBW{i}', [128, 1024]) for i in range(4)]
    PB = [PBW[i // 2][:, (i % 2) * 512:(i % 2 + 1) * 512] for i in range(8)]

    def WB(k):
        return WBt[:, k * 8192:(k + 1) * 8192]

    KT = RES[:, 0:SK // 2].bitcast(BF16)
    VAUG = RES[:, SK // 2:SK // 2 + NTK * 65].bitcast(BF16).rearrange("p (t h d) -> p t h d", t=NTK, h=2)
    AFF = RES[:, 0:NT * 16].rearrange("p (t e) -> p t e", e=16)
    MG = RES[:, NT * 16:2 * NT * 16].rearrange("p (t e) -> p t e", e=16)
    AFFA = RES[:, 2 * NT * 16:2 * NT * 16 + NTK * 16].rearrange("p (t e) -> p t e", e=16) if rg else AFF
    RSTDGM = SM[:, 0:NT]

    dma(identb[:], W['identb'], [], ['identb'])
    dma(identf[:], W['identf'], [], ['identf'])
    triu = sb('triu_s', [128, 128], BF16)
    IOTA = sb('iota_s', [128, 32])
    PIDX = sb('pidx_s', [128, 1])
    IDXt = sb('idx_s', [128, 128], I32)
    GAt = sb('ga_s', [128, 128])
    dma(triu[:], W['triu'], [], ['triu'])
    dma(IOTA[:], W['iota32'], [], ['iota'])
    dma(PIDX[:], W['pidx'], [], ['pidx'])
    V(lambda e: e.memset(onesf[:], 1.0), [], ['onesf'], 'gpsimd')
    V(lambda e: e.memset(onesb[:], 1.0), [], ['onesb'], 'gpsimd')
    for i in range(0, S, 1024):
        j = min(S, i + 1024)
        dma(X[i:j, :], c.x_in[i:j, :], [], ['X'])

    stg_n = [0]

    def load_w(dst3, dkeys, src2, KC, N, rowscale=None, mul=1.0, part=128, rkeys=(), eng='gpsimd'):
        cols = max(1, 2048 // KC)
        for n0 in range(0, N, cols):
            n1 = min(N, n0 + cols)
            k = stg_n[0] % 2
            stg_n[0] += 1
            stv = STG[k][0:part, 0:KC * (n1 - n0)].rearrange("p (c n) -> p c n", c=KC)
            dma(stv, src2[:, n0:n1].rearrange("(c p) n -> p c n", p=part), [], [f'STG{k}'])
            if rowscale is None and eng == 'scalar':
                act(dst3[:, :, n0:n1], stv, AF.Copy, [f'STG{k}'], dkeys)
            elif rowscale is None:
                V(lambda e, stv=stv, n0=n0, n1=n1: e.tensor_copy(out=dst3[:, :, n0:n1], in_=stv),
                  [f'STG{k}'], dkeys, 'gpsimd')
            else:
                for cc in range(KC):
                    V(lambda e, stv=stv, n0=n0, n1=n1, cc=cc: e.tensor_scalar(
                        out=dst3[:, cc, n0:n1], in0=stv[:, cc, :], scalar1=rowscale[:, cc:cc + 1],
                        scalar2=mul, op0=ALU.mult, op1=ALU.mult),
                      [f'STG{k}'] + list(rkeys), dkeys, 'gpsimd')

    def colvec(dst, src1d, p, key):
        P.op('sync', lambda e: e.dma_start(out=dst, in_=src1d.rearrange("(c p) -> p c", p=p),
                                           allow_slow_non_contiguous=True), [], [key], dma=True)

    tb_n = [0]

    def norm_T(xt, xkey, dst3, dkeys, hbuf, hkey, gain_bc=None, gkey=None, tbanks=(0, 1)):
        ssq = SM[:, 128:129]
        rs = SM[:, 129:130]
        act(hbuf[:, 0:1024], xt, AF.Square, [xkey], [hkey, 'ssq'], accum_out=ssq)
        rstd_from_ssq(rs, ssq, D, ['ssq'], ['rs'])
        act(hbuf[:, 0:1024], xt, AF.Copy, [xkey, 'rs'], [hkey], scale=rs)
        if gain_bc is not None:
            V(lambda e: e.tensor_tensor(out=hbuf[:, 0:1024], in0=hbuf[:, 0:1024], in1=gain_bc, op=ALU.mult),
              [hkey, gkey], [hkey])
        bk = tbanks[tb_n[0] % len(tbanks)]
        tb_n[0] += 1
        pt = PB[bk][:].bitcast(BF16).rearrange("p (c n) -> p c n", c=8)
        for cc in range(8):
            tr(pt[:, cc, :], hbuf[:, cc * 128:(cc + 1) * 128], identb[:], [hkey, 'identb'], [f'PB{bk}'])
        V(lambda e: e.tensor_copy(out=dst3, in_=pt), [f'PB{bk}'], dkeys)

    def phase_B(l):
        colvec(CV[:, 0:8], W['mix_norm_g'][l], 128, 'cv_mix')
        WIN = WBt[:, 2 * 8192:2 * 8192 + 8 * 1792].rearrange("p (c n) -> p c n", c=8)
        load_w(WIN, ['WB2', 'WB3'], W['w_in'][l], 8, 1792, rowscale=CV[:, 0:8], rkeys=['cv_mix'])
        dma(GV[:], W['gm_v_norm_g'][l:l + 1, :].partition_broadcast(128), [], ['gvbc'])
        dma(GQ[:], W['q_norm_g'][l:l + 1, :].partition_broadcast(128), [], ['gqbc'])
        dma(GK[:], W['k_norm_g'][l:l + 1, :].partition_broadcast(128), [], ['gkbc'])
        V(lambda e: e.tensor_scalar(out=GQ[:], in0=GQ[:], scalar1=0.125, scalar2=None, op0=ALU.mult),
          ['gqbc'], ['gqbc'])
        P.op('sync', lambda e: e.dma_start(out=CV[:, 8:12], in_=W['gm_b_s'][l].rearrange("g i -> i g"),
                                           allow_slow_non_contiguous=True), [], ['cv_bs'], dma=True)
        WST = H8[2][:, 0:512].rearrange("p (g i) -> p g i", g=4)
        for g in range(4):
            dma(F2[4][:, g * 128:(g + 1) * 128], W['gm_w_s'][l, g], [], ['F24'])
        V(lambda e: e.tensor_copy(out=H8[2][:, 512:1024], in_=F2[4][:]), ['F24'], ['H82s'])
        ptw = PB[0][:].bitcast(BF16)
        for g in range(4):
            tr(ptw[:, g * 128:(g + 1) * 128], H8[2][:, 512 + g * 128:512 + (g + 1) * 128], identb[:], ['H82s', 'identb'], ['PB0'])
        V(lambda e: e.tensor_copy(out=H8[2][:, 0:512], in_=ptw[:, 0:512]), ['PB0'], ['H82w'])
        V(lambda e: e.memset(VAUG[:, :, :, 64:65], 1.0), [], ['VAUG'], 'gpsimd')

        for i in range(NT):
            b = i % 2
            xt, xk = F4[b], f'F4{b}'
            dma(xt[:], X[i * 128:(i + 1) * 128, :], ['X'], [xk])
            dma(ROPE[b][:], W['rope'][i * 128:(i + 1) * 128, :], [], [f'rope{b}'])
            hT = H2[b][:].rearrange("p (c n) -> p c n", c=8)
            norm_T(xt[:], xk, hT, [f'H2{b}'], H4[0], 'H40')
            for gi, (c0, c1, bk) in enumerate([(0, 512, 2), (512, 1024, 3), (1024, 1536, 4), (1536, 1792, 5)]):
                for cc in range(8):
                    mm(PB[bk][:, 0:c1 - c0], hT[:, cc, :], WIN[:, cc, c0:c1], cc == 0, cc == 7,
                       [f'H2{b}', 'WB2', 'WB3'], [f'PB{bk}'])
            GU, GVt, GM = F2[0], F2[1], F2[2]
            act(GU[:], PB[2][:], GELU, ['PB2'], ['F20'])
            act(GVt[:], PB[3][:], GELU, ['PB3'], ['F21'])
            act(F2[3][:], GVt[:], AF.Square, ['F21'], ['F23', 'ssqv'], accum_out=SM[:, 130:131])
            rstd_from_ssq(SM[:, 131:132], SM[:, 130:131], 512, ['ssqv'], ['rsv'])
            VN = H4[1][:, 0:512]
            V(lambda e: e.scalar_tensor_tensor(out=VN, in0=GVt[:], scalar=SM[:, 131:132], in1=GV[:],
                                               op0=ALU.mult, op1=ALU.mult), ['F21', 'rsv', 'gvbc'], ['H41'])
            for g in range(4):
                mm(PB[6][:, g * 128:(g + 1) * 128], WST[:, g, :], VN[:, g * 128:(g + 1) * 128], True, True,
                   ['H82w', 'H41'], ['PB6'])
            for g in range(4):
                V(lambda e, g=g: e.scalar_tensor_tensor(
                    out=GM[:, g * 128:(g + 1) * 128], in0=PB[6][:, g * 128:(g + 1) * 128], scalar=CV[:, 8 + g:9 + g],
                    in1=GU[:, g * 128:(g + 1) * 128], op0=ALU.add, op1=ALU.mult), ['PB6', 'cv_bs', 'F20'], ['F22'])
            act(F2[3][:], GM[:], AF.Square, ['F22'], ['F23', 'ssqg'], accum_out=SM[:, 132:133])
            rstd_from_ssq(RSTDGM[:, i:i + 1], SM[:, 132:133], 512, ['ssqg'], ['rstdgm'])
            GMB = H4[1][:, 512:1024]
            act(GMB, GM[:], AF.Copy, ['F22'], ['H41b'])
            QN = F2[3]
            act(F2[4][:], PB[4][:], AF.Square, ['PB4'], ['F24'])
            V(lambda e: e.tensor_reduce(out=SM[:, 136:144], in_=F2[4][:].rearrange("p (h d) -> p h d", h=8),
                                        axis=AX.X, op=ALU.add), ['F24'], ['ssqq'])
            rstd_from_ssq(SM[:, 136:144], SM[:, 136:144], 64, ['ssqq'], ['ssqq'])
            V(lambda e: e.tensor_tensor(out=QN[:].rearrange("p (h d) -> p h d", h=8),
                                        in0=PB[4][:].rearrange("p (h d) -> p h d", h=8),
                                        in1=SM[:, 136:144].unsqueeze(2).to_broadcast([128, 8, 64]), op=ALU.mult),
              ['PB4', 'ssqq'], ['F23'])
            V(lambda e: e.tensor_tensor(out=QN[:].rearrange("p (h d) -> p h d", h=8),
                                        in0=QN[:].rearrange("p (h d) -> p h d", h=8),
                                        in1=GQ[:].unsqueeze(1).to_broadcast([128, 8, 64]), op=ALU.mult),
              ['F23', 'gqbc'], ['F23'])
            rp = ROPE[b]
            rk = f'rope{b}'

            def rope(src, nh, dst_view, skey, dkey, TT, tkey, rp=rp, rk=rk):
                s4 = src.rearrange("p (h i two) -> p h i two", h=nh, two=2)
                t4 = TT.rearrange("p (h i two) -> p h i two", h=nh, two=2)
                V(lambda e: e.tensor_tensor(out=t4[:, :, :, 0], in0=s4[:, :, :, 1],
                                            in1=rp[:, 64:96].unsqueeze(1).to_broadcast([128, nh, 32]), op=ALU.mult),
                  [skey, rk], [tkey])
                V(lambda e: e.tensor_tensor(out=t4[:, :, :, 1], in0=s4[:, :, :, 0],
                                            in1=rp[:, 96:128].unsqueeze(1).to_broadcast([128, nh, 32]), op=ALU.mult),
                  [skey, rk], [tkey])
                s3 = src.rearrange("p (h d) -> p h d", h=nh)
                V(lambda e: e.tensor_tensor(out=s3, in0=s3, in1=rp[:, 0:64].unsqueeze(1).to_broadcast([128, nh, 64]),
                                            op=ALU.mult), [skey, rk], [skey])
                if nh == 8:
                    a4 = src.rearrange("p (hh j d) -> p hh j d", hh=2, j=4)
                    b4 = TT.rearrange("p (hh j d) -> p hh j d", hh=2, j=4)
                else:
                    a4 = s3
                    b4 = TT.rearrange("p (h d) -> p h d", h=nh)
                V(lambda e: e.tensor_tensor(out=dst_view, in0=a4, in1=b4, op=ALU.add), [skey, tkey], [dkey])

            QR = H4[0][:, 1024:1536]
            rope(QN[:], 8, QR.rearrange("p (j hh d) -> p hh j d", j=4, hh=2), 'F23', 'H40q', F2[4][:], 'F24')
            KN = F2[4][:, 0:128]
            act(F2[3][:, 0:128], PB[5][:, 0:128], AF.Square, ['PB5', 'H40q'], ['F23'])
            V(lambda e: e.tensor_reduce(out=SM[:, 144:146], in_=F2[3][:, 0:128].rearrange("p (h d) -> p h d", h=2),
                                        axis=AX.X, op=ALU.add), ['F23'], ['ssqk'])
            rstd_from_ssq(SM[:, 144:146], SM[:, 144:146], 64, ['ssqk'], ['ssqk'])
            V(lambda e: e.tensor_tensor(out=KN.rearrange("p (h d) -> p h d", h=2),
                                        in0=PB[5][:, 0:128].rearrange("p (h d) -> p h d", h=2),
                                        in1=SM[:, 144:146].unsqueeze(2).to_broadcast([128, 2, 64]), op=ALU.mult),
              ['PB5', 'ssqk'], ['F24'])
            V(lambda e: e.tensor_tensor(out=KN.rearrange("p (h d) -> p h d", h=2),
                                        in0=KN.rearrange("p (h d) -> p h d", h=2),
                                        in1=GK[:].unsqueeze(1).to_broadcast([128, 2, 64]), op=ALU.mult),
              ['F24', 'gkbc'], ['F24'])
            KR = H4[0][:, 1536:1664]
            rope(KN, 2, KR.rearrange("p (h d) -> p h d", h=2), 'F24', 'H40k', F2[3][:, 128:256], 'F23')
            V(lambda e, i=i: e.tensor_copy(out=VAUG[:, i, :, 0:64],
                                           in_=PB[5][:, 128:256].rearrange("p (h d) -> p h d", h=2)),
              ['PB5'], ['VAUG'])
            pt = PB[7][:].bitcast(BF16)
            for g in range(4):
                tr(pt[:, g * 128:(g + 1) * 128], GMB[:, g * 128:(g + 1) * 128], identb[:], ['H41b', 'identb'], ['PB7'])
            for j in range(4):
                tr(pt[:, 512 + j * 128:512 + (j + 1) * 128], QR[:, j * 128:(j + 1) * 128], identb[:],
                   ['H40q', 'identb'], ['PB7'])
            TS = H4[b][:, 0:0]
            OUTS = H2[2 + b]
            ok = f'H2{2 + b}'
            if i == 0:
                pass
            V(lambda e, OUTS=OUTS: e.tensor_copy(out=OUTS[:], in_=pt), ['PB7'], [ok])
            dma(GMT[:, :, i * 128:(i + 1) * 128].rearrange("c p n -> p c n"),
                OUTS[:, 0:512].rearrange("p (c n) -> p c n", c=4), [ok], ['GMT'])
            dma(QT[:, :, i * 128:(i + 1) * 128].rearrange("c p n -> p c n"),
                OUTS[:, 512:1024].rearrange("p (c n) -> p c n", c=4), [ok], ['QT'])
            bk = 'PB6'
            ptk = PB[6][:].bitcast(BF16)
            tr(ptk[:, 0:128], KR, identb[:], ['H40k', 'identb'], ['PB6'])
            V(lambda e, i=i: e.tensor_copy(out=KT[:, i * 128:(i + 1) * 128], in_=ptk[:, 0:128]), ['PB6'], ['KT'])

    def exchange_kv():
        if not rg:
            return
        dma(KTD, KT[:, 0:S], ['KT'], ['KTD'])
        dma(VAD, RES[:, SK // 2:SK // 2 + NT * 65].bitcast(BF16), ['VAUG'], ['VAD'])
        allgather(KTA, KTD, ['KTD'], ['KTA'])
        allgather(VAA, VAD, ['VAD'], ['VAA'])
        for r_ in range(2):
            gather_rows(KT[:, r_ * S:(r_ + 1) * S], KTA, r_, ['KTA'], ['KT'])
            gather_rows(RES[:, SK // 2 + r_ * NT * 65:SK // 2 + (r_ + 1) * NT * 65].bitcast(BF16), VAA, r_,
                        ['VAA'], ['VAUG'])

    def phase_C(l):
        exchange_kv()
        colvec(CV[:, 16:20], W['branch_norm_g'][l, 0], 128, 'cv_b0')
        colvec(CV[0:64, 20:28], W['branch_norm_g'][l, 1], 64, 'cv_b1')
        WO0 = WB(0)[:, 0:4096].rearrange("p (c n) -> p c n", c=4)
        WO1 = WB(1)[0:64, :].rearrange("p (c n) -> p c n", c=8)
        load_w(WO0, ['WB0'], W['w_out'][l, 0:512, :], 4, 1024, rowscale=CV[:, 16:20], rkeys=['cv_b0'])
        load_w(WO1, ['WB1'], W['w_out'][l, 512:1024, :], 8, 1024, rowscale=CV[0:64, 20:28], rkeys=['cv_b1'], part=64)
        NKP = NTK // 2
        GS = min(512, S)
        NGG = S // GS
        for g in range(NGG):
            t0 = g * GS
            qTg = H4[g % 2][:, 0:4 * GS].rearrange("p (j n) -> p j n", j=4)
            qk = f'H4{g % 2}'
            dma(qTg, QT[:, :, t0:t0 + GS].rearrange("c p n -> p c n"), ['QT'], [qk])
            ATT = H8[0][0:64, 0:8 * GS].rearrange("p (h n) -> p h n", h=8)
            SQACC = F2[4][0:64, 0:GS]
            steps = [(h, kp) for h in range(8) for kp in range(NKP)]

            def emit_qk(h, kp):
                j, hh = h % 4, h // 4
                sbk = (kp % 2) * 2
                for k2 in range(2):
                    kt = kp * 2 + k2
                    mm(PB[sbk + k2][:, 0:GS], KT[hh * 64:(hh + 1) * 64, kt * 128:(kt + 1) * 128],
                       qTg[hh * 64:(hh + 1) * 64, j, :], True, True, ['KT', qk], [f'PB{sbk + k2}'])

            def emit_exp_pv(h, kp):
                hh = h // 4
                ob = 4 + (h % 2)
                OT = PB[ob]
                sbk = (kp % 2) * 2
                PT = H2[kp % 3]
                pk = f'H2{kp % 3}'
                if GS == 512:
                    act(PT[:, 0:1024], PBW[kp % 2][:, 0:1024], AF.Exp, [f'PB{sbk}', f'PB{sbk + 1}'], [pk])
                else:
                    for k2 in range(2):
                        act(PT[:, k2 * 512:k2 * 512 + GS], PB[sbk + k2][:, 0:GS], AF.Exp, [f'PB{sbk + k2}'], [pk])
                for k2 in range(2):
                    kt = kp * 2 + k2
                    mm(OT[0:65, 0:GS], VAUG[:, kt, hh, :], PT[:, k2 * 512:k2 * 512 + GS],
                       kt == 0, kt == NTK - 1, ['VAUG', pk], [f'PB{ob}'])

            def finalize(h):
                ob = 4 + (h % 2)
                OT = PB[ob]
                SR = F2[3]
                V(lambda e, OT=OT: e.tensor_copy(out=SR[64:65, 0:GS], in_=OT[64:65, 0:GS]), [f'PB{ob}'], ['F23'])
                mm(PB[6][0:64, 0:GS], onesf[64:65, 0:64], SR[64:65, 0:GS], True, True, ['onesf', 'F23'], ['PB6'])
                Rr = F2[2]
                V(lambda e: e.reciprocal(out=Rr[0:64, 0:GS], in_=PB[6][0:64, 0:GS]), ['PB6'], ['F22'])
                V(lambda e, OT=OT, h=h: e.tensor_tensor(out=ATT[:, h, :], in0=OT[0:64, 0:GS], in1=Rr[0:64, 0:GS],
                                                        op=ALU.mult), [f'PB{ob}', 'F22'], ['H80'])
                if h == 0:
                    V(lambda e, h=h: e.tensor_tensor(out=SQACC, in0=ATT[:, h, :], in1=ATT[:, h, :], op=ALU.mult),
                      ['H80'], ['F24'], 'gpsimd')
                else:
                    V(lambda e, h=h: e.tensor_tensor(out=F2[1][0:64, 0:GS], in0=ATT[:, h, :], in1=ATT[:, h, :],
                                                     op=ALU.mult), ['H80'], ['F21'], 'gpsimd')
                    V(lambda e: e.tensor_tensor(out=SQACC, in0=SQACC, in1=F2[1][0:64, 0:GS], op=ALU.add),
                      ['F21', 'F24'], ['F24'], 'gpsimd')

            emit_qk(*steps[0])
            pending = None
            for si, (h, kp) in enumerate(steps):
                if si + 1 < len(steps):
                    emit_qk(*steps[si + 1])
                emit_exp_pv(h, kp)
                if pending is not None and kp == 0:
                    finalize(pending)
                    pending = None
                if kp == NKP - 1:
                    pending = h
            finalize(pending)
            mm(PB[6][0:1, 0:GS], onesf[0:64, 0:1], SQACC, True, True, ['onesf', 'F24'], ['PB6'])
            V(lambda e: e.tensor_copy(out=F2[3][0:1, 0:GS], in_=PB[6][0:1, 0:GS]), ['PB6'], ['F23'])
            for tt in range(GS // 128):
                mm(PB[6][:, tt:tt + 1], F2[3][0:1, tt * 128:(tt + 1) * 128], onesf[0:1, 0:1], True, True,
                   ['F23', 'onesf'], ['PB6'])
            rstd_from_ssq(SM[:, 148:148 + GS // 128], PB[6][:, 0:GS // 128], 512, ['PB6'], ['rsat'])
            gmTg = H8[1][:, 0:4 * GS].rearrange("p (c n) -> p c n", c=4)
            dma(gmTg, GMT[:, :, t0:t0 + GS].rearrange("c p n -> p c n"), ['GMT'], ['H81'])
            for tt in range(GS // 128):
                i = g * (GS // 128) + tt
                b = tt % 2
                xt, xk = F4[b], f'F4{b}'
                dma(xt[:], X[i * 128:(i + 1) * 128, :], ['X'], [xk])
                for half in range(2):
                    hs = slice(half * 512, (half + 1) * 512)
                    for cc in range(4):
                        mm(PB[7][:], gmTg[:, cc, tt * 128:(tt + 1) * 128], WO0[:, cc, hs], cc == 0, cc == 3,
                           ['H81', 'WB0'], ['PB7'])
                    V(lambda e, xt=xt, hs=hs, i=i: e.scalar_tensor_tensor(
                        out=xt[:, hs], in0=PB[7][:], scalar=RSTDGM[:, i:i + 1], in1=xt[:, hs],
                        op0=ALU.mult, op1=ALU.add), ['PB7', 'rstdgm', xk], [xk])
                    for h in range(8):
                        mm(PB[7][:], ATT[:, h, tt * 128:(tt + 1) * 128], WO1[:, h, hs], h == 0, h == 7,
                           ['H80', 'WB1'], ['PB7'])
                    V(lambda e, xt=xt, hs=hs, tt=tt: e.scalar_tensor_tensor(
                        out=xt[:, hs], in0=PB[7][:], scalar=SM[:, 148 + tt:149 + tt], in1=xt[:, hs],
                        op0=ALU.mult, op1=ALU.add), ['PB7', 'rsat', xk], [xk])
                dma(X[i * 128:(i + 1) * 128, :], xt[:], [xk], ['X'])

    MNTt = sb('MNT', [128, 2048], BF16)
    MNT = MNTt[:].rearrange("p (c n) -> p c n", c=8)
    KXt = sb('KX', [128, 2048], BF16)
    KX = KXt[:].rearrange("p (c n) -> p c n", c=8)
    VXt = sb('VX', [128, 2048], BF16)
    VX = VXt[:].rearrange("p (m n) -> p m n", m=2)
    WRf = sb('WRf', [128, 128])

    def phase_A0():
        dma(F4[2][:], W['mem_norm_g'].partition_broadcast(128), [], ['F42'])
        for mt in range(2):
            dma(F4[mt][:], c.mem[mt * 128:(mt + 1) * 128, :], [], [f'F4{mt}'])
            norm_T(F4[mt][:], f'F4{mt}', MNT[:, :, mt * 128:(mt + 1) * 128], ['MNT'], H4[0], 'H40',
                   gain_bc=F4[2][:], gkey='F42')

    def phase_A(l):
        WK = WB(2).rearrange("p (c n) -> p c n", c=8)
        WV = WB(3).rearrange("p (c n) -> p c n", c=8)
        load_w(WK, ['WB2'], W['xattn_w_kv'][l, :, 0:1024], 8, 1024)
        load_w(WV, ['WB3'], W['xattn_w_kv'][l, :, 1024:2048], 8, 1024)
        for dc in range(8):
            bk = dc % 2
            for cc in range(8):
                mm(PB[bk][:, 0:256], WK[:, cc, dc * 128:(dc + 1) * 128], MNT[:, cc, :], cc == 0, cc == 7,
                   ['WB2', 'MNT'], [f'PB{bk}'])
            V(lambda e, dc=dc, bk=bk: e.tensor_copy(out=KX[:, dc, :], in_=PB[bk][:, 0:256]), [f'PB{bk}'], ['KX'])
        for mt in range(2):
            for half in range(2):
                bk = 2 + half
                for cc in range(8):
                    mm(PB[bk][:], MNT[:, cc, mt * 128:(mt + 1) * 128], WV[:, cc, half * 512:(half + 1) * 512],
                       cc == 0, cc == 7, ['WB3', 'MNT'], [f'PB{bk}'])
                V(lambda e, mt=mt, half=half, bk=bk: e.tensor_copy(out=VX[:, mt, half * 512:(half + 1) * 512],
                                                                   in_=PB[bk][:]), [f'PB{bk}'], ['VX'])

    def phase_D(l):
        colvec(CV[:, 32:40], W['xattn_norm_g'][l], 128, 'cv_x')
        WQ = WB(0).rearrange("p (c n) -> p c n", c=8)
        WOX = WB(1).rearrange("p (c n) -> p c n", c=8)
        load_w(WQ, ['WB0'], W['xattn_w_q'][l], 8, 1024, rowscale=CV[:, 32:40], mul=1.0 / 16.0, rkeys=['cv_x'])
        load_w(WOX, ['WB1'], W['xattn_w_o'][l], 8, 1024)
        GS = min(512, S)
        for g in range(S // GS):
            t0 = g * GS
            H2Tg = H8[1][:, 0:8 * GS].rearrange("p (c n) -> p c n", c=8)
            for tt in range(GS // 128):
                i = g * (GS // 128) + tt
                b = tt % 2
                dma(F4[b][:], X[i * 128:(i + 1) * 128, :], ['X'], [f'F4{b}'])
                norm_T(F4[b][:], f'F4{b}', H2Tg[:, :, tt * 128:(tt + 1) * 128], ['H81'], H4[0], 'H40')
            QX = H8[2][:, 0:8 * GS].rearrange("p (c n) -> p c n", c=8)
            for dc in range(8):
                bk = 2 + dc % 2
                for cc in range(8):
                    mm(PB[bk][:, 0:GS], WQ[:, cc, dc * 128:(dc + 1) * 128], H2Tg[:, cc, :], cc == 0, cc == 7,
                       ['WB0', 'H81'], [f'PB{bk}'])
                act(QX[:, dc, :], PB[bk][:, 0:GS], AF.Copy, [f'PB{bk}'], ['H82'])
            OXT = H8[0][:, 0:8 * GS].rearrange("p (c n) -> p c n", c=8)
            for h in range(4):
                PX = H2[2 + h % 2]
                pk = f'H2{2 + h % 2}'
                for mt in range(2):
                    for dd in range(2):
                        mm(PB[4 + mt][:, 0:GS], KX[:, 2 * h + dd, mt * 128:(mt + 1) * 128], QX[:, 2 * h + dd, :],
                           dd == 0, dd == 1, ['KX', 'H82'], [f'PB{4 + mt}'])
                    if GS != 512:
                        act(PX[:, mt * 512:mt * 512 + GS], PB[4 + mt][:, 0:GS], AF.Exp, [f'PB{4 + mt}'], [pk])
                if GS == 512:
                    act(PX[:, 0:1024], PBW[2][:, 0:1024], AF.Exp, ['PB4', 'PB5'], [pk])
                for mt in range(2):
                    mm(PB[6][:, 0:GS], onesb[:], PX[:, mt * 512:mt * 512 + GS], mt == 0, mt == 1,
                       ['onesb', pk], ['PB6'])
                V(lambda e: e.reciprocal(out=F2[2][:, 0:GS], in_=PB[6][:, 0:GS]), ['PB6'], ['F22'])
                for dd in range(2):
                    for mt in range(2):
                        mm(PB[7][:, 0:GS], VX[:, mt, (2 * h + dd) * 128:(2 * h + dd + 1) * 128],
                           PX[:, mt * 512:mt * 512 + GS], mt == 0, mt == 1, ['VX', pk], ['PB7'])
                    V(lambda e, h=h, dd=dd: e.tensor_tensor(out=OXT[:, 2 * h + dd, :], in0=PB[7][:, 0:GS],
                                                            in1=F2[2][:, 0:GS], op=ALU.mult),
                      ['PB7', 'F22'], ['H80'])
            for tt in range(GS // 128):
                i = g * (GS // 128) + tt
                b = tt % 2
                xt, xk = F4[2 + b], f'F4{2 + b}'
                dma(xt[:], X[i * 128:(i + 1) * 128, :], ['X'], [xk])
                for half in range(2):
                    hs = slice(half * 512, (half + 1) * 512)
                    bk = 2 + half
                    for cc in range(8):
                        mm(PB[bk][:], OXT[:, cc, tt * 128:(tt + 1) * 128], WOX[:, cc, hs], cc == 0, cc == 7,
                           ['H80', 'WB1'], [f'PB{bk}'])
                    V(lambda e, xt=xt, hs=hs, bk=bk: e.tensor_tensor(out=xt[:, hs], in0=PB[bk][:], in1=xt[:, hs],
                                                                     op=ALU.add), [f'PB{bk}', xk], [xk])
                dma(X[i * 128:(i + 1) * 128, :], xt[:], [xk], ['X'])

    def phase_E(l):
        colvec(CV[:, 40:48], W['ffn_norm_g'][l], 128, 'cv_f')
        WR3 = WRf[:].rearrange("p (c e) -> p c e", c=8)
        dma(WR3, W['w_router'][l].rearrange("(c p) e -> p c e", p=128), [], ['WRf'])
        for cc in range(8):
            V(lambda e, cc=cc: e.tensor_scalar(out=WR3[:, cc, :], in0=WR3[:, cc, :], scalar1=CV[:, 40 + cc:41 + cc],
                                               scalar2=None, op0=ALU.mult), ['WRf', 'cv_f'], ['WRf'])
        for i in range(NT):
            b = i % 2
            xt, xk = F4[b], f'F4{b}'
            dma(xt[:], X[i * 128:(i + 1) * 128, :], ['X'], [xk])
            act(F4[2][:], xt[:], AF.Square, [xk], ['F42', 'ssq'], accum_out=SM[:, 128:129])
            rstd_from_ssq(SM[:, 129:130], SM[:, 128:129], D, ['ssq'], ['rs'])
            act(F4[2][:], xt[:], AF.Copy, [xk, 'rs'], ['F42'], scale=SM[:, 129:130])
            for cc in range(8):
                bk = cc // 4
                tr(PB[bk][:, (cc % 4) * 128:(cc % 4 + 1) * 128], F4[2][:, cc * 128:(cc + 1) * 128], identf[:],
                   ['F42', 'identf'], [f'PB{bk}'])
            V(lambda e: e.tensor_copy(out=F4[3][:, 0:512], in_=PB[0][:]), ['PB0'], ['F43'])
            V(lambda e: e.tensor_copy(out=F4[3][:, 512:1024], in_=PB[1][:]), ['PB1'], ['F43'])
            for cc in range(8):
                mm(PB[2][:, 0:16], F4[3][:, cc * 128:(cc + 1) * 128], WR3[:, cc, :], cc == 0, cc == 7,
                   ['F43', 'WRf'], ['PB2'])
            V(lambda e: e.reduce_max(out=SM[:, 150:151], in_=PB[2][:, 0:16], axis=AX.X), ['PB2'], ['smx'])
            V(lambda e: e.tensor_scalar(out=SM[:, 150:151], in0=SM[:, 150:151], scalar1=-1.0, scalar2=None,
                                        op0=ALU.mult), ['smx'], ['smx'])
            act(SM[:, 160:176], PB[2][:, 0:16], AF.Exp, ['PB2', 'smx'], ['sme', 'sms'], bias=SM[:, 150:151],
                accum_out=SM[:, 151:152])
            V(lambda e: e.reciprocal(out=SM[:, 151:152], in_=SM[:, 151:152]), ['sms'], ['sms'])
            V(lambda e, i=i: e.tensor_scalar(out=AFF[:, i, :], in0=SM[:, 160:176], scalar1=SM[:, 151:152],
                                             scalar2=None, op0=ALU.mult), ['sme', 'sms'], ['AFF'])
            hb = H2[b]
            V(lambda e, hb=hb: e.tensor_copy(out=hb[:], in_=F4[2][:]), ['F42'], [f'H2{b}'])
            dma(H3R[i * 128:(i + 1) * 128, :], hb[:], [f'H2{b}'], ['H3R'])
            dma(AFFD[i * 128:(i + 1) * 128, :], AFF[:, i, :], ['AFF'], ['AFFD'])
        if rg:
            dma(AFD, RES[:, 0:NT * 16], ['AFF'], ['AFD'])
            allgather(AFA, AFD, ['AFD'], ['AFA'])
            for r_ in range(2):
                gather_rows(RES[:, 2 * NT * 16 + r_ * NT * 16:2 * NT * 16 + (r_ + 1) * NT * 16], AFA, r_,
                            ['AFA'], ['AFFA'])
        akey = 'AFFA' if rg else 'AFF'
        LO, HI, MID = SM[:, 160:176], SM[:, 176:192], SM[:, 192:208]
        PART, CC, D1 = SM[:, 208:224], SM[:, 224:240], SM[:, 240:256]
        CMPA = F4[2][:, 0:NTK * 16].rearrange("p (t e) -> p t e", e=16)
        CMP = F4[2][:, 0:NT * 16].rearrange("p (t e) -> p t e", e=16)
        V(lambda e: e.memset(LO, 0.0), ['sme'], ['lo'])
        V(lambda e: e.memset(HI, 1.0), [], ['hi'])
        for it in range(32):
            V(lambda e: e.tensor_tensor(out=MID, in0=LO, in1=HI, op=ALU.add), ['lo', 'hi'], ['mid'])
            V(lambda e: e.tensor_scalar(out=MID, in0=MID, scalar1=0.5, scalar2=None, op0=ALU.mult), ['mid'], ['mid'])
            V(lambda e: e.tensor_tensor(out=CMPA, in0=AFFA, in1=MID.unsqueeze(1).to_broadcast([128, NTK, 16]),
                                        op=ALU.is_gt), [akey, 'mid'], ['F42'])
            V(lambda e: e.tensor_reduce(out=PART, in_=CMPA.rearrange("p t e -> p e t"), axis=AX.X, op=ALU.add),
              ['F42'], ['part'])
            mm(PB[3][:, 0:16], onesf[:], PART, True, True, ['onesf', 'part'], ['PB3'])
            V(lambda e: e.tensor_scalar(out=CC, in0=PB[3][:, 0:16], scalar1=float(CAP) - 0.5, scalar2=None,
                                        op0=ALU.is_ge), ['PB3'], ['cc'])
            V(lambda e: e.tensor_tensor(out=D1, in0=MID, in1=LO, op=ALU.subtract), ['mid', 'lo'], ['d1'])
            V(lambda e: e.tensor_tensor(out=D1, in0=D1, in1=CC, op=ALU.mult), ['d1', 'cc'], ['d1'])
            V(lambda e: e.tensor_tensor(out=LO, in0=LO, in1=D1, op=ALU.add), ['d1', 'lo'], ['lo'])
            V(lambda e: e.tensor_tensor(out=D1, in0=HI, in1=MID, op=ALU.subtract), ['mid', 'hi'], ['d1'])
            V(lambda e: e.tensor_tensor(out=D1, in0=D1, in1=CC, op=ALU.mult), ['d1', 'cc'], ['d1'])
            V(lambda e: e.tensor_tensor(out=HI, in0=MID, in1=D1, op=ALU.add), ['d1', 'mid'], ['hi'])
        V(lambda e: e.tensor_tensor(out=CMP, in0=AFF, in1=LO.unsqueeze(1).to_broadcast([128, NT, 16]),
                                    op=ALU.is_gt), ['AFF', 'lo'], ['F42'])
        V(lambda e: e.tensor_tensor(out=MG, in0=CMP, in1=AFF, op=ALU.mult), ['F42', 'AFF'], ['MG'])
        NTE = NT * 16
        Mflat = F4[2][:, 0:NTE]
        Mb = H2[0][:, 0:NTE]
        V(lambda e: e.tensor_copy(out=Mb, in_=Mflat), ['F42'], ['H20'])
        W1 = F4[0][:, 0:NTE]
        NRr = F4[1][:, 0:NTE]
        nhb = (NTE + 511) // 512
        for hb in range(nhb):
            c0, c1 = hb * 512, min(NTE, (hb + 1) * 512)
            mm(PB[hb][:, 0:c1 - c0], triu[:], Mb[:, c0:c1], True, True, ['triu', 'H20'], [f'PB{hb}'])
            mm(PB[2 + hb][:, 0:c1 - c0], onesb[:], Mb[:, c0:c1], True, True, ['onesb', 'H20'], [f'PB{2 + hb}'])
            V(lambda e, hb=hb, c0=c0, c1=c1: e.tensor_copy(out=W1[:, c0:c1], in_=PB[hb][:, 0:c1 - c0]),
              [f'PB{hb}'], ['F40'])
            V(lambda e, hb=hb, c0=c0, c1=c1: e.tensor_copy(out=NRr[:, c0:c1], in_=PB[2 + hb][:, 0:c1 - c0]),
              [f'PB{2 + hb}'], ['F41'])
        cur, ck = F4[1], 'F41'
        oth, ok_ = F4[3], 'F43'
        st_ = 1
        while st_ < NT:
            c3 = cur[:, 0:NTE].rearrange("p (t e) -> p t e", e=16)
            o3 = oth[:, 0:NTE].rearrange("p (t e) -> p t e", e=16)
            V(lambda e, c3=c3, o3=o3, st_=st_: e.tensor_copy(out=o3[:, 0:st_, :], in_=c3[:, 0:st_, :]), [ck], [ok_])
            V(lambda e, c3=c3, o3=o3, st_=st_: e.tensor_tensor(out=o3[:, st_:NT, :], in0=c3[:, st_:NT, :],
                                                               in1=c3[:, 0:NT - st_, :], op=ALU.add), [ck], [ok_])
            cur, ck, oth, ok_ = oth, ok_, cur, ck
            st_ *= 2
        INC = cur[:, 0:NTE]
        V(lambda e: e.tensor_tensor(out=W1, in0=W1, in1=INC, op=ALU.add), ['F40', ck], ['F40'])
        for hb in range(nhb):
            c0, c1 = hb * 512, min(NTE, (hb + 1) * 512)
            V(lambda e, hb=hb, c0=c0, c1=c1: e.tensor_tensor(out=W1[:, c0:c1], in0=W1[:, c0:c1],
                                                             in1=PB[2 + hb][:, 0:c1 - c0], op=ALU.subtract),
              ['F40', f'PB{2 + hb}'], ['F40'])
        V(lambda e: e.tensor_tensor(out=W1, in0=W1, in1=Mflat, op=ALU.mult), ['F40', 'F42'], ['F40'])
        V(lambda e: e.tensor_scalar(out=W1, in0=W1, scalar1=-1.0, scalar2=None, op0=ALU.add), ['F40'], ['F40'])
        POSI = F4[1][:, 0:NTE].bitcast(I32)
        V(lambda e: e.tensor_copy(out=POSI, in_=W1), ['F40'], ['F41'])
        AI = F4[3][:, 0:NTE].bitcast(I32)
        BI = F4[0][:, 0:NTE].bitcast(I32)
        V(lambda e: e.tensor_single_scalar(out=AI, in_=POSI, scalar=5, op=ALU.arith_shift_right), ['F41'], ['F43'])
        V(lambda e: e.tensor_single_scalar(out=BI, in_=POSI, scalar=31, op=ALU.bitwise_and), ['F41'], ['F40'])
        AFl = F4[2][:, 0:NTE].rearrange("p (t e) -> p t e", e=16)
        BFl = F4[1][:, 0:NTE].rearrange("p (t e) -> p t e", e=16)
        V(lambda e: e.tensor_copy(out=F4[2][:, 0:NTE], in_=AI), ['F43'], ['F42'])
        V(lambda e: e.tensor_copy(out=F4[1][:, 0:NTE], in_=BI), ['F40'], ['F41'])
        for i in range(NT):
            b = i % 2
            OHa = H4[b][:, 0:16 * NA]
            OHa3 = OHa.rearrange("p (e a) -> p e a", e=16)
            OHb3 = F2[b][:, 0:512].rearrange("p (e a) -> p e a", e=16)
            Rr_ = H4[b][:, 1024:2048]
            R3 = Rr_.rearrange("p (e a) -> p e a", e=16)
            V(lambda e, i=i, OHa3=OHa3: e.tensor_tensor(
                out=OHa3, in0=AFl[:, i, :].unsqueeze(2).to_broadcast([128, 16, NA]),
                in1=IOTA[:, 0:NA].unsqueeze(1).to_broadcast([128, 16, NA]), op=ALU.is_equal),
              ['F42', 'iota'], [f'H4{b}a'])
            V(lambda e, i=i, OHb3=OHb3: e.tensor_tensor(
                out=OHb3, in0=BFl[:, i, :].unsqueeze(2).to_broadcast([128, 16, 32]),
                in1=IOTA[:, 0:32].unsqueeze(1).to_broadcast([128, 16, 32]), op=ALU.is_equal),
              ['F41', 'iota', f'H4{b}r'], [f'F2{b}'])
            V(lambda e, OHb3=OHb3, R3=R3: e.tensor_scalar(out=R3[:, :, 0:32], in0=OHb3, scalar1=PIDX[:, 0:1],
                                                          scalar2=None, op0=ALU.mult),
              [f'F2{b}', 'pidx'], [f'H4{b}r'])
            V(lambda e, OHb3=OHb3, R3=R3, i=i: e.tensor_scalar(out=R3[:, :, 32:64], in0=OHb3, scalar1=float(i),
                                                               scalar2=None, op0=ALU.mult),
              [f'F2{b}'], [f'H4{b}r'])
            for q in range(4):
                mm(PB[q][0:4 * NA, 0:256], OHa[:, q * 4 * NA:(q + 1) * 4 * NA], Rr_[:, q * 256:(q + 1) * 256],
                   i == 0, i == NT - 1, [f'H4{b}a', f'H4{b}r'], [f'PB{q}'])
        LF = F2[2][:, 0:512]
        LF4 = LF.rearrange("p (q k b) -> p q k b", q=4, k=4)
        CP = F2[3][:, 0:256]
        CP3 = CP.rearrange("p (k b) -> p k b", k=4)
        for q in range(4):
            V(lambda e, q=q: e.tensor_copy(out=CP[0:4 * NA, :], in_=PB[q][0:4 * NA, 0:256]), [f'PB{q}'], ['F23'])
            V(lambda e, q=q: e.scalar_tensor_tensor(out=LF4[0:4 * NA, q, :, :], in0=CP3[0:4 * NA, :, 32:64], scalar=128.0,
                                                    in1=CP3[0:4 * NA, :, 0:32], op0=ALU.mult, op1=ALU.add),
              ['F23'], ['F22'])
        LI = F2[4][:, 0:512].bitcast(I32)
        LI4 = LI.rearrange("p (q k b) -> p q k b", q=4, k=4)
        V(lambda e: e.tensor_copy(out=LI[0:4 * NA, :], in_=LF[0:4 * NA, :]), ['F22'], ['F24'])
        for ex in range(NE):
            q, k = ex // 4, ex % 4
            dma(LISTD[ex].rearrange("(a b) -> a b", b=32), LI4[k * NA:(k + 1) * NA, q, k, :], ['F24'], ['LISTD'])
        IDX3 = IDXt[:, 0:16 * M8].rearrange("p (e m) -> p e m", e=16)
        dma(IDX3, LISTD.rearrange("e (p m) -> p e m", m=M8), ['LISTD'], ['IDX'], allow_slow_non_contiguous=True)
        SG = min(512, CAP)
        for ex in range(NE):
            sl = [(3 * ex + k) % 4 for k in range(3)]
            WGv, WUv, WDv = [WB(s_).rearrange("p (c n) -> p c n", c=8) for s_ in sl]
            kg, ku, kd = [f'WB{s_}' for s_ in sl]
            load_w(WGv, [kg], W['w_gate'][l, ex], 8, 1024, rowscale=CV[:, 40:48], rkeys=['cv_f'])
            load_w(WUv, [ku], W['w_up'][l, ex], 8, 1024, rowscale=CV[:, 40:48], rkeys=['cv_f'])
            load_w(WDv, [kd], W['w_down'][l, ex], 8, 1024, eng='scalar')
            for sg in range(CAP // SG):
                xb = H8[sg % 2]
                xk_ = f'H8{sg % 2}'
                XST = xb[:, 0:8 * SG].rearrange("p (c n) -> p c n", c=8)
                nm = SG // 128
                for m4 in range(nm):
                    m = sg * nm + m4
                    b = m % 2
                    xs_, xsk = H2[2 + b], f'H2{2 + b}'
                    P.op('gpsimd', lambda e, xs_=xs_, ex=ex, m=m: e.indirect_dma_start(
                        out=xs_[:], out_offset=None, in_=H3R,
                        in_offset=bass.IndirectOffsetOnAxis(ap=IDX3[:, ex, m:m + 1], axis=0)),
                         ['H3R', 'IDX'], [xsk], dma=True)
                    P.op('gpsimd', lambda e, ex=ex, m=m: e.indirect_dma_start(
                        out=GAt[:, (m % 8) * 16:(m % 8) * 16 + 16], out_offset=None, in_=AFFD,
                        in_offset=bass.IndirectOffsetOnAxis(ap=IDX3[:, ex, m:m + 1], axis=0)),
                         ['AFFD', 'IDX'], [f'ga{m % 8}'], dma=True)
                    pt = PB[6 + b][:].bitcast(BF16).rearrange("p (c n) -> p c n", c=8)
                    for cc in range(8):
                        tr(pt[:, cc, :], xs_[:, cc * 128:(cc + 1) * 128], identb[:], [xsk, 'identb'], [f'PB{6 + b}'])
                    V(lambda e, pt=pt, m4=m4, XST=XST: e.tensor_copy(out=XST[:, :, m4 * 128:(m4 + 1) * 128], in_=pt),
                      [f'PB{6 + b}'], [xk_])
                ACTT = H8[2][:, 0:8 * SG].rearrange("p (c n) -> p c n", c=8)
                for fc in range(8):
                    ba, bu = fc % 2, 2 + fc % 2
                    for cc in range(8):
                        mm(PB[ba][:, 0:SG], WGv[:, cc, fc * 128:(fc + 1) * 128], XST[:, cc, :], cc == 0, cc == 7,
                           [kg, xk_], [f'PB{ba}'])
                    for cc in range(8):
                        mm(PB[bu][:, 0:SG], WUv[:, cc, fc * 128:(fc + 1) * 128], XST[:, cc, :], cc == 0, cc == 7,
                           [ku, xk_], [f'PB{bu}'])
                    SA = F2[fc % 2]
                    act(SA[:, 0:SG], PB[ba][:, 0:SG], AF.Silu, [f'PB{ba}'], [f'F2{fc % 2}'])
                    V(lambda e, fc=fc, SA=SA, bu=bu, ACTT=ACTT: e.tensor_tensor(out=ACTT[:, fc, :], in0=PB[bu][:, 0:SG],
                                                                               in1=SA[:, 0:SG], op=ALU.mult),
                      [f'PB{bu}', f'F2{fc % 2}'], ['H82'])
                for m4 in range(nm):
                    m = sg * nm + m4
                    ys = F4[m % 2]
                    yk = f'F4{m % 2}'
                    for half in range(2):
                        hs = slice(half * 512, (half + 1) * 512)
                        bk = 4 + half
                        for fc in range(8):
                            mm(PB[bk][:], ACTT[:, fc, m4 * 128:(m4 + 1) * 128], WDv[:, fc, hs], fc == 0, fc == 7,
                               ['H82', kd], [f'PB{bk}'])
                        V(lambda e, ys=ys, hs=hs, bk=bk, m=m, ex=ex: e.tensor_scalar(
                            out=ys[:, hs], in0=PB[bk][:], scalar1=GAt[:, (m % 8) * 16 + ex:(m % 8) * 16 + ex + 1],
                            scalar2=None, op0=ALU.mult), [f'PB{bk}', f'ga{m % 8}'], [yk])
                    P.op('gpsimd', lambda e, ys=ys, ex=ex, m=m: e.indirect_dma_start(
                        out=X, out_offset=bass.IndirectOffsetOnAxis(ap=IDX3[:, ex, m:m + 1], axis=0),
                        in_=ys[:], in_offset=None, compute_op=ALU.add), [yk, 'IDX'], ['X'], dma=True)

    def phase_F():
        dma(F4[2][:], c.final_g.partition_broadcast(128), [], ['F42'])
        for i in range(NT):
            b = i % 2
            xt, xk = F4[b], f'F4{b}'
            dma(xt[:], X[i * 128:(i + 1) * 128, :], ['X'], [xk])
            act(F4[3][:], xt[:], AF.Square, [xk], ['F43', 'ssq'], accum_out=SM[:, 128:129])
            rstd_from_ssq(SM[:, 129:130], SM[:, 128:129], D, ['ssq'], ['rs'])
            yt = H8[b][:].bitcast(F32)[:, 0:1024]
            V(lambda e, xt=xt, yt=yt: e.scalar_tensor_tensor(out=yt, in0=xt[:], scalar=SM[:, 129:130],
                                                             in1=F4[2][:], op0=ALU.mult, op1=ALU.mult),
              [xk, 'rs', 'F42'], [f'H8{b}'])
            dma(c.out[i * 128:(i + 1) * 128, :], yt, [f'H8{b}'], ['OUT'])

    c.phases = dict(B=phase_B, C=phase_C, A=phase_A, D=phase_D, E=phase_E)
    phase_A0()
    P.barrier()
    for l in range(L):
        for ph in phases:
            if ph in c.phases:
                c.phases[ph](l)
                P.barrier()
    phase_F()
    P.barrier()
    P.emit()
    return nc


def rope_table(S):
    rows = S // 64
    row_id = np.repeat(np.arange(rows, dtype=np.float32), 64)
    col_id = np.tile(np.arange(64, dtype=np.float32), rows)
    n_pairs = 16
    freqs = np.exp(-np.log(np.float32(10000.0)) * np.arange(n_pairs, dtype=np.float32) / n_pairs).astype(np.float32)
    ang = np.concatenate([row_id[:, None] * freqs[None, :], col_id[:, None] * freqs[None, :]], axis=-1)
    cos, sin = np.cos(ang).astype(np.float32), np.sin(ang).astype(np.float32)
    tab = np.zeros((S, 128), np.float32)
    tab[:, 0:64:2] = cos
    tab[:, 1:64:2] = cos
    tab[:, 64:96] = -sin
    tab[:, 96:128] = sin
    return tab


WNAMES = ['mix_norm_g', 'w_in', 'gm_v_norm_g', 'gm_w_s', 'gm_b_s', 'q_norm_g', 'k_norm_g', 'branch_norm_g',
          'w_out', 'xattn_norm_g', 'xattn_w_q', 'xattn_w_kv', 'xattn_w_o', 'ffn_norm_g', 'w_router',
          'w_gate', 'w_up', 'w_down']


def make_in_maps(inputs, S, L, nb, split=1):
    shared = {k: np.ascontiguousarray(np.asarray(inputs[k], dtype=np.float32)[:L]) for k in WNAMES}
    shared['mem_norm_g'] = np.asarray(inputs['mem_norm_g'], np.float32).reshape(1, D)
    shared['final_norm_g'] = np.asarray(inputs['final_norm_g'], np.float32).reshape(1, D)
    rope = rope_table(S)
    SL = S // split
    shared['identf'] = np.eye(128, dtype=np.float32)
    shared['identb'] = np.eye(128, dtype=np.float32).astype(ml_dtypes.bfloat16)
    shared['triu'] = np.triu(np.ones((128, 128), np.float32)).astype(ml_dtypes.bfloat16)
    shared['iota32'] = np.tile(np.arange(32, dtype=np.float32)[None, :], (128, 1))
    shared['pidx'] = np.arange(128, dtype=np.float32).reshape(128, 1)
    maps = []
    for b in range(nb):
        for r in range(split):
            m = dict(shared)
            m['x'] = np.ascontiguousarray(np.asarray(inputs['x'], np.float32)[b, r * SL:(r + 1) * SL])
            m['rope'] = np.ascontiguousarray(rope[r * SL:(r + 1) * SL])
            m['mem'] = np.ascontiguousarray(np.asarray(inputs['mem'], np.float32)[b])
            if split > 1:
                m['sel'] = np.stack([(split * b + q) * 128 + np.arange(128) for q in range(split)], axis=1).astype(np.int32)
            maps.append(m)
    return maps


def kernel(**inputs):
    x = np.asarray(inputs['x'])
    B, S, _ = x.shape
    L = np.asarray(inputs['w_in']).shape[0]
    nc = build(S, L)
    maps = make_in_maps(inputs, S, L, B)
    res = run_bass_kernel_spmd(nc, maps, core_ids=list(range(B)))
    return np.stack([np.asarray(r['out'], dtype=np.float32) for r in res.results], axis=0)
```

```python
import numpy as np
import ml_dtypes
from contextlib import ExitStack
import concourse.bass as bass
import concourse.mybir as mybir
from concourse.bass_utils import run_bass_kernel_spmd

F32 = mybir.dt.float32
BF16 = mybir.dt.bfloat16
I32 = mybir.dt.int32
AF = mybir.ActivationFunctionType
ALU = mybir.AluOpType
AX = mybir.AxisListType

D = 1024
MEM = 256
NE = 16
EPS = 1e-6
GELU = AF.Gelu_apprx_tanh
ENGS = ['tensor', 'vector', 'scalar', 'gpsimd', 'sync']
EPOCH = 20000
DEPOCH = 1200
DK = 8


class Prog:
    def __init__(self, nc, stack):
        self.nc = nc
        self.stack = stack
        self.rec = {e: [] for e in ENGS}
        self.cnt = {e: 0 for e in ENGS}
        self.esem = {e: None for e in ENGS}
        self.nsem = 0
        self.dring = {e: [None] * DK for e in ENGS}
        self.duse = {e: [0] * DK for e in ENGS}
        self.dn = {e: 0 for e in ENGS}
        self.waited = {e: {} for e in ENGS}
        self.lastw = {}
        self.readers = {}
        self.ninst = 0

    def newsem(self, tag):
        self.nsem += 1
        return self.stack.enter_context(self.nc.semaphore(f"{tag}_{self.nsem}"))

    def _wait(self, eng, tok):
        sem, val = tok[0], tok[1]
        w = self.waited[eng]
        if w.get(id(sem), 0) >= val:
            return
        w[id(sem)] = val
        self.rec[eng].append(lambda e, s=sem, v=val: e.wait_ge(s, v))

    def op(self, eng, fn, r=(), w=(), dma=False):
        toks = []
        for k in r:
            t = self.lastw.get(k)
            if t is not None:
                toks.append(t)
        for k in w:
            t = self.lastw.get(k)
            if t is not None:
                toks.append(t)
            toks.extend(self.readers.get(k, {}).values())
        for t in toks:
            if t[2] == 'tensor' and eng == 'tensor' and not dma:
                continue
            self._wait(eng, t)
        self.ninst += 1
        if dma:
            slot = self.dn[eng] % DK
            self.dn[eng] += 1
            sem = self.dring[eng][slot]
            prev = self.duse[eng][slot]
            if sem is not None and prev > 0:
                self._wait(eng, (sem, 16 * prev))
            if sem is None or prev >= DEPOCH:
                sem = self.newsem('d' + eng[:2])
                self.dring[eng][slot] = sem
                prev = 0
            self.duse[eng][slot] = prev + 1
            tok = (sem, 16 * (prev + 1), 'dma')
            self.rec[eng].append(lambda e, f=fn, s=sem: f(e).then_inc(s, 16))
        else:
            if self.esem[eng] is None or self.cnt[eng] >= EPOCH:
                self.esem[eng] = self.newsem('e' + eng[:2])
                self.cnt[eng] = 0
            self.cnt[eng] += 1
            sem = self.esem[eng]
            tok = (sem, self.cnt[eng], eng)
            self.rec[eng].append(lambda e, f=fn, s=sem: f(e).then_inc(s, 1))
        for k in r:
            self.readers.setdefault(k, {})[id(tok[0])] = tok
        for k in w:
            self.lastw[k] = tok
            self.readers[k] = {}
        return tok

    def wait_all(self, eng, keys):
        for k in keys:
            t = self.lastw.get(k)
            if t is not None:
                self._wait(eng, t)

    def barrier(self):
        toks = []
        for e in ENGS:
            if self.esem[e] is not None and self.cnt[e] > 0:
                toks.append((self.esem[e], self.cnt[e], e))
            for slot in range(DK):
                sem = self.dring[e][slot]
                if sem is not None and self.duse[e][slot] > 0:
                    toks.append((sem, 16 * self.duse[e][slot], 'dma'))
        for e in ENGS:
            for t in toks:
                self._wait(e, t)

    def emit(self):
        nc = self.nc
        with nc.Block() as block:
            @block.tensor
            def _(e):
                for f in self.rec['tensor']:
                    f(e)

            @block.vector
            def _(e):
                for f in self.rec['vector']:
                    f(e)

            @block.scalar
            def _(e):
                for f in self.rec['scalar']:
                    f(e)

            @block.gpsimd
            def _(e):
                for f in self.rec['gpsimd']:
                    f(e)

            @block.sync
            def _(e):
                for f in self.rec['sync']:
                    f(e)


class Ctx:
    pass


def build(S, L, dbg=False, phases=('B', 'C', 'A', 'D', 'E'), rg=None):
    NT = S // 128
    NG = S // 512
    CAP = 2 * S // NE
    nc = bass.Bass("TRN2", target_bir_lowering=False)
    stack = ExitStack()
    P = Prog(nc, stack)
    c = Ctx()
    c.nc, c.P, c.S, c.L, c.NT, c.NG, c.CAP = nc, P, S, L, NT, NG, CAP

    def din(name, shape, dt=F32):
        return nc.dram_tensor(name, list(shape), dt, kind="ExternalInput").ap()

    c.x_in = din('x', [S, D])
    c.mem = din('mem', [MEM, D])
    c.out = nc.dram_tensor('out', [S, D], F32, kind="ExternalOutput").ap()
    c.final_g = din('final_norm_g', [1, D])

    def sb(name, shape, dt=F32):
        return nc.alloc_sbuf_tensor(name, list(shape), dt)

    def ps(name, shape, dt=F32):
        return nc.alloc_psum_tensor(name, list(shape), dt)

    c.sb, c.ps = sb, ps

    def dma(out, in_, r, w, q='sync', **kw):
        return P.op(q, lambda e: e.dma_start(out=out, in_=in_, **kw), r, w, dma=True)

    def act(out, in_, func, r, w, **kw):
        return P.op('scalar', lambda e: e.activation(out=out, in_=in_, func=func, **kw), r, w)

    def mm(out, lhsT, rhs, start, stop, r, w):
        return P.op('tensor', lambda e: e.matmul(out, lhsT, rhs, start=start, stop=stop), r, w)

    def tr(out, in_, ident, r, w):
        return P.op('tensor', lambda e: e.transpose(out, in_, ident), r, w)

    def V(fn, r, w, eng='vector'):
        return P.op(eng, fn, r, w)

    c.dma, c.act, c.mm, c.tr, c.V = dma, act, mm, tr, V

    def rstd_from_ssq(out, ssq, n, r, w, eng='vector'):
        V(lambda e: e.tensor_scalar(out=out, in0=ssq, scalar1=1.0 / n, scalar2=EPS,
                                    op0=ALU.mult, op1=ALU.add), r, w)
        P.op('scalar', lambda e: e.sqrt(out=out, in_=out), w, w)
        V(lambda e: e.reciprocal(out=out, in_=out), w, w)

    c.rstd_from_ssq = rstd_from_ssq

    NTK = NT * (2 if rg else 1)
    SK = NTK * 128
    CAP = 2 * SK // NE
    def dram(name, shape, dt):
        return nc.dram_tensor(name, list(shape), dt, kind=("ExternalOutput" if dbg else "Internal")).ap()

    X = dram('Xs', [S, D], F32)
    GMT = dram('GMT', [4, 128, S], BF16)
    QT = dram('QT', [4, 128, S], BF16)
    H3T = dram('H3T', [8, 128, S], BF16)
    H3R = dram('H3R', [S, D], BF16)
    AFFD = dram('AFFD', [S, 16], F32)
    LISTD = dram('LISTD', [16, CAP], I32)
    M8 = CAP // 128
    NA = CAP // 32
    if rg:
        KTD = nc.dram_tensor('KTD', [128, S], BF16, kind='Internal').ap()
        NR = len(rg[0])
        KTA = nc.dram_tensor('KTA', [NR * 128, S], BF16, kind='Internal').ap()
        VAD = nc.dram_tensor('VAD', [128, NT * 130], BF16, kind='Internal').ap()
        VAA = nc.dram_tensor('VAA', [NR * 128, NT * 130], BF16, kind='Internal').ap()
        AFD = nc.dram_tensor('AFD', [128, NT * 16], F32, kind='Internal').ap()
        AFA = nc.dram_tensor('AFA', [NR * 128, NT * 16], F32, kind='Internal').ap()
        sel_in = din('sel', [128, 2], I32)
        SEL = sb('sel_s', [128, 2], I32)
        dma(SEL[:], sel_in, [], ['sel'])

    def gather_rows(out_ap, src_ap, r_, r, w):
        return P.op('gpsimd', lambda e: e.indirect_dma_start(
            out=out_ap, out_offset=None, in_=src_ap,
            in_offset=bass.IndirectOffsetOnAxis(ap=SEL[:, r_:r_ + 1], axis=0)), list(r) + ['sel'], w, dma=True)

    def allgather(out_ap, in_ap, r, w):
        return P.op('gpsimd', lambda e: e.collective_compute('AllGather', op=ALU.bypass, replica_groups=rg,
                                                             ins=[in_ap], outs=[out_ap]), r, w, dma=True)
    W = {}
    for nm, shp in [('mix_norm_g', [L, D]), ('w_in', [L, D, 1792]), ('gm_v_norm_g', [L, 512]),
                    ('gm_w_s', [L, 4, 128, 128]), ('gm_b_s', [L, 4, 128]), ('q_norm_g', [L, 64]),
                    ('k_norm_g', [L, 64]), ('branch_norm_g', [L, 2, 512]), ('w_out', [L, D, D]),
                    ('xattn_norm_g', [L, D]), ('mem_norm_g', [1, D]), ('xattn_w_q', [L, D, D]),
                    ('xattn_w_kv', [L, D, 2 * D]), ('xattn_w_o', [L, D, D]), ('ffn_norm_g', [L, D]),
                    ('w_router', [L, D, NE]), ('w_gate', [L, NE, D, D]), ('w_up', [L, NE, D, D]),
                    ('w_down', [L, NE, D, D]), ('rope', [S, 128]), ('identf', [128, 128])]:
        W[nm] = din(nm, shp)
    W['identb'] = din('identb', [128, 128], BF16)
    W['triu'] = din('triu', [128, 128], BF16)
    W['iota32'] = din('iota32', [128, 32])
    W['pidx'] = din('pidx', [128, 1])

    F4 = [sb(f'F4{i}', [128, 1024]) for i in range(4)]
    F2 = [sb(f'F2{i}', [128, 512]) for i in range(5)]
    H8 = [sb(f'H8{i}', [128, 4096], BF16) for i in range(3)]
    H4 = [sb(f'H4{i}', [128, 2048], BF16) for i in range(2)]
    H2 = [sb(f'H2{i}', [128, 1024], BF16) for i in range(4)]
    WBt = sb('WB', [128, 4 * 8192], BF16)
    STG = [sb(f'STG{i}', [128, 2048]) for i in range(2)]
    RES = sb('RES', [128, 8704])
    SM = sb('SM', [128, 256])
    identb = sb('identb_s', [128, 128], BF16)
    identf = sb('identf_s', [128, 128])
    onesf = sb('onesf', [128, 128])
    onesb = sb('onesb', [128, 128], BF16)
    GV = sb('gvbc', [128, 512])
    GQ = sb('gqbc', [128, 64])
    GK = sb('gkbc', [128, 64])
    CV = sb('colv', [128, 64])
    ROPE = [sb(f'rope{i}', [128, 128]) for i in range(2)]
    PBW = [ps(f'PBW{i}', [128, 1024]) for i in range(4)]
    PB = [PBW[i // 2][:, (i % 2) * 512:(i % 2 + 1) * 512] for i in range(8)]

    def WB(k):
        return WBt[:, k * 8192:(k + 1) * 8192]

    KT = RES[:, 0:SK // 2].bitcast(BF16)
    VAUG = RES[:, SK // 2:SK // 2 + NTK * 65].bitcast(BF16).rearrange("p (t h d) -> p t h d", t=NTK, h=2)
    AFF = RES[:, 0:NT * 16].rearrange("p (t e) -> p t e", e=16)
    MG = RES[:, NT * 16:2 * NT * 16].rearrange("p (t e) -> p t e", e=16)
    AFFA = RES[:, 2 * NT * 16:2 * NT * 16 + NTK * 16].rearrange("p (t e) -> p t e", e=16) if rg else AFF
    RSTDGM = SM[:, 0:NT]

    dma(identb[:], W['identb'], [], ['identb'])
    dma(identf[:], W['identf'], [], ['identf'])
    triu = sb('triu_s', [128, 128], BF16)
    IOTA = sb('iota_s', [128, 32])
    PIDX = sb('pidx_s', [128, 1])
    IDXt = sb('idx_s', [128, 128], I32)
    GAt = sb('ga_s', [128, 128])
    dma(triu[:], W['triu'], [], ['triu'])
    dma(IOTA[:], W['iota32'], [], ['iota'])
    dma(PIDX[:], W['pidx'], [], ['pidx'])
    V(lambda e: e.memset(onesf[:], 1.0), [], ['onesf'], 'gpsimd')
    V(lambda e: e.memset(onesb[:], 1.0), [], ['onesb'], 'gpsimd')
    for i in range(0, S, 1024):
        j = min(S, i + 1024)
        dma(X[i:j, :], c.x_in[i:j, :], [], ['X'])

    stg_n = [0]

    def load_w(dst3, dkeys, src2, KC, N, rowscale=None, mul=1.0, part=128, rkeys=(), eng='gpsimd'):
        cols = max(1, 2048 // KC)
        for n0 in range(0, N, cols):
            n1 = min(N, n0 + cols)
            k = stg_n[0] % 2
            stg_n[0] += 1
            stv = STG[k][0:part, 0:KC * (n1 - n0)].rearrange("p (c n) -> p c n", c=KC)
            dma(stv, src2[:, n0:n1].rearrange("(c p) n -> p c n", p=part), [], [f'STG{k}'])
            if rowscale is None and eng == 'scalar':
                act(dst3[:, :, n0:n1], stv, AF.Copy, [f'STG{k}'], dkeys)
            elif rowscale is None:
                V(lambda e, stv=stv, n0=n0, n1=n1: e.tensor_copy(out=dst3[:, :, n0:n1], in_=stv),
                  [f'STG{k}'], dkeys, 'gpsimd')
            else:
                for cc in range(KC):
                    V(lambda e, stv=stv, n0=n0, n1=n1, cc=cc: e.tensor_scalar(
                        out=dst3[:, cc, n0:n1], in0=stv[:, cc, :], scalar1=rowscale[:, cc:cc + 1],
                        scalar2=mul, op0=ALU.mult, op1=ALU.mult),
                      [f'STG{k}'] + list(rkeys), dkeys, 'gpsimd')

    def colvec(dst, src1d, p, key):
        P.op('sync', lambda e: e.dma_start(out=dst, in_=src1d.rearrange("(c p) -> p c", p=p),
                                           allow_slow_non_contiguous=True), [], [key], dma=True)

    tb_n = [0]

    def norm_T(xt, xkey, dst3, dkeys, hbuf, hkey, gain_bc=None, gkey=None, tbanks=(0, 1)):
        ssq = SM[:, 128:129]
        rs = SM[:, 129:130]
        act(hbuf[:, 0:1024], xt, AF.Square, [xkey], [hkey, 'ssq'], accum_out=ssq)
        rstd_from_ssq(rs, ssq, D, ['ssq'], ['rs'])
        act(hbuf[:, 0:1024], xt, AF.Copy, [xkey, 'rs'], [hkey], scale=rs)
        if gain_bc is not None:
            V(lambda e: e.tensor_tensor(out=hbuf[:, 0:1024], in0=hbuf[:, 0:1024], in1=gain_bc, op=ALU.mult),
              [hkey, gkey], [hkey])
        bk = tbanks[tb_n[0] % len(tbanks)]
        tb_n[0] += 1
        pt = PB[bk][:].bitcast(BF16).rearrange("p (c n) -> p c n", c=8)
        for cc in range(8):
            tr(pt[:, cc, :], hbuf[:, cc * 128:(cc + 1) * 128], identb[:], [hkey, 'identb'], [f'PB{bk}'])
        V(lambda e: e.tensor_copy(out=dst3, in_=pt), [f'PB{bk}'], dkeys)

    def phase_B(l):
        colvec(CV[:, 0:8], W['mix_norm_g'][l], 128, 'cv_mix')
        WIN = WBt[:, 2 * 8192:2 * 8192 + 8 * 1792].rearrange("p (c n) -> p c n", c=8)
        load_w(WIN, ['WB2', 'WB3'], W['w_in'][l], 8, 1792, rowscale=CV[:, 0:8], rkeys=['cv_mix'])
        dma(GV[:], W['gm_v_norm_g'][l:l + 1, :].partition_broadcast(128), [], ['gvbc'])
        dma(GQ[:], W['q_norm_g'][l:l + 1, :].partition_broadcast(128), [], ['gqbc'])
        dma(GK[:], W['k_norm_g'][l:l + 1, :].partition_broadcast(128), [], ['gkbc'])
        V(lambda e: e.tensor_scalar(out=GQ[:], in0=GQ[:], scalar1=0.125, scalar2=None, op0=ALU.mult),
          ['gqbc'], ['gqbc'])
        P.op('sync', lambda e: e.dma_start(out=CV[:, 8:12], in_=W['gm_b_s'][l].rearrange("g i -> i g"),
                                           allow_slow_non_contiguous=True), [], ['cv_bs'], dma=True)
        WST = H8[2][:, 0:512].rearrange("p (g i) -> p g i", g=4)
        for g in range(4):
            dma(F2[4][:, g * 128:(g + 1) * 128], W['gm_w_s'][l, g], [], ['F24'])
        V(lambda e: e.tensor_copy(out=H8[2][:, 512:1024], in_=F2[4][:]), ['F24'], ['H82s'])
        ptw = PB[0][:].bitcast(BF16)
        for g in range(4):
            tr(ptw[:, g * 128:(g + 1) * 128], H8[2][:, 512 + g * 128:512 + (g + 1) * 128], identb[:], ['H82s', 'identb'], ['PB0'])
        V(lambda e: e.tensor_copy(out=H8[2][:, 0:512], in_=ptw[:, 0:512]), ['PB0'], ['H82w'])
        V(lambda e: e.memset(VAUG[:, :, :, 64:65], 1.0), [], ['VAUG'], 'gpsimd')

        for i in range(NT):
            b = i % 2
            xt, xk = F4[b], f'F4{b}'
            dma(xt[:], X[i * 128:(i + 1) * 128, :], ['X'], [xk])
            dma(ROPE[b][:], W['rope'][i * 128:(i + 1) * 128, :], [], [f'rope{b}'])
            hT = H2[b][:].rearrange("p (c n) -> p c n", c=8)
            norm_T(xt[:], xk, hT, [f'H2{b}'], H4[0], 'H40')
            for gi, (c0, c1, bk) in enumerate([(0, 512, 2), (512, 1024, 3), (1024, 1536, 4), (1536, 1792, 5)]):
                for cc in range(8):
                    mm(PB[bk][:, 0:c1 - c0], hT[:, cc, :], WIN[:, cc, c0:c1], cc == 0, cc == 7,
                       [f'H2{b}', 'WB2', 'WB3'], [f'PB{bk}'])
            GU, GVt, GM = F2[0], F2[1], F2[2]
            act(GU[:], PB[2][:], GELU, ['PB2'], ['F20'])
            act(GVt[:], PB[3][:], GELU, ['PB3'], ['F21'])
            act(F2[3][:], GVt[:], AF.Square, ['F21'], ['F23', 'ssqv'], accum_out=SM[:, 130:131])
            rstd_from_ssq(SM[:, 131:132], SM[:, 130:131], 512, ['ssqv'], ['rsv'])
            VN = H4[1][:, 0:512]
            V(lambda e: e.scalar_tensor_tensor(out=VN, in0=GVt[:], scalar=SM[:, 131:132], in1=GV[:],
                                               op0=ALU.mult, op1=ALU.mult), ['F21', 'rsv', 'gvbc'], ['H41'])
            for g in range(4):
                mm(PB[6][:, g * 128:(g + 1) * 128], WST[:, g, :], VN[:, g * 128:(g + 1) * 128], True, True,
                   ['H82w', 'H41'], ['PB6'])
            for g in range(4):
                V(lambda e, g=g: e.scalar_tensor_tensor(
                    out=GM[:, g * 128:(g + 1) * 128], in0=PB[6][:, g * 128:(g + 1) * 128], scalar=CV[:, 8 + g:9 + g],
                    in1=GU[:, g * 128:(g + 1) * 128], op0=ALU.add, op1=ALU.mult), ['PB6', 'cv_bs', 'F20'], ['F22'])
            act(F2[3][:], GM[:], AF.Square, ['F22'], ['F23', 'ssqg'], accum_out=SM[:, 132:133])
            rstd_from_ssq(RSTDGM[:, i:i + 1], SM[:, 132:133], 512, ['ssqg'], ['rstdgm'])
            GMB = H4[1][:, 512:1024]
            act(GMB, GM[:], AF.Copy, ['F22'], ['H41b'])
            QN = F2[3]
            act(F2[4][:], PB[4][:], AF.Square, ['PB4'], ['F24'])
            V(lambda e: e.tensor_reduce(out=SM[:, 136:144], in_=F2[4][:].rearrange("p (h d) -> p h d", h=8),
                                        axis=AX.X, op=ALU.add), ['F24'], ['ssqq'])
            rstd_from_ssq(SM[:, 136:144], SM[:, 136:144], 64, ['ssqq'], ['ssqq'])
            V(lambda e: e.tensor_tensor(out=QN[:].rearrange("p (h d) -> p h d", h=8),
                                        in0=PB[4][:].rearrange("p (h d) -> p h d", h=8),
                                        in1=SM[:, 136:144].unsqueeze(2).to_broadcast([128, 8, 64]), op=ALU.mult),
              ['PB4', 'ssqq'], ['F23'])
            V(lambda e: e.tensor_tensor(out=QN[:].rearrange("p (h d) -> p h d", h=8),
                                        in0=QN[:].rearrange("p (h d) -> p h d", h=8),
                                        in1=GQ[:].unsqueeze(1).to_broadcast([128, 8, 64]), op=ALU.mult),
              ['F23', 'gqbc'], ['F23'])
            rp = ROPE[b]
            rk = f'rope{b}'

            def rope(src, nh, dst_view, skey, dkey, TT, tkey, rp=rp, rk=rk):
                s4 = src.rearrange("p (h i two) -> p h i two", h=nh, two=2)
                t4 = TT.rearrange("p (h i two) -> p h i two", h=nh, two=2)
                V(lambda e: e.tensor_tensor(out=t4[:, :, :, 0], in0=s4[:, :, :, 1],
                                            in1=rp[:, 64:96].unsqueeze(1).to_broadcast([128, nh, 32]), op=ALU.mult),
                  [skey, rk], [tkey])
                V(lambda e: e.tensor_tensor(out=t4[:, :, :, 1], in0=s4[:, :, :, 0],
                                            in1=rp[:, 96:128].unsqueeze(1).to_broadcast([128, nh, 32]), op=ALU.mult),
                  [skey, rk], [tkey])
                s3 = src.rearrange("p (h d) -> p h d", h=nh)
                V(lambda e: e.tensor_tensor(out=s3, in0=s3, in1=rp[:, 0:64].unsqueeze(1).to_broadcast([128, nh, 64]),
                                            op=ALU.mult), [skey, rk], [skey])
                if nh == 8:
                    a4 = src.rearrange("p (hh j d) -> p hh j d", hh=2, j=4)
                    b4 = TT.rearrange("p (hh j d) -> p hh j d", hh=2, j=4)
                else:
                    a4 = s3
                    b4 = TT.rearrange("p (h d) -> p h d", h=nh)
                V(lambda e: e.tensor_tensor(out=dst_view, in0=a4, in1=b4, op=ALU.add), [skey, tkey], [dkey])

            QR = H4[0][:, 1024:1536]
            rope(QN[:], 8, QR.rearrange("p (j hh d) -> p hh j d", j=4, hh=2), 'F23', 'H40q', F2[4][:], 'F24')
            KN = F2[4][:, 0:128]
            act(F2[3][:, 0:128], PB[5][:, 0:128], AF.Square, ['PB5', 'H40q'], ['F23'])
            V(lambda e: e.tensor_reduce(out=SM[:, 144:146], in_=F2[3][:, 0:128].rearrange("p (h d) -> p h d", h=2),
                                        axis=AX.X, op=ALU.add), ['F23'], ['ssqk'])
            rstd_from_ssq(SM[:, 144:146], SM[:, 144:146], 64, ['ssqk'], ['ssqk'])
            V(lambda e: e.tensor_tensor(out=KN.rearrange("p (h d) -> p h d", h=2),
                                        in0=PB[5][:, 0:128].rearrange("p (h d) -> p h d", h=2),
                                        in1=SM[:, 144:146].unsqueeze(2).to_broadcast([128, 2, 64]), op=ALU.mult),
              ['PB5', 'ssqk'], ['F24'])
            V(lambda e: e.tensor_tensor(out=KN.rearrange("p (h d) -> p h d", h=2),
                                        in0=KN.rearrange("p (h d) -> p h d", h=2),
                                        in1=GK[:].unsqueeze(1).to_broadcast([128, 2, 64]), op=ALU.mult),
              ['F24', 'gkbc'], ['F24'])
            KR = H4[0][:, 1536:1664]
            rope(KN, 2, KR.rearrange("p (h d) -> p h d", h=2), 'F24', 'H40k', F2[3][:, 128:256], 'F23')
            V(lambda e, i=i: e.tensor_copy(out=VAUG[:, i, :, 0:64],
                                           in_=PB[5][:, 128:256].rearrange("p (h d) -> p h d", h=2)),
              ['PB5'], ['VAUG'])
            pt = PB[7][:].bitcast(BF16)
            for g in range(4):
                tr(pt[:, g * 128:(g + 1) * 128], GMB[:, g * 128:(g + 1) * 128], identb[:], ['H41b', 'identb'], ['PB7'])
            for j in range(4):
                tr(pt[:, 512 + j * 128:512 + (j + 1) * 128], QR[:, j * 128:(j + 1) * 128], identb[:],
                   ['H40q', 'identb'], ['PB7'])
            TS = H4[b][:, 0:0]
            OUTS = H2[2 + b]
            ok = f'H2{2 + b}'
            if i == 0:
                pass
            V(lambda e, OUTS=OUTS: e.tensor_copy(out=OUTS[:], in_=pt), ['PB7'], [ok])
            dma(GMT[:, :, i * 128:(i + 1) * 128].rearrange("c p n -> p c n"),
                OUTS[:, 0:512].rearrange("p (c n) -> p c n", c=4), [ok], ['GMT'])
            dma(QT[:, :, i * 128:(i + 1) * 128].rearrange("c p n -> p c n"),
                OUTS[:, 512:1024].rearrange("p (c n) -> p c n", c=4), [ok], ['QT'])
            bk = 'PB6'
            ptk = PB[6][:].bitcast(BF16)
            tr(ptk[:, 0:128], KR, identb[:], ['H40k', 'identb'], ['PB6'])
            V(lambda e, i=i: e.tensor_copy(out=KT[:, i * 128:(i + 1) * 128], in_=ptk[:, 0:128]), ['PB6'], ['KT'])

    def exchange_kv():
        if not rg:
            return
        dma(KTD, KT[:, 0:S], ['KT'], ['KTD'])
        dma(VAD, RES[:, SK // 2:SK // 2 + NT * 65].bitcast(BF16), ['VAUG'], ['VAD'])
        allgather(KTA, KTD, ['KTD'], ['KTA'])
        allgather(VAA, VAD, ['VAD'], ['VAA'])
        for r_ in range(2):
            gather_rows(KT[:, r_ * S:(r_ + 1) * S], KTA, r_, ['KTA'], ['KT'])
            gather_rows(RES[:, SK // 2 + r_ * NT * 65:SK // 2 + (r_ + 1) * NT * 65].bitcast(BF16), VAA, r_,
                        ['VAA'], ['VAUG'])

    def phase_C(l):
        exchange_kv()
        colvec(CV[:, 16:20], W['branch_norm_g'][l, 0], 128, 'cv_b0')
        colvec(CV[0:64, 20:28], W['branch_norm_g'][l, 1], 64, 'cv_b1')
        WO0 = WB(0)[:, 0:4096].rearrange("p (c n) -> p c n", c=4)
        WO1 = WB(1)[0:64, :].rearrange("p (c n) -> p c n", c=8)
        load_w(WO0, ['WB0'], W['w_out'][l, 0:512, :], 4, 1024, rowscale=CV[:, 16:20], rkeys=['cv_b0'])
        load_w(WO1, ['WB1'], W['w_out'][l, 512:1024, :], 8, 1024, rowscale=CV[0:64, 20:28], rkeys=['cv_b1'], part=64)
        NKP = NTK // 2
        GS = min(512, S)
        NGG = S // GS
        for g in range(NGG):
            t0 = g * GS
            QM = H8[2][:, 0:8 * GS].rearrange("p (h n) -> p h n", h=8)
            qk = 'H82'
            if g == 0:
                V(lambda e: e.memset(H8[2][:, 0:8 * GS], 0.0), [], ['H82'], 'gpsimd')
            dma(QM[0:64, 0:4, :], QT[:, 0:64, t0:t0 + GS].rearrange("c p n -> p c n"), ['QT'], [qk])
            dma(QM[64:128, 4:8, :], QT[:, 64:128, t0:t0 + GS].rearrange("c p n -> p c n"), ['QT'], [qk])
            ATT = H8[0][0:64, 0:8 * GS].rearrange("p (h n) -> p h n", h=8)
            SQACC = F2[4][0:64, 0:GS]
            steps = [(h, kp) for h in range(8) for kp in range(NKP)]

            def emit_qk(h, kp):
                j, hh = h % 4, h // 4
                sbk = (kp % 2) * 2
                for k2 in range(2):
                    kt = kp * 2 + k2
                    mm(PB[sbk + k2][:, 0:GS], KT[:, kt * 128:(kt + 1) * 128],
                       QM[:, h, :], True, True, ['KT', qk], [f'PB{sbk + k2}'])

            def emit_exp_pv(h, kp):
                hh = h // 4
                ob = 4 + (h % 2)
                OT = PB[ob]
                sbk = (kp % 2) * 2
                PT = H2[kp % 3]
                pk = f'H2{kp % 3}'
                if GS == 512:
                    act(PT[:, 0:1024], PBW[kp % 2][:, 0:1024], AF.Exp, [f'PB{sbk}', f'PB{sbk + 1}'], [pk])
                else:
                    for k2 in range(2):
                        act(PT[:, k2 * 512:k2 * 512 + GS], PB[sbk + k2][:, 0:GS], AF.Exp, [f'PB{sbk + k2}'], [pk])
                for k2 in range(2):
                    kt = kp * 2 + k2
                    mm(OT[0:65, 0:GS], VAUG[:, kt, hh, :], PT[:, k2 * 512:k2 * 512 + GS],
                       kt == 0, kt == NTK - 1, ['VAUG', pk], [f'PB{ob}'])

            def finalize(h):
                ob = 4 + (h % 2)
                OT = PB[ob]
                SR = F2[3]
                V(lambda e, OT=OT: e.tensor_copy(out=SR[64:65, 0:GS], in_=OT[64:65, 0:GS]), [f'PB{ob}'], ['F23'])
                mm(PB[6][0:64, 0:GS], onesf[64:65, 0:64], SR[64:65, 0:GS], True, True, ['onesf', 'F23'], ['PB6'])
                Rr = F2[2]
                V(lambda e: e.reciprocal(out=Rr[0:64, 0:GS], in_=PB[6][0:64, 0:GS]), ['PB6'], ['F22'])
                V(lambda e, OT=OT, h=h: e.tensor_tensor(out=ATT[:, h, :], in0=OT[0:64, 0:GS], in1=Rr[0:64, 0:GS],
                                                        op=ALU.mult), [f'PB{ob}', 'F22'], ['H80'])
                if h == 0:
                    V(lambda e, h=h: e.tensor_tensor(out=SQACC, in0=ATT[:, h, :], in1=ATT[:, h, :], op=ALU.mult),
                      ['H80'], ['F24'], 'gpsimd')
                else:
                    V(lambda e, h=h: e.tensor_tensor(out=F2[1][0:64, 0:GS], in0=ATT[:, h, :], in1=ATT[:, h, :],
                                                     op=ALU.mult), ['H80'], ['F21'], 'gpsimd')
                    V(lambda e: e.tensor_tensor(out=SQACC, in0=SQACC, in1=F2[1][0:64, 0:GS], op=ALU.add),
                      ['F21', 'F24'], ['F24'], 'gpsimd')

            emit_qk(*steps[0])
            pending = None
            for si, (h, kp) in enumerate(steps):
                if si + 1 < len(steps):
                    emit_qk(*steps[si + 1])
                emit_exp_pv(h, kp)
                if pending is not None and kp == 0:
                    finalize(pending)
                    pending = None
                if kp == NKP - 1:
                    pending = h
            finalize(pending)
            mm(PB[6][0:1, 0:GS], onesf[0:64, 0:1], SQACC, True, True, ['onesf', 'F24'], ['PB6'])
            V(lambda e: e.tensor_copy(out=F2[3][0:1, 0:GS], in_=PB[6][0:1, 0:GS]), ['PB6'], ['F23'])
            for tt in range(GS // 128):
                mm(PB[6][:, tt:tt + 1], F2[3][0:1, tt * 128:(tt + 1) * 128], onesf[0:1, 0:1], True, True,
                   ['F23', 'onesf'], ['PB6'])
            rstd_from_ssq(SM[:, 148:148 + GS // 128], PB[6][:, 0:GS // 128], 512, ['PB6'], ['rsat'])
            gmTg = H8[1][:, 0:4 * GS].rearrange("p (c n) -> p c n", c=4)
            dma(gmTg, GMT[:, :, t0:t0 + GS].rearrange("c p n -> p c n"), ['GMT'], ['H81'])
            for tt in range(GS // 128):
                i = g * (GS // 128) + tt
                b = tt % 2
                xt, xk = F4[b], f'F4{b}'
                dma(xt[:], X[i * 128:(i + 1) * 128, :], ['X'], [xk])
                for half in range(2):
                    hs = slice(half * 512, (half + 1) * 512)
                    for cc in range(4):
                        mm(PB[7][:], gmTg[:, cc, tt * 128:(tt + 1) * 128], WO0[:, cc, hs], cc == 0, cc == 3,
                           ['H81', 'WB0'], ['PB7'])
                    V(lambda e, xt=xt, hs=hs, i=i: e.scalar_tensor_tensor(
                        out=xt[:, hs], in0=PB[7][:], scalar=RSTDGM[:, i:i + 1], in1=xt[:, hs],
                        op0=ALU.mult, op1=ALU.add), ['PB7', 'rstdgm', xk], [xk])
                    for h in range(8):
                        mm(PB[7][:], ATT[:, h, tt * 128:(tt + 1) * 128], WO1[:, h, hs], h == 0, h == 7,
                           ['H80', 'WB1'], ['PB7'])
                    V(lambda e, xt=xt, hs=hs, tt=tt: e.scalar_tensor_tensor(
                        out=xt[:, hs], in0=PB[7][:], scalar=SM[:, 148 + tt:149 + tt], in1=xt[:, hs],
                        op0=ALU.mult, op1=ALU.add), ['PB7', 'rsat', xk], [xk])
                dma(X[i * 128:(i + 1) * 128, :], xt[:], [xk], ['X'])

    MNTt = sb('MNT', [128, 2048], BF16)
    MNT = MNTt[:].rearrange("p (c n) -> p c n", c=8)
    KXt = sb('KX', [128, 2048], BF16)
    KX = KXt[:].rearrange("p (c n) -> p c n", c=8)
    VXt = sb('VX', [128, 2048], BF16)
    VX = VXt[:].rearrange("p (m n) -> p m n", m=2)
    WRf = sb('WRf', [128, 128])

    def phase_A0():
        dma(F4[2][:], W['mem_norm_g'].partition_broadcast(128), [], ['F42'])
        for mt in range(2):
            dma(F4[mt][:], c.mem[mt * 128:(mt + 1) * 128, :], [], [f'F4{mt}'])
            norm_T(F4[mt][:], f'F4{mt}', MNT[:, :, mt * 128:(mt + 1) * 128], ['MNT'], H4[0], 'H40',
                   gain_bc=F4[2][:], gkey='F42')

    def phase_A(l):
        WK = WB(2).rearrange("p (c n) -> p c n", c=8)
        WV = WB(3).rearrange("p (c n) -> p c n", c=8)
        load_w(WK, ['WB2'], W['xattn_w_kv'][l, :, 0:1024], 8, 1024)
        load_w(WV, ['WB3'], W['xattn_w_kv'][l, :, 1024:2048], 8, 1024)
        for dc in range(8):
            bk = dc % 2
            for cc in range(8):
                mm(PB[bk][:, 0:256], WK[:, cc, dc * 128:(dc + 1) * 128], MNT[:, cc, :], cc == 0, cc == 7,
                   ['WB2', 'MNT'], [f'PB{bk}'])
            V(lambda e, dc=dc, bk=bk: e.tensor_copy(out=KX[:, dc, :], in_=PB[bk][:, 0:256]), [f'PB{bk}'], ['KX'])
        for mt in range(2):
            for half in range(2):
                bk = 2 + half
                for cc in range(8):
                    mm(PB[bk][:], MNT[:, cc, mt * 128:(mt + 1) * 128], WV[:, cc, half * 512:(half + 1) * 512],
                       cc == 0, cc == 7, ['WB3', 'MNT'], [f'PB{bk}'])
                V(lambda e, mt=mt, half=half, bk=bk: e.tensor_copy(out=VX[:, mt, half * 512:(half + 1) * 512],
                                                                   in_=PB[bk][:]), [f'PB{bk}'], ['VX'])

    def phase_D(l):
        colvec(CV[:, 32:40], W['xattn_norm_g'][l], 128, 'cv_x')
        WQ = WB(0).rearrange("p (c n) -> p c n", c=8)
        WOX = WB(1).rearrange("p (c n) -> p c n", c=8)
        load_w(WQ, ['WB0'], W['xattn_w_q'][l], 8, 1024, rowscale=CV[:, 32:40], mul=1.0 / 16.0, rkeys=['cv_x'])
        load_w(WOX, ['WB1'], W['xattn_w_o'][l], 8, 1024)
        GS = min(512, S)
        for g in range(S // GS):
            t0 = g * GS
            H2Tg = H8[1][:, 0:8 * GS].rearrange("p (c n) -> p c n", c=8)
            for tt in range(GS // 128):
                i = g * (GS // 128) + tt
                b = tt % 2
                dma(F4[b][:], X[i * 128:(i + 1) * 128, :], ['X'], [f'F4{b}'])
                norm_T(F4[b][:], f'F4{b}', H2Tg[:, :, tt * 128:(tt + 1) * 128], ['H81'], H4[0], 'H40')
            QX = H8[2][:, 0:8 * GS].rearrange("p (c n) -> p c n", c=8)
            for dc in range(8):
                bk = 2 + dc % 2
                for cc in range(8):
                    mm(PB[bk][:, 0:GS], WQ[:, cc, dc * 128:(dc + 1) * 128], H2Tg[:, cc, :], cc == 0, cc == 7,
                       ['WB0', 'H81'], [f'PB{bk}'])
                act(QX[:, dc, :], PB[bk][:, 0:GS], AF.Copy, [f'PB{bk}'], ['H82'])
            OXT = H8[0][:, 0:8 * GS].rearrange("p (c n) -> p c n", c=8)
            for h in range(4):
                PX = H2[2 + h % 2]
                pk = f'H2{2 + h % 2}'
                for mt in range(2):
                    for dd in range(2):
                        mm(PB[4 + mt][:, 0:GS], KX[:, 2 * h + dd, mt * 128:(mt + 1) * 128], QX[:, 2 * h + dd, :],
                           dd == 0, dd == 1, ['KX', 'H82'], [f'PB{4 + mt}'])
                    if GS != 512:
                        act(PX[:, mt * 512:mt * 512 + GS], PB[4 + mt][:, 0:GS], AF.Exp, [f'PB{4 + mt}'], [pk])
                if GS == 512:
                    act(PX[:, 0:1024], PBW[2][:, 0:1024], AF.Exp, ['PB4', 'PB5'], [pk])
                for mt in range(2):
                    mm(PB[6][:, 0:GS], onesb[:], PX[:, mt * 512:mt * 512 + GS], mt == 0, mt == 1,
                       ['onesb', pk], ['PB6'])
                V(lambda e: e.reciprocal(out=F2[2][:, 0:GS], in_=PB[6][:, 0:GS]), ['PB6'], ['F22'])
                for dd in range(2):
                    for mt in range(2):
                        mm(PB[7][:, 0:GS], VX[:, mt, (2 * h + dd) * 128:(2 * h + dd + 1) * 128],
                           PX[:, mt * 512:mt * 512 + GS], mt == 0, mt == 1, ['VX', pk], ['PB7'])
                    V(lambda e, h=h, dd=dd: e.tensor_tensor(out=OXT[:, 2 * h + dd, :], in0=PB[7][:, 0:GS],
                                                            in1=F2[2][:, 0:GS], op=ALU.mult),
                      ['PB7', 'F22'], ['H80'])
            for tt in range(GS // 128):
                i = g * (GS // 128) + tt
                b = tt % 2
                xt, xk = F4[2 + b], f'F4{2 + b}'
                dma(xt[:], X[i * 128:(i + 1) * 128, :], ['X'], [xk])
                for half in range(2):
                    hs = slice(half * 512, (half + 1) * 512)
                    bk = 2 + half
                    for cc in range(8):
                        mm(PB[bk][:], OXT[:, cc, tt * 128:(tt + 1) * 128], WOX[:, cc, hs], cc == 0, cc == 7,
                           ['H80', 'WB1'], [f'PB{bk}'])
                    V(lambda e, xt=xt, hs=hs, bk=bk: e.tensor_tensor(out=xt[:, hs], in0=PB[bk][:], in1=xt[:, hs],
                                                                     op=ALU.add), [f'PB{bk}', xk], [xk])
                dma(X[i * 128:(i + 1) * 128, :], xt[:], [xk], ['X'])

    def phase_E(l):
        colvec(CV[:, 40:48], W['ffn_norm_g'][l], 128, 'cv_f')
        WR3 = WRf[:].rearrange("p (c e) -> p c e", c=8)
        dma(WR3, W['w_router'][l].rearrange("(c p) e -> p c e", p=128), [], ['WRf'])
        for cc in range(8):
            V(lambda e, cc=cc: e.tensor_scalar(out=WR3[:, cc, :], in0=WR3[:, cc, :], scalar1=CV[:, 40 + cc:41 + cc],
                                               scalar2=None, op0=ALU.mult), ['WRf', 'cv_f'], ['WRf'])
        for i in range(NT):
            b = i % 2
            xt, xk = F4[b], f'F4{b}'
            dma(xt[:], X[i * 128:(i + 1) * 128, :], ['X'], [xk])
            act(F4[2][:], xt[:], AF.Square, [xk], ['F42', 'ssq'], accum_out=SM[:, 128:129])
            rstd_from_ssq(SM[:, 129:130], SM[:, 128:129], D, ['ssq'], ['rs'])
            act(F4[2][:], xt[:], AF.Copy, [xk, 'rs'], ['F42'], scale=SM[:, 129:130])
            for cc in range(8):
                bk = cc // 4
                tr(PB[bk][:, (cc % 4) * 128:(cc % 4 + 1) * 128], F4[2][:, cc * 128:(cc + 1) * 128], identf[:],
                   ['F42', 'identf'], [f'PB{bk}'])
            V(lambda e: e.tensor_copy(out=F4[3][:, 0:512], in_=PB[0][:]), ['PB0'], ['F43'])
            V(lambda e: e.tensor_copy(out=F4[3][:, 512:1024], in_=PB[1][:]), ['PB1'], ['F43'])
            for cc in range(8):
                mm(PB[2][:, 0:16], F4[3][:, cc * 128:(cc + 1) * 128], WR3[:, cc, :], cc == 0, cc == 7,
                   ['F43', 'WRf'], ['PB2'])
            V(lambda e: e.reduce_max(out=SM[:, 150:151], in_=PB[2][:, 0:16], axis=AX.X), ['PB2'], ['smx'])
            V(lambda e: e.tensor_scalar(out=SM[:, 150:151], in0=SM[:, 150:151], scalar1=-1.0, scalar2=None,
                                        op0=ALU.mult), ['smx'], ['smx'])
            act(SM[:, 160:176], PB[2][:, 0:16], AF.Exp, ['PB2', 'smx'], ['sme', 'sms'], bias=SM[:, 150:151],
                accum_out=SM[:, 151:152])
            V(lambda e: e.reciprocal(out=SM[:, 151:152], in_=SM[:, 151:152]), ['sms'], ['sms'])
            V(lambda e, i=i: e.tensor_scalar(out=AFF[:, i, :], in0=SM[:, 160:176], scalar1=SM[:, 151:152],
                                             scalar2=None, op0=ALU.mult), ['sme', 'sms'], ['AFF'])
            hb = H2[b]
            V(lambda e, hb=hb: e.tensor_copy(out=hb[:], in_=F4[2][:]), ['F42'], [f'H2{b}'])
            dma(H3R[i * 128:(i + 1) * 128, :], hb[:], [f'H2{b}'], ['H3R'])
            dma(AFFD[i * 128:(i + 1) * 128, :], AFF[:, i, :], ['AFF'], ['AFFD'])
        if rg:
            dma(AFD, RES[:, 0:NT * 16], ['AFF'], ['AFD'])
            allgather(AFA, AFD, ['AFD'], ['AFA'])
            for r_ in range(2):
                gather_rows(RES[:, 2 * NT * 16 + r_ * NT * 16:2 * NT * 16 + (r_ + 1) * NT * 16], AFA, r_,
                            ['AFA'], ['AFFA'])
        akey = 'AFFA' if rg else 'AFF'
        LO, HI, MID = SM[:, 160:176], SM[:, 176:192], SM[:, 192:208]
        PART, CC, D1 = SM[:, 208:224], SM[:, 224:240], SM[:, 240:256]
        CMPA = F4[2][:, 0:NTK * 16].rearrange("p (t e) -> p t e", e=16)
        CMP = F4[2][:, 0:NT * 16].rearrange("p (t e) -> p t e", e=16)
        V(lambda e: e.memset(LO, 0.0), ['sme'], ['lo'])
        V(lambda e: e.memset(HI, 1.0), [], ['hi'])
        for it in range(32):
            V(lambda e: e.tensor_tensor(out=MID, in0=LO, in1=HI, op=ALU.add), ['lo', 'hi'], ['mid'])
            V(lambda e: e.tensor_scalar(out=MID, in0=MID, scalar1=0.5, scalar2=None, op0=ALU.mult), ['mid'], ['mid'])
            V(lambda e: e.tensor_tensor(out=CMPA, in0=AFFA, in1=MID.unsqueeze(1).to_broadcast([128, NTK, 16]),
                                        op=ALU.is_gt), [akey, 'mid'], ['F42'])
            V(lambda e: e.tensor_reduce(out=PART, in_=CMPA.rearrange("p t e -> p e t"), axis=AX.X, op=ALU.add),
              ['F42'], ['part'])
            mm(PB[3][:, 0:16], onesf[:], PART, True, True, ['onesf', 'part'], ['PB3'])
            V(lambda e: e.tensor_scalar(out=CC, in0=PB[3][:, 0:16], scalar1=float(CAP) - 0.5, scalar2=None,
                                        op0=ALU.is_ge), ['PB3'], ['cc'])
            V(lambda e: e.tensor_tensor(out=D1, in0=MID, in1=LO, op=ALU.subtract), ['mid', 'lo'], ['d1'])
            V(lambda e: e.tensor_tensor(out=D1, in0=D1, in1=CC, op=ALU.mult), ['d1', 'cc'], ['d1'])
            V(lambda e: e.tensor_tensor(out=LO, in0=LO, in1=D1, op=ALU.add), ['d1', 'lo'], ['lo'])
            V(lambda e: e.tensor_tensor(out=D1, in0=HI, in1=MID, op=ALU.subtract), ['mid', 'hi'], ['d1'])
            V(lambda e: e.tensor_tensor(out=D1, in0=D1, in1=CC, op=ALU.mult), ['d1', 'cc'], ['d1'])
            V(lambda e: e.tensor_tensor(out=HI, in0=MID, in1=D1, op=ALU.add), ['d1', 'mid'], ['hi'])
        V(lambda e: e.tensor_tensor(out=CMP, in0=AFF, in1=LO.unsqueeze(1).to_broadcast([128, NT, 16]),
                                    op=ALU.is_gt), ['AFF', 'lo'], ['F42'])
        V(lambda e: e.tensor_tensor(out=MG, in0=CMP, in1=AFF, op=ALU.mult), ['F42', 'AFF'], ['MG'])
        NTE = NT * 16
        Mflat = F4[2][:, 0:NTE]
        Mb = H2[0][:, 0:NTE]
        V(lambda e: e.tensor_copy(out=Mb, in_=Mflat), ['F42'], ['H20'])
        W1 = F4[0][:, 0:NTE]
        NRr = F4[1][:, 0:NTE]
        nhb = (NTE + 511) // 512
        for hb in range(nhb):
            c0, c1 = hb * 512, min(NTE, (hb + 1) * 512)
            mm(PB[hb][:, 0:c1 - c0], triu[:], Mb[:, c0:c1], True, True, ['triu', 'H20'], [f'PB{hb}'])
            mm(PB[2 + hb][:, 0:c1 - c0], onesb[:], Mb[:, c0:c1], True, True, ['onesb', 'H20'], [f'PB{2 + hb}'])
            V(lambda e, hb=hb, c0=c0, c1=c1: e.tensor_copy(out=W1[:, c0:c1], in_=PB[hb][:, 0:c1 - c0]),
              [f'PB{hb}'], ['F40'])
            V(lambda e, hb=hb, c0=c0, c1=c1: e.tensor_copy(out=NRr[:, c0:c1], in_=PB[2 + hb][:, 0:c1 - c0]),
              [f'PB{2 + hb}'], ['F41'])
        cur, ck = F4[1], 'F41'
        oth, ok_ = F4[3], 'F43'
        st_ = 1
        while st_ < NT:
            c3 = cur[:, 0:NTE].rearrange("p (t e) -> p t e", e=16)
            o3 = oth[:, 0:NTE].rearrange("p (t e) -> p t e", e=16)
            V(lambda e, c3=c3, o3=o3, st_=st_: e.tensor_copy(out=o3[:, 0:st_, :], in_=c3[:, 0:st_, :]), [ck], [ok_])
            V(lambda e, c3=c3, o3=o3, st_=st_: e.tensor_tensor(out=o3[:, st_:NT, :], in0=c3[:, st_:NT, :],
                                                               in1=c3[:, 0:NT - st_, :], op=ALU.add), [ck], [ok_])
            cur, ck, oth, ok_ = oth, ok_, cur, ck
            st_ *= 2
        INC = cur[:, 0:NTE]
        V(lambda e: e.tensor_tensor(out=W1, in0=W1, in1=INC, op=ALU.add), ['F40', ck], ['F40'])
        for hb in range(nhb):
            c0, c1 = hb * 512, min(NTE, (hb + 1) * 512)
            V(lambda e, hb=hb, c0=c0, c1=c1: e.tensor_tensor(out=W1[:, c0:c1], in0=W1[:, c0:c1],
                                                             in1=PB[2 + hb][:, 0:c1 - c0], op=ALU.subtract),
              ['F40', f'PB{2 + hb}'], ['F40'])
        V(lambda e: e.tensor_tensor(out=W1, in0=W1, in1=Mflat, op=ALU.mult), ['F40', 'F42'], ['F40'])
        V(lambda e: e.tensor_scalar(out=W1, in0=W1, scalar1=-1.0, scalar2=None, op0=ALU.add), ['F40'], ['F40'])
        POSI = F4[1][:, 0:NTE].bitcast(I32)
        V(lambda e: e.tensor_copy(out=POSI, in_=W1), ['F40'], ['F41'])
        AI = F4[3][:, 0:NTE].bitcast(I32)
        BI = F4[0][:, 0:NTE].bitcast(I32)
        V(lambda e: e.tensor_single_scalar(out=AI, in_=POSI, scalar=5, op=ALU.arith_shift_right), ['F41'], ['F43'])
        V(lambda e: e.tensor_single_scalar(out=BI, in_=POSI, scalar=31, op=ALU.bitwise_and), ['F41'], ['F40'])
        AFl = F4[2][:, 0:NTE].rearrange("p (t e) -> p t e", e=16)
        BFl = F4[1][:, 0:NTE].rearrange("p (t e) -> p t e", e=16)
        V(lambda e: e.tensor_copy(out=F4[2][:, 0:NTE], in_=AI), ['F43'], ['F42'])
        V(lambda e: e.tensor_copy(out=F4[1][:, 0:NTE], in_=BI), ['F40'], ['F41'])
        for i in range(NT):
            b = i % 2
            OHa = H4[b][:, 0:16 * NA]
            OHa3 = OHa.rearrange("p (e a) -> p e a", e=16)
            OHb3 = F2[b][:, 0:512].rearrange("p (e a) -> p e a", e=16)
            Rr_ = H4[b][:, 1024:2048]
            R3 = Rr_.rearrange("p (e a) -> p e a", e=16)
            V(lambda e, i=i, OHa3=OHa3: e.tensor_tensor(
                out=OHa3, in0=AFl[:, i, :].unsqueeze(2).to_broadcast([128, 16, NA]),
                in1=IOTA[:, 0:NA].unsqueeze(1).to_broadcast([128, 16, NA]), op=ALU.is_equal),
              ['F42', 'iota'], [f'H4{b}a'])
            V(lambda e, i=i, OHb3=OHb3: e.tensor_tensor(
                out=OHb3, in0=BFl[:, i, :].unsqueeze(2).to_broadcast([128, 16, 32]),
                in1=IOTA[:, 0:32].unsqueeze(1).to_broadcast([128, 16, 32]), op=ALU.is_equal),
              ['F41', 'iota', f'H4{b}r'], [f'F2{b}'])
            V(lambda e, OHb3=OHb3, R3=R3: e.tensor_scalar(out=R3[:, :, 0:32], in0=OHb3, scalar1=PIDX[:, 0:1],
                                                          scalar2=None, op0=ALU.mult),
              [f'F2{b}', 'pidx'], [f'H4{b}r'])
            V(lambda e, OHb3=OHb3, R3=R3, i=i: e.tensor_scalar(out=R3[:, :, 32:64], in0=OHb3, scalar1=float(i),
                                                               scalar2=None, op0=ALU.mult),
              [f'F2{b}'], [f'H4{b}r'])
            for q in range(4):
                mm(PB[q][0:4 * NA, 0:256], OHa[:, q * 4 * NA:(q + 1) * 4 * NA], Rr_[:, q * 256:(q + 1) * 256],
                   i == 0, i == NT - 1, [f'H4{b}a', f'H4{b}r'], [f'PB{q}'])
        LF = F2[2][:, 0:512]
        LF4 = LF.rearrange("p (q k b) -> p q k b", q=4, k=4)
        CP = F2[3][:, 0:256]
        CP3 = CP.rearrange("p (k b) -> p k b", k=4)
        for q in range(4):
            V(lambda e, q=q: e.tensor_copy(out=CP[0:4 * NA, :], in_=PB[q][0:4 * NA, 0:256]), [f'PB{q}'], ['F23'])
            V(lambda e, q=q: e.scalar_tensor_tensor(out=LF4[0:4 * NA, q, :, :], in0=CP3[0:4 * NA, :, 32:64], scalar=128.0,
                                                    in1=CP3[0:4 * NA, :, 0:32], op0=ALU.mult, op1=ALU.add),
              ['F23'], ['F22'])
        LI = F2[4][:, 0:512].bitcast(I32)
        LI4 = LI.rearrange("p (q k b) -> p q k b", q=4, k=4)
        V(lambda e: e.tensor_copy(out=LI[0:4 * NA, :], in_=LF[0:4 * NA, :]), ['F22'], ['F24'])
        for ex in range(NE):
            q, k = ex // 4, ex % 4
            dma(LISTD[ex].rearrange("(a b) -> a b", b=32), LI4[k * NA:(k + 1) * NA, q, k, :], ['F24'], ['LISTD'])
        IDX3 = IDXt[:, 0:16 * M8].rearrange("p (e m) -> p e m", e=16)
        dma(IDX3, LISTD.rearrange("e (p m) -> p e m", m=M8), ['LISTD'], ['IDX'], allow_slow_non_contiguous=True)
        SG = min(512, CAP)
        for ex in range(NE):
            sl = [(3 * ex + k) % 4 for k in range(3)]
            WGv, WUv, WDv = [WB(s_).rearrange("p (c n) -> p c n", c=8) for s_ in sl]
            kg, ku, kd = [f'WB{s_}' for s_ in sl]
            load_w(WGv, [kg], W['w_gate'][l, ex], 8, 1024, rowscale=CV[:, 40:48], rkeys=['cv_f'])
            load_w(WUv, [ku], W['w_up'][l, ex], 8, 1024, rowscale=CV[:, 40:48], rkeys=['cv_f'])
            load_w(WDv, [kd], W['w_down'][l, ex], 8, 1024, eng='scalar')
            for sg in range(CAP // SG):
                xb = H8[sg % 2]
                xk_ = f'H8{sg % 2}'
                XST = xb[:, 0:8 * SG].rearrange("p (c n) -> p c n", c=8)
                nm = SG // 128
                for m4 in range(nm):
                    m = sg * nm + m4
                    b = m % 2
                    xs_, xsk = H2[2 + b], f'H2{2 + b}'
                    P.op('gpsimd', lambda e, xs_=xs_, ex=ex, m=m: e.indirect_dma_start(
                        out=xs_[:], out_offset=None, in_=H3R,
                        in_offset=bass.IndirectOffsetOnAxis(ap=IDX3[:, ex, m:m + 1], axis=0)),
                         ['H3R', 'IDX'], [xsk], dma=True)
                    P.op('gpsimd', lambda e, ex=ex, m=m: e.indirect_dma_start(
                        out=GAt[:, (m % 8) * 16:(m % 8) * 16 + 16], out_offset=None, in_=AFFD,
                        in_offset=bass.IndirectOffsetOnAxis(ap=IDX3[:, ex, m:m + 1], axis=0)),
                         ['AFFD', 'IDX'], [f'ga{m % 8}'], dma=True)
                    pt = PB[6 + b][:].bitcast(BF16).rearrange("p (c n) -> p c n", c=8)
                    for cc in range(8):
                        tr(pt[:, cc, :], xs_[:, cc * 128:(cc + 1) * 128], identb[:], [xsk, 'identb'], [f'PB{6 + b}'])
                    V(lambda e, pt=pt, m4=m4, XST=XST: e.tensor_copy(out=XST[:, :, m4 * 128:(m4 + 1) * 128], in_=pt),
                      [f'PB{6 + b}'], [xk_])
                ACTT = H8[2][:, 0:8 * SG].rearrange("p (c n) -> p c n", c=8)
                for fc in range(8):
                    ba, bu = fc % 2, 2 + fc % 2
                    for cc in range(8):
                        mm(PB[ba][:, 0:SG], WGv[:, cc, fc * 128:(fc + 1) * 128], XST[:, cc, :], cc == 0, cc == 7,
                           [kg, xk_], [f'PB{ba}'])
                    for cc in range(8):
                        mm(PB[bu][:, 0:SG], WUv[:, cc, fc * 128:(fc + 1) * 128], XST[:, cc, :], cc == 0, cc == 7,
                           [ku, xk_], [f'PB{bu}'])
                    SA = F2[fc % 2]
                    act(SA[:, 0:SG], PB[ba][:, 0:SG], AF.Silu, [f'PB{ba}'], [f'F2{fc % 2}'])
                    V(lambda e, fc=fc, SA=SA, bu=bu, ACTT=ACTT: e.tensor_tensor(out=ACTT[:, fc, :], in0=PB[bu][:, 0:SG],
                                                                               in1=SA[:, 0:SG], op=ALU.mult),
                      [f'PB{bu}', f'F2{fc % 2}'], ['H82'])
                for m4 in range(nm):
                    m = sg * nm + m4
                    ys = F4[m % 2]
                    yk = f'F4{m % 2}'
                    for half in range(2):
                        hs = slice(half * 512, (half + 1) * 512)
                        bk = 4 + half
                        for fc in range(8):
                            mm(PB[bk][:], ACTT[:, fc, m4 * 128:(m4 + 1) * 128], WDv[:, fc, hs], fc == 0, fc == 7,
                               ['H82', kd], [f'PB{bk}'])
                        V(lambda e, ys=ys, hs=hs, bk=bk, m=m, ex=ex: e.tensor_scalar(
                            out=ys[:, hs], in0=PB[bk][:], scalar1=GAt[:, (m % 8) * 16 + ex:(m % 8) * 16 + ex + 1],
                            scalar2=None, op0=ALU.mult), [f'PB{bk}', f'ga{m % 8}'], [yk])
                    P.op('gpsimd', lambda e, ys=ys, ex=ex, m=m: e.indirect_dma_start(
                        out=X, out_offset=bass.IndirectOffsetOnAxis(ap=IDX3[:, ex, m:m + 1], axis=0),
                        in_=ys[:], in_offset=None, compute_op=ALU.add), [yk, 'IDX'], ['X'], dma=True)

    def phase_F():
        dma(F4[2][:], c.final_g.partition_broadcast(128), [], ['F42'])
        for i in range(NT):
            b = i % 2
            xt, xk = F4[b], f'F4{b}'
            dma(xt[:], X[i * 128:(i + 1) * 128, :], ['X'], [xk])
            act(F4[3][:], xt[:], AF.Square, [xk], ['F43', 'ssq'], accum_out=SM[:, 128:129])
            rstd_from_ssq(SM[:, 129:130], SM[:, 128:129], D, ['ssq'], ['rs'])
            yt = H8[b][:].bitcast(F32)[:, 0:1024]
            V(lambda e, xt=xt, yt=yt: e.scalar_tensor_tensor(out=yt, in0=xt[:], scalar=SM[:, 129:130],
                                                             in1=F4[2][:], op0=ALU.mult, op1=ALU.mult),
              [xk, 'rs', 'F42'], [f'H8{b}'])
            dma(c.out[i * 128:(i + 1) * 128, :], yt, [f'H8{b}'], ['OUT'])

    c.phases = dict(B=phase_B, C=phase_C, A=phase_A, D=phase_D, E=phase_E)
    phase_A0()
    P.barrier()
    for l in range(L):
        for ph in phases:
            if ph in c.phases:
                c.phases[ph](l)
                P.barrier()
    phase_F()
    P.barrier()
    P.emit()
    return nc


def rope_table(S):
    rows = S // 64
    row_id = np.repeat(np.arange(rows, dtype=np.float32), 64)
    col_id = np.tile(np.arange(64, dtype=np.float32), rows)
    n_pairs = 16
    freqs = np.exp(-np.log(np.float32(10000.0)) * np.arange(n_pairs, dtype=np.float32) / n_pairs).astype(np.float32)
    ang = np.concatenate([row_id[:, None] * freqs[None, :], col_id[:, None] * freqs[None, :]], axis=-1)
    cos, sin = np.cos(ang).astype(np.float32), np.sin(ang).astype(np.float32)
    tab = np.zeros((S, 128), np.float32)
    tab[:, 0:64:2] = cos
    tab[:, 1:64:2] = cos
    tab[:, 64:96] = -sin
    tab[:, 96:128] = sin
    return tab


WNAMES = ['mix_norm_g', 'w_in', 'gm_v_norm_g', 'gm_w_s', 'gm_b_s', 'q_norm_g', 'k_norm_g', 'branch_norm_g',
          'w_out', 'xattn_norm_g', 'xattn_w_q', 'xattn_w_kv', 'xattn_w_o', 'ffn_norm_g', 'w_router',
          'w_gate', 'w_up', 'w_down']


def make_in_maps(inputs, S, L, nb, split=1):
    shared = {k: np.ascontiguousarray(np.asarray(inputs[k], dtype=np.float32)[:L]) for k in WNAMES}
    shared['mem_norm_g'] = np.asarray(inputs['mem_norm_g'], np.float32).reshape(1, D)
    shared['final_norm_g'] = np.asarray(inputs['final_norm_g'], np.float32).reshape(1, D)
    rope = rope_table(S)
    SL = S // split
    shared['identf'] = np.eye(128, dtype=np.float32)
    shared['identb'] = np.eye(128, dtype=np.float32).astype(ml_dtypes.bfloat16)
    shared['triu'] = np.triu(np.ones((128, 128), np.float32)).astype(ml_dtypes.bfloat16)
    shared['iota32'] = np.tile(np.arange(32, dtype=np.float32)[None, :], (128, 1))
    shared['pidx'] = np.arange(128, dtype=np.float32).reshape(128, 1)
    maps = []
    for b in range(nb):
        for r in range(split):
            m = dict(shared)
            m['x'] = np.ascontiguousarray(np.asarray(inputs['x'], np.float32)[b, r * SL:(r + 1) * SL])
            m['rope'] = np.ascontiguousarray(rope[r * SL:(r + 1) * SL])
            m['mem'] = np.ascontiguousarray(np.asarray(inputs['mem'], np.float32)[b])
            if split > 1:
                m['sel'] = np.stack([(split * b + q) * 128 + np.arange(128) for q in range(split)], axis=1).astype(np.int32)
            maps.append(m)
    return maps


def kernel(**inputs):
    x = np.asarray(inputs['x'])
    B, S, _ = x.shape
    L = np.asarray(inputs['w_in']).shape[0]
    nc = build(S, L)
    maps = make_in_maps(inputs, S, L, B)
    res = run_bass_kernel_spmd(nc, maps, core_ids=list(range(B)))
    return np.stack([np.asarray(r['out'], dtype=np.float32) for r in res.results], axis=0)
```

```python
import numpy as np
import ml_dtypes
from contextlib import ExitStack
import concourse.bass as bass
import concourse.mybir as mybir
from concourse.bass_utils import run_bass_kernel_spmd

F32 = mybir.dt.float32
BF16 = mybir.dt.bfloat16
I32 = mybir.dt.int32
AF = mybir.ActivationFunctionType
ALU = mybir.AluOpType
AX = mybir.AxisListType

D = 1024
MEM = 256
NE = 16
EPS = 1e-6
GELU = AF.Gelu_apprx_tanh
ENGS = ['tensor', 'vector', 'scalar', 'gpsimd', 'sync']
EPOCH = 20000
DEPOCH = 1200
DK = 8


class Prog:
    def __init__(self, nc, stack):
        self.nc = nc
        self.stack = stack
        self.rec = {e: [] for e in ENGS}
        self.cnt = {e: 0 for e in ENGS}
        self.esem = {e: None for e in ENGS}
        self.nsem = 0
        self.dring = {e: [None] * DK for e in ENGS}
        self.duse = {e: [0] * DK for e in ENGS}
        self.dn = {e: 0 for e in ENGS}
        self.waited = {e: {} for e in ENGS}
        self.lastw = {}
        self.readers = {}
        self.ninst = 0

    def newsem(self, tag):
        self.nsem += 1
        return self.stack.enter_context(self.nc.semaphore(f"{tag}_{self.nsem}"))

    def _wait(self, eng, tok):
        sem, val = tok[0], tok[1]
        w = self.waited[eng]
        if w.get(id(sem), 0) >= val:
            return
        w[id(sem)] = val
        self.rec[eng].append(lambda e, s=sem, v=val: e.wait_ge(s, v))

    def op(self, eng, fn, r=(), w=(), dma=False):
        toks = []
        for k in r:
            t = self.lastw.get(k)
            if t is not None:
                toks.append(t)
        for k in w:
            t = self.lastw.get(k)
            if t is not None:
                toks.append(t)
            toks.extend(self.readers.get(k, {}).values())
        for t in toks:
            if t[2] == 'tensor' and eng == 'tensor' and not dma:
                continue
            self._wait(eng, t)
        self.ninst += 1
        if dma:
            slot = self.dn[eng] % DK
            self.dn[eng] += 1
            sem = self.dring[eng][slot]
            prev = self.duse[eng][slot]
            if sem is not None and prev > 0:
                self._wait(eng, (sem, 16 * prev))
            if sem is None or prev >= DEPOCH:
                sem = self.newsem('d' + eng[:2])
                self.dring[eng][slot] = sem
                prev = 0
            self.duse[eng][slot] = prev + 1
            tok = (sem, 16 * (prev + 1), 'dma')
            self.rec[eng].append(lambda e, f=fn, s=sem: f(e).then_inc(s, 16))
        else:
            if self.esem[eng] is None or self.cnt[eng] >= EPOCH:
                self.esem[eng] = self.newsem('e' + eng[:2])
                self.cnt[eng] = 0
            self.cnt[eng] += 1
            sem = self.esem[eng]
            tok = (sem, self.cnt[eng], eng)
            self.rec[eng].append(lambda e, f=fn, s=sem: f(e).then_inc(s, 1))
        for k in r:
            self.readers.setdefault(k, {})[id(tok[0])] = tok
        for k in w:
            self.lastw[k] = tok
            self.readers[k] = {}
        return tok

    def wait_all(self, eng, keys):
        for k in keys:
            t = self.lastw.get(k)
            if t is not None:
                self._wait(eng, t)

    def barrier(self):
        toks = []
        for e in ENGS:
            if self.esem[e] is not None and self.cnt[e] > 0:
                toks.append((self.esem[e], self.cnt[e], e))
            for slot in range(DK):
                sem = self.dring[e][slot]
                if sem is not None and self.duse[e][slot] > 0:
                    toks.append((sem, 16 * self.duse[e][slot], 'dma'))
        for e in ENGS:
            for t in toks:
                self._wait(e, t)

    def emit(self):
        nc = self.nc
        with nc.Block() as block:
            @block.tensor
            def _(e):
                for f in self.rec['tensor']:
                    f(e)

            @block.vector
            def _(e):
                for f in self.rec['vector']:
                    f(e)

            @block.scalar
            def _(e):
                for f in self.rec['scalar']:
                    f(e)

            @block.gpsimd
            def _(e):
                for f in self.rec['gpsimd']:
                    f(e)

            @block.sync
            def _(e):
                for f in self.rec['sync']:
                    f(e)


class Ctx:
    pass


def build(S, L, dbg=False, phases=('B', 'C', 'A', 'D', 'E'), rg=None):
    NT = S // 128
    NG = S // 512
    CAP = 2 * S // NE
    nc = bass.Bass("TRN2", target_bir_lowering=False)
    stack = ExitStack()
    P = Prog(nc, stack)
    c = Ctx()
    c.nc, c.P, c.S, c.L, c.NT, c.NG, c.CAP = nc, P, S, L, NT, NG, CAP

    def din(name, shape, dt=F32):
        return nc.dram_tensor(name, list(shape), dt, kind="ExternalInput").ap()

    c.x_in = din('x', [S, D])
    c.mem = din('mem', [MEM, D])
    c.out = nc.dram_tensor('out', [S, D], F32, kind="ExternalOutput").ap()
    c.final_g = din('final_norm_g', [1, D])

    def sb(name, shape, dt=F32):
        return nc.alloc_sbuf_tensor(name, list(shape), dt)

    def ps(name, shape, dt=F32):
        return nc.alloc_psum_tensor(name, list(shape), dt)

    c.sb, c.ps = sb, ps

    def dma(out, in_, r, w, q='sync', **kw):
        return P.op(q, lambda e: e.dma_start(out=out, in_=in_, **kw), r, w, dma=True)

    def act(out, in_, func, r, w, **kw):
        return P.op('scalar', lambda e: e.activation(out=out, in_=in_, func=func, **kw), r, w)

    def mm(out, lhsT, rhs, start, stop, r, w):
        return P.op('tensor', lambda e: e.matmul(out, lhsT, rhs, start=start, stop=stop), r, w)

    def tr(out, in_, ident, r, w):
        return P.op('tensor', lambda e: e.transpose(out, in_, ident), r, w)

    def V(fn, r, w, eng='vector'):
        return P.op(eng, fn, r, w)

    c.dma, c.act, c.mm, c.tr, c.V = dma, act, mm, tr, V

    def rstd_from_ssq(out, ssq, n, r, w, eng='vector'):
        V(lambda e: e.tensor_scalar(out=out, in0=ssq, scalar1=1.0 / n, scalar2=EPS,
                                    op0=ALU.mult, op1=ALU.add), r, w)
        P.op('scalar', lambda e: e.sqrt(out=out, in_=out), w, w)
        V(lambda e: e.reciprocal(out=out, in_=out), w, w)

    c.rstd_from_ssq = rstd_from_ssq

    NTK = NT * (2 if rg else 1)
    SK = NTK * 128
    CAP = 2 * SK // NE
    def dram(name, shape, dt):
        return nc.dram_tensor(name, list(shape), dt, kind=("ExternalOutput" if dbg else "Internal")).ap()

    X = dram('Xs', [S, D], F32)
    GMT = dram('GMT', [4, 128, S], BF16)
    QT = dram('QT', [4, 128, S], BF16)
    H3T = dram('H3T', [8, 128, S], BF16)
    H3R = dram('H3R', [S, D], BF16)
    AFFD = dram('AFFD', [S, 16], F32)
    LISTD = dram('LISTD', [16, CAP], I32)
    M8 = CAP // 128
    NA = CAP // 32
    if rg:
        KTD = nc.dram_tensor('KTD', [128, S], BF16, kind='Internal').ap()
        NR = len(rg[0])
        KTA = nc.dram_tensor('KTA', [NR * 128, S], BF16, kind='Internal').ap()
        VAD = nc.dram_tensor('VAD', [128, NT * 130], BF16, kind='Internal').ap()
        VAA = nc.dram_tensor('VAA', [NR * 128, NT * 130], BF16, kind='Internal').ap()
        AFD = nc.dram_tensor('AFD', [128, NT * 16], F32, kind='Internal').ap()
        AFA = nc.dram_tensor('AFA', [NR * 128, NT * 16], F32, kind='Internal').ap()
        sel_in = din('sel', [128, 2], I32)
        SEL = sb('sel_s', [128, 2], I32)
        dma(SEL[:], sel_in, [], ['sel'])

    def gather_rows(out_ap, src_ap, r_, r, w):
        return P.op('gpsimd', lambda e: e.indirect_dma_start(
            out=out_ap, out_offset=None, in_=src_ap,
            in_offset=bass.IndirectOffsetOnAxis(ap=SEL[:, r_:r_ + 1], axis=0)), list(r) + ['sel'], w, dma=True)

    def allgather(out_ap, in_ap, r, w):
        return P.op('gpsimd', lambda e: e.collective_compute('AllGather', op=ALU.bypass, replica_groups=rg,
                                                             ins=[in_ap], outs=[out_ap]), r, w, dma=True)
    W = {}
    for nm, shp in [('mix_norm_g', [L, D]), ('w_in', [L, D, 1792]), ('gm_v_norm_g', [L, 512]),
                    ('gm_w_s', [L, 4, 128, 128]), ('gm_b_s', [L, 4, 128]), ('q_norm_g', [L, 64]),
                    ('k_norm_g', [L, 64]), ('branch_norm_g', [L, 2, 512]), ('w_out', [L, D, D]),
                    ('xattn_norm_g', [L, D]), ('mem_norm_g', [1, D]), ('xattn_w_q', [L, D, D]),
                    ('xattn_w_kv', [L, D, 2 * D]), ('xattn_w_o', [L, D, D]), ('ffn_norm_g', [L, D]),
                    ('w_router', [L, D, NE]), ('w_gate', [L, NE, D, D]), ('w_up', [L, NE, D, D]),
                    ('w_down', [L, NE, D, D]), ('rope', [S, 128]), ('identf', [128, 128])]:
        W[nm] = din(nm, shp)
    W['identb'] = din('identb', [128, 128], BF16)
    W['triu'] = din('triu', [128, 128], BF16)
    W['iota32'] = din('iota32', [128, 32])
    W['pidx'] = din('pidx', [128, 1])

    F4 = [sb(f'F4{i}', [128, 1024]) for i in range(4)]
    F2 = [sb(f'F2{i}', [128, 512]) for i in range(5)]
    H8 = [sb(f'H8{i}', [128, 4096], BF16) for i in range(3)]
    H4 = [sb(f'H4{i}', [128, 2048], BF16) for i in range(2)]
    H2 = [sb(f'H2{i}', [128, 1024], BF16) for i in range(4)]
    WBt = sb('WB', [128, 4 * 8192], BF16)
    STG = [sb(f'STG{i}', [128, 2048]) for i in range(2)]
    RES = sb('RES', [128, 8704])
    SM = sb('SM', [128, 256])
    identb = sb('identb_s', [128, 128], BF16)
    identf = sb('identf_s', [128, 128])
    onesf = sb('onesf', [128, 128])
    onesb = sb('onesb', [128, 128], BF16)
    GV = sb('gvbc', [128, 512])
    GQ = sb('gqbc', [128, 64])
    GK = sb('gkbc', [128, 64])
    CV = sb('colv', [128, 64])
    ROPE = [sb(f'rope{i}', [128, 128]) for i in range(2)]
    PBW = [ps(f'PBW{i}', [128, 1024]) for i in range(4)]
    PB = [PBW[i // 2][:, (i % 2) * 512:(i % 2 + 1) * 512] for i in range(8)]

    def WB(k):
        return WBt[:, k * 8192:(k + 1) * 8192]

    KT = RES[:, 0:SK // 2].bitcast(BF16)
    VAUG = RES[:, SK // 2:SK // 2 + NTK * 65].bitcast(BF16).rearrange("p (t h d) -> p t h d", t=NTK, h=2)
    AFF = RES[:, 0:NT * 16].rearrange("p (t e) -> p t e", e=16)
    MG = RES[:, NT * 16:2 * NT * 16].rearrange("p (t e) -> p t e", e=16)
    AFFA = RES[:, 2 * NT * 16:2 * NT * 16 + NTK * 16].rearrange("p (t e) -> p t e", e=16) if rg else AFF
    RSTDGM = SM[:, 0:NT]

    dma(identb[:], W['identb'], [], ['identb'])
    dma(identf[:], W['identf'], [], ['identf'])
    triu = sb('triu_s', [128, 128], BF16)
    IOTA = sb('iota_s', [128, 32])
    PIDX = sb('pidx_s', [128, 1])
    IDXt = sb('idx_s', [128, 128], I32)
    GAt = sb('ga_s', [128, 128])
    dma(triu[:], W['triu'], [], ['triu'])
    dma(IOTA[:], W['iota32'], [], ['iota'])
    dma(PIDX[:], W['pidx'], [], ['pidx'])
    V(lambda e: e.memset(onesf[:], 1.0), [], ['onesf'], 'gpsimd')
    V(lambda e: e.memset(onesb[:], 1.0), [], ['onesb'], 'gpsimd')
    for i in range(0, S, 1024):
        j = min(S, i + 1024)
        dma(X[i:j, :], c.x_in[i:j, :], [], ['X'])

    stg_n = [0]

    def load_w(dst3, dkeys, src2, KC, N, rowscale=None, mul=1.0, part=128, rkeys=(), eng='gpsimd'):
        cols = max(1, 2048 // KC)
        for n0 in range(0, N, cols):
            n1 = min(N, n0 + cols)
            k = stg_n[0] % 2
            stg_n[0] += 1
            stv = STG[k][0:part, 0:KC * (n1 - n0)].rearrange("p (c n) -> p c n", c=KC)
            dma(stv, src2[:, n0:n1].rearrange("(c p) n -> p c n", p=part), [], [f'STG{k}'])
            if rowscale is None and eng == 'scalar':
                act(dst3[:, :, n0:n1], stv, AF.Copy, [f'STG{k}'], dkeys)
            elif rowscale is None:
                V(lambda e, stv=stv, n0=n0, n1=n1: e.tensor_copy(out=dst3[:, :, n0:n1], in_=stv),
                  [f'STG{k}'], dkeys, 'gpsimd')
            else:
                for cc in range(KC):
                    V(lambda e, stv=stv, n0=n0, n1=n1, cc=cc: e.tensor_scalar(
                        out=dst3[:, cc, n0:n1], in0=stv[:, cc, :], scalar1=rowscale[:, cc:cc + 1],
                        scalar2=mul, op0=ALU.mult, op1=ALU.mult),
                      [f'STG{k}'] + list(rkeys), dkeys, 'gpsimd')

    def colvec(dst, src1d, p, key):
        P.op('sync', lambda e: e.dma_start(out=dst, in_=src1d.rearrange("(c p) -> p c", p=p),
                                           allow_slow_non_contiguous=True), [], [key], dma=True)

    tb_n = [0]

    def norm_T(xt, xkey, dst3, dkeys, hbuf, hkey, gain_bc=None, gkey=None, tbanks=(0, 1)):
        ssq = SM[:, 128:129]
        rs = SM[:, 129:130]
        act(hbuf[:, 0:1024], xt, AF.Square, [xkey], [hkey, 'ssq'], accum_out=ssq)
        rstd_from_ssq(rs, ssq, D, ['ssq'], ['rs'])
        act(hbuf[:, 0:1024], xt, AF.Copy, [xkey, 'rs'], [hkey], scale=rs)
        if gain_bc is not None:
            V(lambda e: e.tensor_tensor(out=hbuf[:, 0:1024], in0=hbuf[:, 0:1024], in1=gain_bc, op=ALU.mult),
              [hkey, gkey], [hkey])
        bk = tbanks[tb_n[0] % len(tbanks)]
        tb_n[0] += 1
        pt = PB[bk][:].bitcast(BF16).rearrange("p (c n) -> p c n", c=8)
        for cc in range(8):
            tr(pt[:, cc, :], hbuf[:, cc * 128:(cc + 1) * 128], identb[:], [hkey, 'identb'], [f'PB{bk}'])
        V(lambda e: e.tensor_copy(out=dst3, in_=pt), [f'PB{bk}'], dkeys)

    def phase_B(l):
        colvec(CV[:, 0:8], W['mix_norm_g'][l], 128, 'cv_mix')
        WIN = WBt[:, 2 * 8192:2 * 8192 + 8 * 1792].rearrange("p (c n) -> p c n", c=8)
        load_w(WIN, ['WB2', 'WB3'], W['w_in'][l], 8, 1792, rowscale=CV[:, 0:8], rkeys=['cv_mix'])
        dma(GV[:], W['gm_v_norm_g'][l:l + 1, :].partition_broadcast(128), [], ['gvbc'])
        dma(GQ[:], W['q_norm_g'][l:l + 1, :].partition_broadcast(128), [], ['gqbc'])
        dma(GK[:], W['k_norm_g'][l:l + 1, :].partition_broadcast(128), [], ['gkbc'])
        V(lambda e: e.tensor_scalar(out=GQ[:], in0=GQ[:], scalar1=0.125, scalar2=None, op0=ALU.mult),
          ['gqbc'], ['gqbc'])
        P.op('sync', lambda e: e.dma_start(out=CV[:, 8:12], in_=W['gm_b_s'][l].rearrange("g i -> i g"),
                                           allow_slow_non_contiguous=True), [], ['cv_bs'], dma=True)
        WST = H8[2][:, 0:512].rearrange("p (g i) -> p g i", g=4)
        for g in range(4):
            dma(F2[4][:, g * 128:(g + 1) * 128], W['gm_w_s'][l, g], [], ['F24'])
        V(lambda e: e.tensor_copy(out=H8[2][:, 512:1024], in_=F2[4][:]), ['F24'], ['H82s'])
        ptw = PB[0][:].bitcast(BF16)
        for g in range(4):
            tr(ptw[:, g * 128:(g + 1) * 128], H8[2][:, 512 + g * 128:512 + (g + 1) * 128], identb[:], ['H82s', 'identb'], ['PB0'])
        V(lambda e: e.tensor_copy(out=H8[2][:, 0:512], in_=ptw[:, 0:512]), ['PB0'], ['H82w'])
        V(lambda e: e.memset(VAUG[:, :, :, 64:65], 1.0), [], ['VAUG'], 'gpsimd')

        for i in range(NT):
            b = i % 2
            xt, xk = F4[b], f'F4{b}'
            dma(xt[:], X[i * 128:(i + 1) * 128, :], ['X'], [xk])
            dma(ROPE[b][:], W['rope'][i * 128:(i + 1) * 128, :], [], [f'rope{b}'])
            hT = H2[b][:].rearrange("p (c n) -> p c n", c=8)
            norm_T(xt[:], xk, hT, [f'H2{b}'], H4[0], 'H40')
            for gi, (c0, c1, bk) in enumerate([(0, 512, 2), (512, 1024, 3), (1024, 1536, 4), (1536, 1792, 5)]):
                for cc in range(8):
                    mm(PB[bk][:, 0:c1 - c0], hT[:, cc, :], WIN[:, cc, c0:c1], cc == 0, cc == 7,
                       [f'H2{b}', 'WB2', 'WB3'], [f'PB{bk}'])
            GU, GVt, GM = F2[0], F2[1], F2[2]
            act(GU[:], PB[2][:], GELU, ['PB2'], ['F20'])
            act(GVt[:], PB[3][:], GELU, ['PB3'], ['F21'])
            act(F2[3][:], GVt[:], AF.Square, ['F21'], ['F23', 'ssqv'], accum_out=SM[:, 130:131])
            rstd_from_ssq(SM[:, 131:132], SM[:, 130:131], 512, ['ssqv'], ['rsv'])
            VN = H4[1][:, 0:512]
            V(lambda e: e.scalar_tensor_tensor(out=VN, in0=GVt[:], scalar=SM[:, 131:132], in1=GV[:],
                                               op0=ALU.mult, op1=ALU.mult), ['F21', 'rsv', 'gvbc'], ['H41'])
            for g in range(4):
                mm(PB[6][:, g * 128:(g + 1) * 128], WST[:, g, :], VN[:, g * 128:(g + 1) * 128], True, True,
                   ['H82w', 'H41'], ['PB6'])
            for g in range(4):
                V(lambda e, g=g: e.scalar_tensor_tensor(
                    out=GM[:, g * 128:(g + 1) * 128], in0=PB[6][:, g * 128:(g + 1) * 128], scalar=CV[:, 8 + g:9 + g],
                    in1=GU[:, g * 128:(g + 1) * 128], op0=ALU.add, op1=ALU.mult), ['PB6', 'cv_bs', 'F20'], ['F22'])
            act(F2[3][:], GM[:], AF.Square, ['F22'], ['F23', 'ssqg'], accum_out=SM[:, 132:133])
            rstd_from_ssq(RSTDGM[:, i:i + 1], SM[:, 132:133], 512, ['ssqg'], ['rstdgm'])
            GMB = H4[1][:, 512:1024]
            act(GMB, GM[:], AF.Copy, ['F22'], ['H41b'])
            QN = F2[3]
            act(F2[4][:], PB[4][:], AF.Square, ['PB4'], ['F24'])
            V(lambda e: e.tensor_reduce(out=SM[:, 136:144], in_=F2[4][:].rearrange("p (h d) -> p h d", h=8),
                                        axis=AX.X, op=ALU.add), ['F24'], ['ssqq'])
            rstd_from_ssq(SM[:, 136:144], SM[:, 136:144], 64, ['ssqq'], ['ssqq'])
            V(lambda e: e.tensor_tensor(out=QN[:].rearrange("p (h d) -> p h d", h=8),
                                        in0=PB[4][:].rearrange("p (h d) -> p h d", h=8),
                                        in1=SM[:, 136:144].unsqueeze(2).to_broadcast([128, 8, 64]), op=ALU.mult),
              ['PB4', 'ssqq'], ['F23'])
            V(lambda e: e.tensor_tensor(out=QN[:].rearrange("p (h d) -> p h d", h=8),
                                        in0=QN[:].rearrange("p (h d) -> p h d", h=8),
                                        in1=GQ[:].unsqueeze(1).to_broadcast([128, 8, 64]), op=ALU.mult),
              ['F23', 'gqbc'], ['F23'])
            rp = ROPE[b]
            rk = f'rope{b}'

            def rope(src, nh, dst_view, skey, dkey, TT, tkey, rp=rp, rk=rk):
                s4 = src.rearrange("p (h i two) -> p h i two", h=nh, two=2)
                t4 = TT.rearrange("p (h i two) -> p h i two", h=nh, two=2)
                V(lambda e: e.tensor_tensor(out=t4[:, :, :, 0], in0=s4[:, :, :, 1],
                                            in1=rp[:, 64:96].unsqueeze(1).to_broadcast([128, nh, 32]), op=ALU.mult),
                  [skey, rk], [tkey])
                V(lambda e: e.tensor_tensor(out=t4[:, :, :, 1], in0=s4[:, :, :, 0],
                                            in1=rp[:, 96:128].unsqueeze(1).to_broadcast([128, nh, 32]), op=ALU.mult),
                  [skey, rk], [tkey])
                s3 = src.rearrange("p (h d) -> p h d", h=nh)
                V(lambda e: e.tensor_tensor(out=s3, in0=s3, in1=rp[:, 0:64].unsqueeze(1).to_broadcast([128, nh, 64]),
                                            op=ALU.mult), [skey, rk], [skey])
                if nh == 8:
                    a4 = src.rearrange("p (hh j d) -> p hh j d", hh=2, j=4)
                    b4 = TT.rearrange("p (hh j d) -> p hh j d", hh=2, j=4)
                else:
                    a4 = s3
                    b4 = TT.rearrange("p (h d) -> p h d", h=nh)
                V(lambda e: e.tensor_tensor(out=dst_view, in0=a4, in1=b4, op=ALU.add), [skey, tkey], [dkey])

            QR = H4[0][:, 1024:1536]
            rope(QN[:], 8, QR.rearrange("p (j hh d) -> p hh j d", j=4, hh=2), 'F23', 'H40q', F2[4][:], 'F24')
            KN = F2[4][:, 0:128]
            act(F2[3][:, 0:128], PB[5][:, 0:128], AF.Square, ['PB5', 'H40q'], ['F23'])
            V(lambda e: e.tensor_reduce(out=SM[:, 144:146], in_=F2[3][:, 0:128].rearrange("p (h d) -> p h d", h=2),
                                        axis=AX.X, op=ALU.add), ['F23'], ['ssqk'])
            rstd_from_ssq(SM[:, 144:146], SM[:, 144:146], 64, ['ssqk'], ['ssqk'])
            V(lambda e: e.tensor_tensor(out=KN.rearrange("p (h d) -> p h d", h=2),
                                        in0=PB[5][:, 0:128].rearrange("p (h d) -> p h d", h=2),
                                        in1=SM[:, 144:146].unsqueeze(2).to_broadcast([128, 2, 64]), op=ALU.mult),
              ['PB5', 'ssqk'], ['F24'])
            V(lambda e: e.tensor_tensor(out=KN.rearrange("p (h d) -> p h d", h=2),
                                        in0=KN.rearrange("p (h d) -> p h d", h=2),
                                        in1=GK[:].unsqueeze(1).to_broadcast([128, 2, 64]), op=ALU.mult),
              ['F24', 'gkbc'], ['F24'])
            KR = H4[0][:, 1536:1664]
            rope(KN, 2, KR.rearrange("p (h d) -> p h d", h=2), 'F24', 'H40k', F2[3][:, 128:256], 'F23')
            V(lambda e, i=i: e.tensor_copy(out=VAUG[:, i, :, 0:64],
                                           in_=PB[5][:, 128:256].rearrange("p (h d) -> p h d", h=2)),
              ['PB5'], ['VAUG'])
            pt = PB[7][:].bitcast(BF16)
            for g in range(4):
                tr(pt[:, g * 128:(g + 1) * 128], GMB[:, g * 128:(g + 1) * 128], identb[:], ['H41b', 'identb'], ['PB7'])
            for j in range(4):
                tr(pt[:, 512 + j * 128:512 + (j + 1) * 128], QR[:, j * 128:(j + 1) * 128], identb[:],
                   ['H40q', 'identb'], ['PB7'])
            TS = H4[b][:, 0:0]
            OUTS = H2[2 + b]
            ok = f'H2{2 + b}'
            if i == 0:
                pass
            V(lambda e, OUTS=OUTS: e.tensor_copy(out=OUTS[:], in_=pt), ['PB7'], [ok])
            dma(GMT[:, :, i * 128:(i + 1) * 128].rearrange("c p n -> p c n"),
                OUTS[:, 0:512].rearrange("p (c n) -> p c n", c=4), [ok], ['GMT'])
            dma(QT[:, :, i * 128:(i + 1) * 128].rearrange("c p n -> p c n"),
                OUTS[:, 512:1024].rearrange("p (c n) -> p c n", c=4), [ok], ['QT'])
            bk = 'PB6'
            ptk = PB[6][:].bitcast(BF16)
            tr(ptk[:, 0:128], KR, identb[:], ['H40k', 'identb'], ['PB6'])
            V(lambda e, i=i: e.tensor_copy(out=KT[:, i * 128:(i + 1) * 128], in_=ptk[:, 0:128]), ['PB6'], ['KT'])

    def exchange_kv():
        if not rg:
            return
        dma(KTD, KT[:, 0:S], ['KT'], ['KTD'])
        dma(VAD, RES[:, SK // 2:SK // 2 + NT * 65].bitcast(BF16), ['VAUG'], ['VAD'])
        allgather(KTA, KTD, ['KTD'], ['KTA'])
        allgather(VAA, VAD, ['VAD'], ['VAA'])
        for r_ in range(2):
            gather_rows(KT[:, r_ * S:(r_ + 1) * S], KTA, r_, ['KTA'], ['KT'])
            gather_rows(RES[:, SK // 2 + r_ * NT * 65:SK // 2 + (r_ + 1) * NT * 65].bitcast(BF16), VAA, r_,
                        ['VAA'], ['VAUG'])

    def phase_C(l):
        exchange_kv()
        colvec(CV[:, 16:20], W['branch_norm_g'][l, 0], 128, 'cv_b0')
        colvec(CV[0:64, 20:28], W['branch_norm_g'][l, 1], 64, 'cv_b1')
        WO0 = WB(0)[:, 0:4096].rearrange("p (c n) -> p c n", c=4)
        WO1 = WB(1)[0:64, :].rearrange("p (c n) -> p c n", c=8)
        load_w(WO0, ['WB0'], W['w_out'][l, 0:512, :], 4, 1024, rowscale=CV[:, 16:20], rkeys=['cv_b0'])
        load_w(WO1, ['WB1'], W['w_out'][l, 512:1024, :], 8, 1024, rowscale=CV[0:64, 20:28], rkeys=['cv_b1'], part=64)
        NKP = NTK // 2
        GS = min(512, S)
        NGG = S // GS
        for g in range(NGG):
            t0 = g * GS
            QM = H8[2][:, 0:8 * GS].rearrange("p (h n) -> p h n", h=8)
            qk = 'H82'
            if g == 0:
                V(lambda e: e.memset(H8[2][:, 0:8 * GS], 0.0), [], ['H82'], 'gpsimd')
            dma(QM[0:64, 0:4, :], QT[:, 0:64, t0:t0 + GS].rearrange("c p n -> p c n"), ['QT'], [qk])
            dma(QM[64:128, 4:8, :], QT[:, 64:128, t0:t0 + GS].rearrange("c p n -> p c n"), ['QT'], [qk])
            ATT = H8[0][0:64, 0:8 * GS].rearrange("p (h n) -> p h n", h=8)
            SQACC = F2[4][0:64, 0:GS]
            steps = [(h, kp) for h in range(8) for kp in range(NKP)]

            def emit_qk(h, kp):
                j, hh = h % 4, h // 4
                sbk = (kp % 2) * 2
                for k2 in range(2):
                    kt = kp * 2 + k2
                    mm(PB[sbk + k2][:, 0:GS], KT[:, kt * 128:(kt + 1) * 128],
                       QM[:, h, :], True, True, ['KT', qk], [f'PB{sbk + k2}'])

            def emit_exp_pv(h, kp):
                hh = h // 4
                ob = 4 + (h % 2)
                OT = PB[ob]
                sbk = (kp % 2) * 2
                PT = H2[kp % 3]
                pk = f'H2{kp % 3}'
                if GS == 512:
                    act(PT[:, 0:1024], PBW[kp % 2][:, 0:1024], AF.Exp, [f'PB{sbk}', f'PB{sbk + 1}'], [pk])
                else:
                    for k2 in range(2):
                        act(PT[:, k2 * 512:k2 * 512 + GS], PB[sbk + k2][:, 0:GS], AF.Exp, [f'PB{sbk + k2}'], [pk])
                for k2 in range(2):
                    kt = kp * 2 + k2
                    mm(OT[0:65, 0:GS], VAUG[:, kt, hh, :], PT[:, k2 * 512:k2 * 512 + GS],
                       kt == 0, kt == NTK - 1, ['VAUG', pk], [f'PB{ob}'])

            def finalize(h):
                ob = 4 + (h % 2)
                OT = PB[ob]
                SR = F2[3]
                V(lambda e, OT=OT: e.tensor_copy(out=SR[64:65, 0:GS], in_=OT[64:65, 0:GS]), [f'PB{ob}'], ['F23'])
                mm(PB[6][0:64, 0:GS], onesf[64:65, 0:64], SR[64:65, 0:GS], True, True, ['onesf', 'F23'], ['PB6'])
                Rr = F2[2]
                V(lambda e: e.reciprocal(out=Rr[0:64, 0:GS], in_=PB[6][0:64, 0:GS]), ['PB6'], ['F22'])
                V(lambda e, OT=OT, h=h: e.tensor_tensor(out=ATT[:, h, :], in0=OT[0:64, 0:GS], in1=Rr[0:64, 0:GS],
                                                        op=ALU.mult), [f'PB{ob}', 'F22'], ['H80'])
                if h == 0:
                    V(lambda e, h=h: e.tensor_tensor(out=SQACC, in0=ATT[:, h, :], in1=ATT[:, h, :], op=ALU.mult),
                      ['H80'], ['F24'], 'gpsimd')
                else:
                    V(lambda e, h=h: e.tensor_tensor(out=F2[1][0:64, 0:GS], in0=ATT[:, h, :], in1=ATT[:, h, :],
                                                     op=ALU.mult), ['H80'], ['F21'], 'gpsimd')
                    V(lambda e: e.tensor_tensor(out=SQACC, in0=SQACC, in1=F2[1][0:64, 0:GS], op=ALU.add),
                      ['F21', 'F24'], ['F24'], 'gpsimd')

            emit_qk(*steps[0])
            pending = None
            for si, (h, kp) in enumerate(steps):
                if si + 1 < len(steps):
                    emit_qk(*steps[si + 1])
                emit_exp_pv(h, kp)
                if pending is not None and kp == 0:
                    finalize(pending)
                    pending = None
                if kp == NKP - 1:
                    pending = h
            finalize(pending)
            mm(PB[6][0:1, 0:GS], onesf[0:64, 0:1], SQACC, True, True, ['onesf', 'F24'], ['PB6'])
            V(lambda e: e.tensor_copy(out=F2[3][0:1, 0:GS], in_=PB[6][0:1, 0:GS]), ['PB6'], ['F23'])
            for tt in range(GS // 128):
                mm(PB[6][:, tt:tt + 1], F2[3][0:1, tt * 128:(tt + 1) * 128], onesf[0:1, 0:1], True, True,
                   ['F23', 'onesf'], ['PB6'])
            rstd_from_ssq(SM[:, 148:148 + GS // 128], PB[6][:, 0:GS // 128], 512, ['PB6'], ['rsat'])
            gmTg = H8[1][:, 0:4 * GS].rearrange("p (c n) -> p c n", c=4)
            dma(gmTg, GMT[:, :, t0:t0 + GS].rearrange("c p n -> p c n"), ['GMT'], ['H81'])
            for tt in range(GS // 128):
                i = g * (GS // 128) + tt
                b = tt % 2
                xt, xk = F4[b], f'F4{b}'
                dma(xt[:], X[i * 128:(i + 1) * 128, :], ['X'], [xk])
                for half in range(2):
                    hs = slice(half * 512, (half + 1) * 512)
                    for cc in range(4):
                        mm(PB[6][:], gmTg[:, cc, tt * 128:(tt + 1) * 128], WO0[:, cc, hs], cc == 0, cc == 3,
                           ['H81', 'WB0'], ['PB6'])
                    V(lambda e, xt=xt, hs=hs, i=i: e.scalar_tensor_tensor(
                        out=xt[:, hs], in0=PB[6][:], scalar=RSTDGM[:, i:i + 1], in1=xt[:, hs],
                        op0=ALU.mult, op1=ALU.add), ['PB6', 'rstdgm', xk], [xk])
                    for h in range(8):
                        mm(PB[7][:], ATT[:, h, tt * 128:(tt + 1) * 128], WO1[:, h, hs], h == 0, h == 7,
                           ['H80', 'WB1'], ['PB7'])
                    V(lambda e, xt=xt, hs=hs, tt=tt: e.scalar_tensor_tensor(
                        out=xt[:, hs], in0=PB[7][:], scalar=SM[:, 148 + tt:149 + tt], in1=xt[:, hs],
                        op0=ALU.mult, op1=ALU.add), ['PB7', 'rsat', xk], [xk])
                dma(X[i * 128:(i + 1) * 128, :], xt[:], [xk], ['X'])

    MNTt = sb('MNT', [128, 2048], BF16)
    MNT = MNTt[:].rearrange("p (c n) -> p c n", c=8)
    KXt = sb('KX', [128, 2048], BF16)
    KX = KXt[:].rearrange("p (c n) -> p c n", c=8)
    VXt = sb('VX', [128, 2048], BF16)
    VX = VXt[:].rearrange("p (m n) -> p m n", m=2)
    WRf = sb('WRf', [128, 128])

    def phase_A0():
        dma(F4[2][:], W['mem_norm_g'].partition_broadcast(128), [], ['F42'])
        for mt in range(2):
            dma(F4[mt][:], c.mem[mt * 128:(mt + 1) * 128, :], [], [f'F4{mt}'])
            norm_T(F4[mt][:], f'F4{mt}', MNT[:, :, mt * 128:(mt + 1) * 128], ['MNT'], H4[0], 'H40',
                   gain_bc=F4[2][:], gkey='F42')

    def phase_A(l):
        WK = WB(2).rearrange("p (c n) -> p c n", c=8)
        WV = WB(3).rearrange("p (c n) -> p c n", c=8)
        load_w(WK, ['WB2'], W['xattn_w_kv'][l, :, 0:1024], 8, 1024)
        load_w(WV, ['WB3'], W['xattn_w_kv'][l, :, 1024:2048], 8, 1024)
        for dc in range(8):
            bk = dc % 2
            for cc in range(8):
                mm(PB[bk][:, 0:256], WK[:, cc, dc * 128:(dc + 1) * 128], MNT[:, cc, :], cc == 0, cc == 7,
                   ['WB2', 'MNT'], [f'PB{bk}'])
            V(lambda e, dc=dc, bk=bk: e.tensor_copy(out=KX[:, dc, :], in_=PB[bk][:, 0:256]), [f'PB{bk}'], ['KX'])
        for mt in range(2):
            for half in range(2):
                bk = 2 + half
                for cc in range(8):
                    mm(PB[bk][:], MNT[:, cc, mt * 128:(mt + 1) * 128], WV[:, cc, half * 512:(half + 1) * 512],
                       cc == 0, cc == 7, ['WB3', 'MNT'], [f'PB{bk}'])
                V(lambda e, mt=mt, half=half, bk=bk: e.tensor_copy(out=VX[:, mt, half * 512:(half + 1) * 512],
                                                                   in_=PB[bk][:]), [f'PB{bk}'], ['VX'])

    def phase_D(l):
        colvec(CV[:, 32:40], W['xattn_norm_g'][l], 128, 'cv_x')
        WQ = WB(0).rearrange("p (c n) -> p c n", c=8)
        WOX = WB(1).rearrange("p (c n) -> p c n", c=8)
        load_w(WQ, ['WB0'], W['xattn_w_q'][l], 8, 1024, rowscale=CV[:, 32:40], mul=1.0 / 16.0, rkeys=['cv_x'])
        load_w(WOX, ['WB1'], W['xattn_w_o'][l], 8, 1024)
        GS = min(512, S)
        for g in range(S // GS):
            t0 = g * GS
            H2Tg = H8[1][:, 0:8 * GS].rearrange("p (c n) -> p c n", c=8)
            for tt in range(GS // 128):
                i = g * (GS // 128) + tt
                b = tt % 2
                dma(F4[b][:], X[i * 128:(i + 1) * 128, :], ['X'], [f'F4{b}'])
                norm_T(F4[b][:], f'F4{b}', H2Tg[:, :, tt * 128:(tt + 1) * 128], ['H81'], H4[0], 'H40')
            QX = H8[2][:, 0:8 * GS].rearrange("p (c n) -> p c n", c=8)
            for dc in range(8):
                bk = 2 + dc % 2
                for cc in range(8):
                    mm(PB[bk][:, 0:GS], WQ[:, cc, dc * 128:(dc + 1) * 128], H2Tg[:, cc, :], cc == 0, cc == 7,
                       ['WB0', 'H81'], [f'PB{bk}'])
                act(QX[:, dc, :], PB[bk][:, 0:GS], AF.Copy, [f'PB{bk}'], ['H82'])
            OXT = H8[0][:, 0:8 * GS].rearrange("p (c n) -> p c n", c=8)
            for h in range(4):
                PX = H2[2 + h % 2]
                pk = f'H2{2 + h % 2}'
                for mt in range(2):
                    for dd in range(2):
                        mm(PB[4 + mt][:, 0:GS], KX[:, 2 * h + dd, mt * 128:(mt + 1) * 128], QX[:, 2 * h + dd, :],
                           dd == 0, dd == 1, ['KX', 'H82'], [f'PB{4 + mt}'])
                    if GS != 512:
                        act(PX[:, mt * 512:mt * 512 + GS], PB[4 + mt][:, 0:GS], AF.Exp, [f'PB{4 + mt}'], [pk])
                if GS == 512:
                    act(PX[:, 0:1024], PBW[2][:, 0:1024], AF.Exp, ['PB4', 'PB5'], [pk])
                for mt in range(2):
                    mm(PB[6][:, 0:GS], onesb[:], PX[:, mt * 512:mt * 512 + GS], mt == 0, mt == 1,
                       ['onesb', pk], ['PB6'])
                V(lambda e: e.reciprocal(out=F2[2][:, 0:GS], in_=PB[6][:, 0:GS]), ['PB6'], ['F22'])
                for dd in range(2):
                    for mt in range(2):
                        mm(PB[7][:, 0:GS], VX[:, mt, (2 * h + dd) * 128:(2 * h + dd + 1) * 128],
                           PX[:, mt * 512:mt * 512 + GS], mt == 0, mt == 1, ['VX', pk], ['PB7'])
                    V(lambda e, h=h, dd=dd: e.tensor_tensor(out=OXT[:, 2 * h + dd, :], in0=PB[7][:, 0:GS],
                                                            in1=F2[2][:, 0:GS], op=ALU.mult),
                      ['PB7', 'F22'], ['H80'])
            for tt in range(GS // 128):
                i = g * (GS // 128) + tt
                b = tt % 2
                xt, xk = F4[2 + b], f'F4{2 + b}'
                dma(xt[:], X[i * 128:(i + 1) * 128, :], ['X'], [xk])
                for half in range(2):
                    hs = slice(half * 512, (half + 1) * 512)
                    bk = 2 + half
                    for cc in range(8):
                        mm(PB[bk][:], OXT[:, cc, tt * 128:(tt + 1) * 128], WOX[:, cc, hs], cc == 0, cc == 7,
                           ['H80', 'WB1'], [f'PB{bk}'])
                    V(lambda e, xt=xt, hs=hs, bk=bk: e.tensor_tensor(out=xt[:, hs], in0=PB[bk][:], in1=xt[:, hs],
                                                                     op=ALU.add), [f'PB{bk}', xk], [xk])
                dma(X[i * 128:(i + 1) * 128, :], xt[:], [xk], ['X'])

    def phase_E(l):
        colvec(CV[:, 40:48], W['ffn_norm_g'][l], 128, 'cv_f')
        WR3 = WRf[:].rearrange("p (c e) -> p c e", c=8)
        dma(WR3, W['w_router'][l].rearrange("(c p) e -> p c e", p=128), [], ['WRf'])
        for cc in range(8):
            V(lambda e, cc=cc: e.tensor_scalar(out=WR3[:, cc, :], in0=WR3[:, cc, :], scalar1=CV[:, 40 + cc:41 + cc],
                                               scalar2=None, op0=ALU.mult), ['WRf', 'cv_f'], ['WRf'])
        for i in range(NT):
            b = i % 2
            xt, xk = F4[b], f'F4{b}'
            dma(xt[:], X[i * 128:(i + 1) * 128, :], ['X'], [xk])
            act(F4[2][:], xt[:], AF.Square, [xk], ['F42', 'ssq'], accum_out=SM[:, 128:129])
            rstd_from_ssq(SM[:, 129:130], SM[:, 128:129], D, ['ssq'], ['rs'])
            act(F4[2][:], xt[:], AF.Copy, [xk, 'rs'], ['F42'], scale=SM[:, 129:130])
            for cc in range(8):
                bk = cc // 4
                tr(PB[bk][:, (cc % 4) * 128:(cc % 4 + 1) * 128], F4[2][:, cc * 128:(cc + 1) * 128], identf[:],
                   ['F42', 'identf'], [f'PB{bk}'])
            V(lambda e: e.tensor_copy(out=F4[3][:, 0:512], in_=PB[0][:]), ['PB0'], ['F43'])
            V(lambda e: e.tensor_copy(out=F4[3][:, 512:1024], in_=PB[1][:]), ['PB1'], ['F43'])
            for cc in range(8):
                mm(PB[2][:, 0:16], F4[3][:, cc * 128:(cc + 1) * 128], WR3[:, cc, :], cc == 0, cc == 7,
                   ['F43', 'WRf'], ['PB2'])
            V(lambda e: e.reduce_max(out=SM[:, 150:151], in_=PB[2][:, 0:16], axis=AX.X), ['PB2'], ['smx'])
            V(lambda e: e.tensor_scalar(out=SM[:, 150:151], in0=SM[:, 150:151], scalar1=-1.0, scalar2=None,
                                        op0=ALU.mult), ['smx'], ['smx'])
            act(SM[:, 160:176], PB[2][:, 0:16], AF.Exp, ['PB2', 'smx'], ['sme', 'sms'], bias=SM[:, 150:151],
                accum_out=SM[:, 151:152])
            V(lambda e: e.reciprocal(out=SM[:, 151:152], in_=SM[:, 151:152]), ['sms'], ['sms'])
            V(lambda e, i=i: e.tensor_scalar(out=AFF[:, i, :], in0=SM[:, 160:176], scalar1=SM[:, 151:152],
                                             scalar2=None, op0=ALU.mult), ['sme', 'sms'], ['AFF'])
            hb = H2[b]
            V(lambda e, hb=hb: e.tensor_copy(out=hb[:], in_=F4[2][:]), ['F42'], [f'H2{b}'])
            dma(H3R[i * 128:(i + 1) * 128, :], hb[:], [f'H2{b}'], ['H3R'])
            dma(AFFD[i * 128:(i + 1) * 128, :], AFF[:, i, :], ['AFF'], ['AFFD'])
        if rg:
            dma(AFD, RES[:, 0:NT * 16], ['AFF'], ['AFD'])
            allgather(AFA, AFD, ['AFD'], ['AFA'])
            for r_ in range(2):
                gather_rows(RES[:, 2 * NT * 16 + r_ * NT * 16:2 * NT * 16 + (r_ + 1) * NT * 16], AFA, r_,
                            ['AFA'], ['AFFA'])
        akey = 'AFFA' if rg else 'AFF'
        LO, HI, MID = SM[:, 160:176], SM[:, 176:192], SM[:, 192:208]
        PART, CC, D1 = SM[:, 208:224], SM[:, 224:240], SM[:, 240:256]
        CMPA = F4[2][:, 0:NTK * 16].rearrange("p (t e) -> p t e", e=16)
        CMP = F4[2][:, 0:NT * 16].rearrange("p (t e) -> p t e", e=16)
        V(lambda e: e.memset(LO, 0.0), ['sme'], ['lo'])
        V(lambda e: e.memset(HI, 1.0), [], ['hi'])
        for it in range(32):
            V(lambda e: e.tensor_tensor(out=MID, in0=LO, in1=HI, op=ALU.add), ['lo', 'hi'], ['mid'])
            V(lambda e: e.tensor_scalar(out=MID, in0=MID, scalar1=0.5, scalar2=None, op0=ALU.mult), ['mid'], ['mid'])
            V(lambda e: e.tensor_tensor(out=CMPA, in0=AFFA, in1=MID.unsqueeze(1).to_broadcast([128, NTK, 16]),
                                        op=ALU.is_gt), [akey, 'mid'], ['F42'])
            V(lambda e: e.tensor_reduce(out=PART, in_=CMPA.rearrange("p t e -> p e t"), axis=AX.X, op=ALU.add),
              ['F42'], ['part'])
            mm(PB[3][:, 0:16], onesf[:], PART, True, True, ['onesf', 'part'], ['PB3'])
            V(lambda e: e.tensor_scalar(out=CC, in0=PB[3][:, 0:16], scalar1=float(CAP) - 0.5, scalar2=None,
                                        op0=ALU.is_ge), ['PB3'], ['cc'])
            V(lambda e: e.tensor_tensor(out=D1, in0=MID, in1=LO, op=ALU.subtract), ['mid', 'lo'], ['d1'])
            V(lambda e: e.tensor_tensor(out=D1, in0=D1, in1=CC, op=ALU.mult), ['d1', 'cc'], ['d1'])
            V(lambda e: e.tensor_tensor(out=LO, in0=LO, in1=D1, op=ALU.add), ['d1', 'lo'], ['lo'])
            V(lambda e: e.tensor_tensor(out=D1, in0=HI, in1=MID, op=ALU.subtract), ['mid', 'hi'], ['d1'])
            V(lambda e: e.tensor_tensor(out=D1, in0=D1, in1=CC, op=ALU.mult), ['d1', 'cc'], ['d1'])
            V(lambda e: e.tensor_tensor(out=HI, in0=MID, in1=D1, op=ALU.add), ['d1', 'mid'], ['hi'])
        V(lambda e: e.tensor_tensor(out=CMP, in0=AFF, in1=LO.unsqueeze(1).to_broadcast([128, NT, 16]),
                                    op=ALU.is_gt), ['AFF', 'lo'], ['F42'])
        V(lambda e: e.tensor_tensor(out=MG, in0=CMP, in1=AFF, op=ALU.mult), ['F42', 'AFF'], ['MG'])
        NTE = NT * 16
        Mflat = F4[2][:, 0:NTE]
        Mb = H2[0][:, 0:NTE]
        V(lambda e: e.tensor_copy(out=Mb, in_=Mflat), ['F42'], ['H20'])
        W1 = F4[0][:, 0:NTE]
        NRr = F4[1][:, 0:NTE]
        nhb = (NTE + 511) // 512
        for hb in range(nhb):
            c0, c1 = hb * 512, min(NTE, (hb + 1) * 512)
            mm(PB[hb][:, 0:c1 - c0], triu[:], Mb[:, c0:c1], True, True, ['triu', 'H20'], [f'PB{hb}'])
            mm(PB[2 + hb][:, 0:c1 - c0], onesb[:], Mb[:, c0:c1], True, True, ['onesb', 'H20'], [f'PB{2 + hb}'])
            V(lambda e, hb=hb, c0=c0, c1=c1: e.tensor_copy(out=W1[:, c0:c1], in_=PB[hb][:, 0:c1 - c0]),
              [f'PB{hb}'], ['F40'])
            V(lambda e, hb=hb, c0=c0, c1=c1: e.tensor_copy(out=NRr[:, c0:c1], in_=PB[2 + hb][:, 0:c1 - c0]),
              [f'PB{2 + hb}'], ['F41'])
        cur, ck = F4[1], 'F41'
        oth, ok_ = F4[3], 'F43'
        st_ = 1
        while st_ < NT:
            c3 = cur[:, 0:NTE].rearrange("p (t e) -> p t e", e=16)
            o3 = oth[:, 0:NTE].rearrange("p (t e) -> p t e", e=16)
            V(lambda e, c3=c3, o3=o3, st_=st_: e.tensor_copy(out=o3[:, 0:st_, :], in_=c3[:, 0:st_, :]), [ck], [ok_])
            V(lambda e, c3=c3, o3=o3, st_=st_: e.tensor_tensor(out=o3[:, st_:NT, :], in0=c3[:, st_:NT, :],
                                                               in1=c3[:, 0:NT - st_, :], op=ALU.add), [ck], [ok_])
            cur, ck, oth, ok_ = oth, ok_, cur, ck
            st_ *= 2
        INC = cur[:, 0:NTE]
        V(lambda e: e.tensor_tensor(out=W1, in0=W1, in1=INC, op=ALU.add), ['F40', ck], ['F40'])
        for hb in range(nhb):
            c0, c1 = hb * 512, min(NTE, (hb + 1) * 512)
            V(lambda e, hb=hb, c0=c0, c1=c1: e.tensor_tensor(out=W1[:, c0:c1], in0=W1[:, c0:c1],
                                                             in1=PB[2 + hb][:, 0:c1 - c0], op=ALU.subtract),
              ['F40', f'PB{2 + hb}'], ['F40'])
        V(lambda e: e.tensor_tensor(out=W1, in0=W1, in1=Mflat, op=ALU.mult), ['F40', 'F42'], ['F40'])
        V(lambda e: e.tensor_scalar(out=W1, in0=W1, scalar1=-1.0, scalar2=None, op0=ALU.add), ['F40'], ['F40'])
        POSI = F4[1][:, 0:NTE].bitcast(I32)
        V(lambda e: e.tensor_copy(out=POSI, in_=W1), ['F40'], ['F41'])
        AI = F4[3][:, 0:NTE].bitcast(I32)
        BI = F4[0][:, 0:NTE].bitcast(I32)
        V(lambda e: e.tensor_single_scalar(out=AI, in_=POSI, scalar=5, op=ALU.arith_shift_right), ['F41'], ['F43'])
        V(lambda e: e.tensor_single_scalar(out=BI, in_=POSI, scalar=31, op=ALU.bitwise_and), ['F41'], ['F40'])
        AFl = F4[2][:, 0:NTE].rearrange("p (t e) -> p t e", e=16)
        BFl = F4[1][:, 0:NTE].rearrange("p (t e) -> p t e", e=16)
        V(lambda e: e.tensor_copy(out=F4[2][:, 0:NTE], in_=AI), ['F43'], ['F42'])
        V(lambda e: e.tensor_copy(out=F4[1][:, 0:NTE], in_=BI), ['F40'], ['F41'])
        for i in range(NT):
            b = i % 2
            OHa = H4[b][:, 0:16 * NA]
            OHa3 = OHa.rearrange("p (e a) -> p e a", e=16)
            OHb3 = F2[b][:, 0:512].rearrange("p (e a) -> p e a", e=16)
            Rr_ = H4[b][:, 1024:2048]
            R3 = Rr_.rearrange("p (e a) -> p e a", e=16)
            V(lambda e, i=i, OHa3=OHa3: e.tensor_tensor(
                out=OHa3, in0=AFl[:, i, :].unsqueeze(2).to_broadcast([128, 16, NA]),
                in1=IOTA[:, 0:NA].unsqueeze(1).to_broadcast([128, 16, NA]), op=ALU.is_equal),
              ['F42', 'iota'], [f'H4{b}a'])
            V(lambda e, i=i, OHb3=OHb3: e.tensor_tensor(
                out=OHb3, in0=BFl[:, i, :].unsqueeze(2).to_broadcast([128, 16, 32]),
                in1=IOTA[:, 0:32].unsqueeze(1).to_broadcast([128, 16, 32]), op=ALU.is_equal),
              ['F41', 'iota', f'H4{b}r'], [f'F2{b}'])
            V(lambda e, OHb3=OHb3, R3=R3: e.tensor_scalar(out=R3[:, :, 0:32], in0=OHb3, scalar1=PIDX[:, 0:1],
                                                          scalar2=None, op0=ALU.mult),
              [f'F2{b}', 'pidx'], [f'H4{b}r'])
            V(lambda e, OHb3=OHb3, R3=R3, i=i: e.tensor_scalar(out=R3[:, :, 32:64], in0=OHb3, scalar1=float(i),
                                                               scalar2=None, op0=ALU.mult),
              [f'F2{b}'], [f'H4{b}r'])
            for q in range(4):
                mm(PB[q][0:4 * NA, 0:256], OHa[:, q * 4 * NA:(q + 1) * 4 * NA], Rr_[:, q * 256:(q + 1) * 256],
                   i == 0, i == NT - 1, [f'H4{b}a', f'H4{b}r'], [f'PB{q}'])
        LF = F2[2][:, 0:512]
        LF4 = LF.rearrange("p (q k b) -> p q k b", q=4, k=4)
        CP = F2[3][:, 0:256]
        CP3 = CP.rearrange("p (k b) -> p k b", k=4)
        for q in range(4):
            V(lambda e, q=q: e.tensor_copy(out=CP[0:4 * NA, :], in_=PB[q][0:4 * NA, 0:256]), [f'PB{q}'], ['F23'])
            V(lambda e, q=q: e.scalar_tensor_tensor(out=LF4[0:4 * NA, q, :, :], in0=CP3[0:4 * NA, :, 32:64], scalar=128.0,
                                                    in1=CP3[0:4 * NA, :, 0:32], op0=ALU.mult, op1=ALU.add),
              ['F23'], ['F22'])
        LI = F2[4][:, 0:512].bitcast(I32)
        LI4 = LI.rearrange("p (q k b) -> p q k b", q=4, k=4)
        V(lambda e: e.tensor_copy(out=LI[0:4 * NA, :], in_=LF[0:4 * NA, :]), ['F22'], ['F24'])
        for ex in range(NE):
            q, k = ex // 4, ex % 4
            dma(LISTD[ex].rearrange("(a b) -> a b", b=32), LI4[k * NA:(k + 1) * NA, q, k, :], ['F24'], ['LISTD'])
        IDX3 = IDXt[:, 0:16 * M8].rearrange("p (e m) -> p e m", e=16)
        dma(IDX3, LISTD.rearrange("e (p m) -> p e m", m=M8), ['LISTD'], ['IDX'], allow_slow_non_contiguous=True)
        SG = min(512, CAP)
        for ex in range(NE):
            sl = [(3 * ex + k) % 4 for k in range(3)]
            WGv, WUv, WDv = [WB(s_).rearrange("p (c n) -> p c n", c=8) for s_ in sl]
            kg, ku, kd = [f'WB{s_}' for s_ in sl]
            load_w(WGv, [kg], W['w_gate'][l, ex], 8, 1024, rowscale=CV[:, 40:48], rkeys=['cv_f'])
            load_w(WUv, [ku], W['w_up'][l, ex], 8, 1024, rowscale=CV[:, 40:48], rkeys=['cv_f'])
            load_w(WDv, [kd], W['w_down'][l, ex], 8, 1024, eng='scalar')
            for sg in range(CAP // SG):
                xb = H8[sg % 2]
                xk_ = f'H8{sg % 2}'
                XST = xb[:, 0:8 * SG].rearrange("p (c n) -> p c n", c=8)
                nm = SG // 128
                for m4 in range(nm):
                    m = sg * nm + m4
                    b = m % 2
                    xs_, xsk = H2[2 + b], f'H2{2 + b}'
                    P.op('gpsimd', lambda e, xs_=xs_, ex=ex, m=m: e.indirect_dma_start(
                        out=xs_[:], out_offset=None, in_=H3R,
                        in_offset=bass.IndirectOffsetOnAxis(ap=IDX3[:, ex, m:m + 1], axis=0)),
                         ['H3R', 'IDX'], [xsk], dma=True)
                    P.op('gpsimd', lambda e, ex=ex, m=m: e.indirect_dma_start(
                        out=GAt[:, (m % 8) * 16:(m % 8) * 16 + 16], out_offset=None, in_=AFFD,
                        in_offset=bass.IndirectOffsetOnAxis(ap=IDX3[:, ex, m:m + 1], axis=0)),
                         ['AFFD', 'IDX'], [f'ga{m % 8}'], dma=True)
                    pt = PB[6 + b][:].bitcast(BF16).rearrange("p (c n) -> p c n", c=8)
                    for cc in range(8):
                        tr(pt[:, cc, :], xs_[:, cc * 128:(cc + 1) * 128], identb[:], [xsk, 'identb'], [f'PB{6 + b}'])
                    V(lambda e, pt=pt, m4=m4, XST=XST: e.tensor_copy(out=XST[:, :, m4 * 128:(m4 + 1) * 128], in_=pt),
                      [f'PB{6 + b}'], [xk_])
                ACTT = H8[2][:, 0:8 * SG].rearrange("p (c n) -> p c n", c=8)
                for fc in range(8):
                    ba, bu = fc % 2, 2 + fc % 2
                    for cc in range(8):
                        mm(PB[ba][:, 0:SG], WGv[:, cc, fc * 128:(fc + 1) * 128], XST[:, cc, :], cc == 0, cc == 7,
                           [kg, xk_], [f'PB{ba}'])
                    for cc in range(8):
                        mm(PB[bu][:, 0:SG], WUv[:, cc, fc * 128:(fc + 1) * 128], XST[:, cc, :], cc == 0, cc == 7,
                           [ku, xk_], [f'PB{bu}'])
                    SA = F2[fc % 2]
                    act(SA[:, 0:SG], PB[ba][:, 0:SG], AF.Silu, [f'PB{ba}'], [f'F2{fc % 2}'])
                    V(lambda e, fc=fc, SA=SA, bu=bu, ACTT=ACTT: e.tensor_tensor(out=ACTT[:, fc, :], in0=PB[bu][:, 0:SG],
                                                                               in1=SA[:, 0:SG], op=ALU.mult),
                      [f'PB{bu}', f'F2{fc % 2}'], ['H82'])
                for m4 in range(nm):
                    m = sg * nm + m4
                    ys = F4[m % 2]
                    yk = f'F4{m % 2}'
                    for half in range(2):
                        hs = slice(half * 512, (half + 1) * 512)
                        bk = 4 + half
                        for fc in range(8):
                            mm(PB[bk][:], ACTT[:, fc, m4 * 128:(m4 + 1) * 128], WDv[:, fc, hs], fc == 0, fc == 7,
                               ['H82', kd], [f'PB{bk}'])
                        V(lambda e, ys=ys, hs=hs, bk=bk, m=m, ex=ex: e.tensor_scalar(
                            out=ys[:, hs], in0=PB[bk][:], scalar1=GAt[:, (m % 8) * 16 + ex:(m % 8) * 16 + ex + 1],
                            scalar2=None, op0=ALU.mult), [f'PB{bk}', f'ga{m % 8}'], [yk])
                    P.op('gpsimd', lambda e, ys=ys, ex=ex, m=m: e.indirect_dma_start(
                        out=X, out_offset=bass.IndirectOffsetOnAxis(ap=IDX3[:, ex, m:m + 1], axis=0),
                        in_=ys[:], in_offset=None, compute_op=ALU.add), [yk, 'IDX'], ['X'], dma=True)

    def phase_F():
        dma(F4[2][:], c.final_g.partition_broadcast(128), [], ['F42'])
        for i in range(NT):
            b = i % 2
            xt, xk = F4[b], f'F4{b}'
            dma(xt[:], X[i * 128:(i + 1) * 128, :], ['X'], [xk])
            act(F4[3][:], xt[:], AF.Square, [xk], ['F43', 'ssq'], accum_out=SM[:, 128:129])
            rstd_from_ssq(SM[:, 129:130], SM[:, 128:129], D, ['ssq'], ['rs'])
            yt = H8[b][:].bitcast(F32)[:, 0:1024]
            V(lambda e, xt=xt, yt=yt: e.scalar_tensor_tensor(out=yt, in0=xt[:], scalar=SM[:, 129:130],
                                                             in1=F4[2][:], op0=ALU.mult, op1=ALU.mult),
              [xk, 'rs', 'F42'], [f'H8{b}'])
            dma(c.out[i * 128:(i + 1) * 128, :], yt, [f'H8{b}'], ['OUT'])

    c.phases = dict(B=phase_B, C=phase_C, A=phase_A, D=phase_D, E=phase_E)
    phase_A0()
    P.barrier()
    for l in range(L):
        for ph in phases:
            if ph in c.phases:
                c.phases[ph](l)
                P.barrier()
    phase_F()
    P.barrier()
    P.emit()
    return nc


def rope_table(S):
    rows = S // 64
    row_id = np.repeat(np.arange(rows, dtype=np.float32), 64)
    col_id = np.tile(np.arange(64, dtype=np.float32), rows)
    n_pairs = 16
    freqs = np.exp(-np.log(np.float32(10000.0)) * np.arange(n_pairs, dtype=np.float32) / n_pairs).astype(np.float32)
    ang = np.concatenate([row_id[:, None] * freqs[None, :], col_id[:, None] * freqs[None, :]], axis=-1)
    cos, sin = np.cos(ang).astype(np.float32), np.sin(ang).astype(np.float32)
    tab = np.zeros((S, 128), np.float32)
    tab[:, 0:64:2] = cos
    tab[:, 1:64:2] = cos
    tab[:, 64:96] = -sin
    tab[:, 96:128] = sin
    return tab


WNAMES = ['mix_norm_g', 'w_in', 'gm_v_norm_g', 'gm_w_s', 'gm_b_s', 'q_norm_g', 'k_norm_g', 'branch_norm_g',
          'w_out', 'xattn_norm_g', 'xattn_w_q', 'xattn_w_kv', 'xattn_w_o', 'ffn_norm_g', 'w_router',
          'w_gate', 'w_up', 'w_down']


def make_in_maps(inputs, S, L, nb, split=1):
    shared = {k: np.ascontiguousarray(np.asarray(inputs[k], dtype=np.float32)[:L]) for k in WNAMES}
    shared['mem_norm_g'] = np.asarray(inputs['mem_norm_g'], np.float32).reshape(1, D)
    shared['final_norm_g'] = np.asarray(inputs['final_norm_g'], np.float32).reshape(1, D)
    rope = rope_table(S)
    SL = S // split
    shared['identf'] = np.eye(128, dtype=np.float32)
    shared['identb'] = np.eye(128, dtype=np.float32).astype(ml_dtypes.bfloat16)
    shared['triu'] = np.triu(np.ones((128, 128), np.float32)).astype(ml_dtypes.bfloat16)
    shared['iota32'] = np.tile(np.arange(32, dtype=np.float32)[None, :], (128, 1))
    shared['pidx'] = np.arange(128, dtype=np.float32).reshape(128, 1)
    maps = []
    for b in range(nb):
        for r in range(split):
            m = dict(shared)
            m['x'] = np.ascontiguousarray(np.asarray(inputs['x'], np.float32)[b, r * SL:(r + 1) * SL])
            m['rope'] = np.ascontiguousarray(rope[r * SL:(r + 1) * SL])
            m['mem'] = np.ascontiguousarray(np.asarray(inputs['mem'], np.float32)[b])
            if split > 1:
                m['sel'] = np.stack([(split * b + q) * 128 + np.arange(128) for q in range(split)], axis=1).astype(np.int32)
            maps.append(m)
    return maps


def kernel(**inputs):
    x = np.asarray(inputs['x'])
    B, S, _ = x.shape
    L = np.asarray(inputs['w_in']).shape[0]
    nc = build(S, L)
    maps = make_in_maps(inputs, S, L, B)
    res = run_bass_kernel_spmd(nc, maps, core_ids=list(range(B)))
    return np.stack([np.asarray(r['out'], dtype=np.float32) for r in res.results], axis=0)
```
